# Optimizing a Trainium2 kernel written in Bass

```python
import math
import jax, jax.numpy as jnp
from jax import lax
import numpy as np

D_MODEL = 2048
BATCH = 4
SEQ = 4096
DEPTH = 2

HEAD_DIM = 128
A_HEADS = 6
A_HALF = HEAD_DIM // 2
A_WIDTH = A_HEADS * HEAD_DIM
B_HEADS = 6
B_WIDTH = B_HEADS * HEAD_DIM
B_Q_RANK = 512
B_KV_RANK = 256
IDX_HEADS = 16
IDX_DIM = 64
TOPK_MAX = 256
C_GROUPS = 4
C_GROUP_DIM = 128
C_WIDTH = C_GROUPS * C_GROUP_DIM
CHUNK = 128
MIX_WIDTH = A_WIDTH + B_WIDTH + C_WIDTH
IN_SIZES = (A_WIDTH, A_WIDTH, A_WIDTH, B_Q_RANK, B_KV_RANK, IDX_DIM, IDX_HEADS, C_WIDTH, C_WIDTH)
IN_COLS = sum(IN_SIZES)
MEM_LEN = 256
X_HEADS = 4
X_DIM = 128
FFN_HIDDEN = ((-(-8 * D_MODEL // 3) + 255) // 256) * 256
REL_BUCKETS = 32
REL_MAX_DIST = 128
REL_HEADS = A_HEADS + B_HEADS
Q_BLOCK = 128
EPS = 1e-6

kernel_name = "hybrid_diffattn_dsa_gmlp_block"


def rms_norm(x, g):
    xf = x.astype(jnp.float32)
    y = xf * lax.rsqrt(jnp.mean(xf * xf, axis=-1, keepdims=True) + EPS)
    return (y * g.astype(jnp.float32)).astype(x.dtype)


def layer_norm(x, g, b):
    xf = x.astype(jnp.float32)
    mu = jnp.mean(xf, axis=-1, keepdims=True)
    xc = xf - mu
    var = jnp.mean(xc * xc, axis=-1, keepdims=True)
    return (xc * lax.rsqrt(var + EPS) * g.astype(jnp.float32) + b.astype(jnp.float32)).astype(x.dtype)


def rel_bucket(dist):
    n = jnp.maximum(dist, 0)
    max_exact = REL_BUCKETS // 2
    nf = jnp.maximum(n, 1).astype(jnp.float32)
    large = max_exact + (jnp.log(nf / max_exact) / math.log(REL_MAX_DIST / max_exact)
                         * (REL_BUCKETS - max_exact)).astype(jnp.int32)
    large = jnp.minimum(large, REL_BUCKETS - 1)
    return jnp.where(n < max_exact, n, large)


def to_blocks(t):
    b, s = t.shape[:2]
    return jnp.moveaxis(t.reshape((b, s // Q_BLOCK, Q_BLOCK) + t.shape[2:]), 1, 0)


def from_blocks(t):
    t = jnp.moveaxis(t, 0, 1)
    return t.reshape((t.shape[0], t.shape[1] * t.shape[2]) + t.shape[3:])


def diff_attention(q, k, v, lam, bias_tab, sub_g, lam_init):
    s_len = q.shape[1]
    kpos = jnp.arange(s_len)
    scale = A_HALF ** -0.5

    def block(args):
        qi, i = args
        qpos = i * Q_BLOCK + jnp.arange(Q_BLOCK)
        dist = qpos[:, None] - kpos[None, :]
        bias = jnp.transpose(bias_tab[rel_bucket(dist)], (2, 0, 1)).astype(jnp.float32)
        logits = jnp.einsum('bqhmd,bshmd->bhmqs', qi, k).astype(jnp.float32) * scale
        logits = logits + bias[None, :, None]
        logits = jnp.where((dist >= 0)[None, None, None], logits, -jnp.inf)
        p = jax.nn.softmax(logits, axis=-1)
        attn = p[:, :, 0] - lam * p[:, :, 1]
        return jnp.einsum('bhqs,bshd->bqhd', attn.astype(v.dtype), v)

    nblk = s_len // Q_BLOCK
    o = from_blocks(lax.map(block, (to_blocks(q), jnp.arange(nblk))))
    return rms_norm(o, sub_g) * (1.0 - lam_init)


def dsa_attention(c_q, c_kv, k_idx, w_idx, w_uq, w_qidx, w_uk, w_uv, bias_tab):
    bsz, s_len = c_q.shape[:2]
    topk = min(TOPK_MAX, s_len // 4)
    q = jnp.einsum('bsr,rhd->bshd', c_q, w_uq)
    q_lat = jnp.einsum('bshd,rhd->bshr', q, w_uk)
    q_idx = jnp.einsum('bsr,rgd->bsgd', c_q, w_qidx)
    w_idx = w_idx * (IDX_HEADS ** -0.5)
    kpos = jnp.arange(s_len)
    bidx = jnp.arange(bsz)[:, None, None]

    def block(args):
        qi, wi, ql, i = args
        qpos = i * Q_BLOCK + jnp.arange(Q_BLOCK)
        dots = jnp.einsum('bqgd,bsd->bqsg', qi, k_idx) * (IDX_DIM ** -0.5)
        score = jnp.einsum('bqsg,bqg->bqs', jax.nn.relu(dots), wi).astype(jnp.float32)
        causal = kpos[None, :] <= qpos[:, None]
        score = jnp.where(causal[None], score, -jnp.inf)
        top_score, idx = lax.top_k(score, topk)
        valid = jnp.isfinite(top_score)
        c_sel = c_kv[bidx, idx]
        dist = qpos[None, :, None] - idx
        bias = jnp.transpose(bias_tab[rel_bucket(dist)], (0, 3, 1, 2)).astype(jnp.float32)
        logits = jnp.einsum('bqhr,bqkr->bhqk', ql, c_sel).astype(jnp.float32) * (HEAD_DIM ** -0.5) + bias
        logits = jnp.where(valid[:, None], logits, -jnp.inf)
        p = jax.nn.softmax(logits, axis=-1)
        return jnp.einsum('bhqk,bqkr->bqhr', p.astype(c_sel.dtype), c_sel)

    nblk = s_len // Q_BLOCK
    o_lat = from_blocks(lax.map(block, (to_blocks(q_idx), to_blocks(w_idx), to_blocks(q_lat),
                                        jnp.arange(nblk))))
    return jnp.einsum('bshr,rhd->bshd', o_lat, w_uv)


def chunk_spatial_gating(u, v, ln_g, ln_b, w_s, b_s):
    v = layer_norm(v, ln_g, ln_b)
    bsz, s_len = v.shape[:2]
    vc = v.reshape(bsz, s_len // CHUNK, CHUNK, C_GROUPS, C_GROUP_DIM)
    mask = jnp.tril(jnp.ones((CHUNK, CHUNK), dtype=bool))
    w = jnp.where(mask[None], w_s, jnp.zeros_like(w_s))
    y = jnp.einsum('gts,bnsgc->bntgc', w, vc) + jnp.transpose(b_s)[None, None, :, :, None]
    return u * y.reshape(bsz, s_len, C_WIDTH)


def memory_attention(h, mem_n, wq, wkv, wo):
    q = jnp.einsum('bsd,dhe->bshe', h, wq)
    kv = jnp.einsum('bmd,dche->bmche', mem_n, wkv)
    k, v = kv[:, :, 0], kv[:, :, 1]
    logits = jnp.einsum('bshe,bmhe->bhsm', q, k).astype(jnp.float32) * (X_DIM ** -0.5)
    p = jax.nn.softmax(logits, axis=-1)
    o = jnp.einsum('bhsm,bmhe->bshe', p.astype(v.dtype), v)
    return jnp.einsum('bshe,hed->bsd', o, wo)


def swiglu(h, w_gu, w_down):
    gu = jnp.einsum('bsd,dcf->bscf', h, w_gu)
    return jnp.einsum('bsf,fd->bsd', jax.nn.silu(gu[:, :, 0]) * gu[:, :, 1], w_down)


def setup_inputs(seed: int = 0) -> dict:
    key = jax.random.key(seed)
    ks = iter(jax.random.split(key, 40))
    f32 = jnp.float32

    def nrm(shape, scale):
        return jax.random.normal(next(ks), shape, f32) * scale

    def gain(shape):
        return 1.0 + 0.05 * jax.random.normal(next(ks), shape, f32)

    L = DEPTH
    return {
        "x": nrm((BATCH, SEQ, D_MODEL), 1.0),
        "mem": nrm((BATCH, MEM_LEN, D_MODEL), 1.0),
        "rel_bias": nrm((REL_BUCKETS, REL_HEADS), 0.5),
        "mix_pre_g": gain((L, D_MODEL)),
        "mix_post_g": gain((L, D_MODEL)),
        "w_in": nrm((L, D_MODEL, IN_COLS), D_MODEL ** -0.5),
        "w_out": nrm((L, MIX_WIDTH, D_MODEL), MIX_WIDTH ** -0.5),
        "a_lambda": nrm((L, 4, A_HALF), 0.1),
        "a_sub_g": gain((L, HEAD_DIM)),
        "b_cq_g": gain((L, B_Q_RANK)),
        "b_ckv_g": gain((L, B_KV_RANK)),
        "b_w_uq": nrm((L, B_Q_RANK, B_HEADS, HEAD_DIM), B_Q_RANK ** -0.5),
        "b_w_qidx": nrm((L, B_Q_RANK, IDX_HEADS, IDX_DIM), B_Q_RANK ** -0.5),
        "b_w_uk": nrm((L, B_KV_RANK, B_HEADS, HEAD_DIM), B_KV_RANK ** -0.5),
        "b_w_uv": nrm((L, B_KV_RANK, B_HEADS, HEAD_DIM), B_KV_RANK ** -0.5),
        "c_ln_g": gain((L, C_WIDTH)),
        "c_ln_b": nrm((L, C_WIDTH), 0.02),
        "c_w_s": nrm((L, C_GROUPS, CHUNK, CHUNK), CHUNK ** -0.5),
        "c_b_s": 1.0 + nrm((L, C_GROUPS, CHUNK), 0.1),
        "x_pre_g": gain((L, D_MODEL)),
        "x_post_g": gain((L, D_MODEL)),
        "mem_g": gain((L, D_MODEL)),
        "x_wq": nrm((L, D_MODEL, X_HEADS, X_DIM), D_MODEL ** -0.5),
        "x_wkv": nrm((L, D_MODEL, 2, X_HEADS, X_DIM), D_MODEL ** -0.5),
        "x_wo": nrm((L, X_HEADS, X_DIM, D_MODEL), (X_HEADS * X_DIM) ** -0.5),
        "f_pre_g": gain((L, D_MODEL)),
        "f_post_g": gain((L, D_MODEL)),
        "f_w_gu": nrm((L, D_MODEL, 2, FFN_HIDDEN), D_MODEL ** -0.5),
        "f_w_down": nrm((L, FFN_HIDDEN, D_MODEL), FFN_HIDDEN ** -0.5),
    }


def reference(x, mem, rel_bias, mix_pre_g, mix_post_g, w_in, w_out, a_lambda, a_sub_g,
              b_cq_g, b_ckv_g, b_w_uq, b_w_qidx, b_w_uk, b_w_uv,
              c_ln_g, c_ln_b, c_w_s, c_b_s,
              x_pre_g, x_post_g, mem_g, x_wq, x_wkv, x_wo,
              f_pre_g, f_post_g, f_w_gu, f_w_down):
    bsz, s_len, _ = x.shape
    bias_a = rel_bias[:, :A_HEADS]
    bias_b = rel_bias[:, A_HEADS:]
    split_at = np.cumsum(IN_SIZES)[:-1].tolist()
    h = x
    for l in range(DEPTH):
        lam_init = 0.8 - 0.6 * math.exp(-0.3 * l)
        xn = rms_norm(h, mix_pre_g[l])
        proj = jnp.einsum('bsd,dc->bsc', xn, w_in[l])
        qa, ka, va, cq, ckv, kidx, widx, cu, cv = jnp.split(proj, split_at, axis=-1)
        lp = a_lambda[l].astype(jnp.float32)
        lam = jnp.exp(jnp.sum(lp[0] * lp[1])) - jnp.exp(jnp.sum(lp[2] * lp[3])) + lam_init
        oa = diff_attention(qa.reshape(bsz, s_len, A_HEADS, 2, A_HALF),
                            ka.reshape(bsz, s_len, A_HEADS, 2, A_HALF),
                            va.reshape(bsz, s_len, A_HEADS, HEAD_DIM),
                            lam, bias_a, a_sub_g[l], lam_init)
        ob = dsa_attention(rms_norm(cq, b_cq_g[l]), rms_norm(ckv, b_ckv_g[l]), kidx, widx,
                           b_w_uq[l], b_w_qidx[l], b_w_uk[l], b_w_uv[l], bias_b)
        oc = chunk_spatial_gating(jax.nn.gelu(cu), jax.nn.gelu(cv), c_ln_g[l], c_ln_b[l],
                                  c_w_s[l], c_b_s[l])
        mix = jnp.concatenate([oa.reshape(bsz, s_len, A_WIDTH),
                               ob.reshape(bsz, s_len, B_WIDTH), oc], axis=-1)
        h = h + rms_norm(jnp.einsum('bsc,cd->bsd', mix, w_out[l]), mix_post_g[l])
        xa = memory_attention(rms_norm(h, x_pre_g[l]), rms_norm(mem, mem_g[l]),
                              x_wq[l], x_wkv[l], x_wo[l])
        h = h + rms_norm(xa, x_post_g[l])
        ff = swiglu(rms_norm(h, f_pre_g[l]), f_w_gu[l], f_w_down[l])
        h = h + rms_norm(ff, f_post_g[l])
    return h
```

```python
import math
from contextlib import ExitStack
import numpy as np
import concourse.bass as bass
import concourse.mybir as mybir
from concourse.bass_utils import run_bass_kernel_spmd

F32 = mybir.dt.float32
BF16 = mybir.dt.bfloat16
AF = mybir.ActivationFunctionType
ALU = mybir.AluOpType
AX = mybir.AxisListType

D = 2048
NDC = 16
IN_COLS = 4176
FFN = 5632
NFC = 44
EPS = 1e-6
TOPK = 256
NBIS = 18
NEG = -30000.0

VP_GIN, VP_CQ, VP_CKV, VP_XPRE, VP_MEMG, VP_FPRE, VP_SUBG = 0, 16, 20, 22, 38, 54, 70
NVP = 71
VB_MIXPOST, VB_XPOST, VB_FPOST, VB_LNG, VB_LNB, VB_BS, VB_LAM = 0, 2048, 4096, 6144, 6656, 7168, 7680
VB_FPRE = 7936
NVB = 9984


class Buf:
    __slots__ = ("w", "r", "dsem")

    def __init__(self):
        self.w = None
        self.r = {}
        self.dsem = None


class _Sem:
    def __init__(self):
        self.sem = None
        self.n = 0


class Eng:
    def __init__(self, name):
        self.name = name
        self.sem = None
        self.n = 0
        self.prog = []
        self.waited = {}

    def wait(self, tok):
        src, val = tok
        if self.waited.get(id(src), 0) >= val:
            return
        self.waited[id(src)] = val
        self.prog.append(lambda h, src=src, val=val: h.wait_ge(src.sem, val))


class Phase:
    def __init__(self, kern, name):
        self.kern = kern
        self.nc = kern.nc
        self.name = name
        self.pe, self.dve, self.act, self.pool, self.sp = kern.engs
        self.engs = kern.engs
        for e in self.engs:
            e.prog = []
        self.dsems = []
        self.npool = 0

    @staticmethod
    def _deps(reads, writes):
        deps = []
        for b in reads:
            if b.w is not None:
                deps.append(b.w)
        for b in writes:
            if b.w is not None:
                deps.append(b.w)
            deps.extend(b.r.values())
        return deps

    @staticmethod
    def _commit(tok, reads, writes):
        k = id(tok[0])
        for b in reads:
            b.r[k] = tok
        for b in writes:
            b.w = tok
            b.r = {}

    def op(self, eng, fn, reads=(), writes=(), pe_acc=False):
        for d in self._deps(reads, writes):
            if pe_acc and d[0] is eng:
                continue
            eng.wait(d)
        eng.n += 1
        tok = (eng, eng.n)
        eng.prog.append(lambda h, fn=fn, eng=eng: fn(h).then_inc(eng.sem, 1))
        self._commit(tok, reads, writes)
        return tok

    def dma(self, out, in_, reads=(), writes=(), sbuf=None, **kw):
        eng = self.sp
        for d in self._deps(reads, writes):
            eng.wait(d)
        if sbuf.dsem is None:
            sbuf.dsem = self.kern.get_dsem(self.npool)
            self.npool += 1
            self.dsems.append(sbuf.dsem)
        ds = sbuf.dsem
        ds.n += 16
        tok = (ds, ds.n)
        eng.prog.append(lambda h, out=out, in_=in_, kw=kw, ds=ds: h.dma_start(out=out, in_=in_, **kw).then_inc(ds.sem, 16))
        self._commit(tok, reads, writes)
        return tok

    def load(self, out, in_, buf, **kw):
        return self.dma(out, in_, writes=[buf], sbuf=buf, **kw)

    def store(self, out, in_, buf, **kw):
        return self.dma(out, in_, reads=[buf], sbuf=buf, **kw)

    def emit(self, st):
        nc = self.nc
        for ds in self.dsems:
            self.sp.wait((ds, ds.n))
        block = st.enter_context(nc.Block(self.name))
        for e in self.engs:
            def body(h, e=e):
                for c in e.prog:
                    c(h)
            getattr(block, e.name)(body)


class Kern:
    def __init__(self, S, L, debug=False):
        self.S, self.L, self.debug = S, L, debug
        self.nb = S // 128
        self.ng = S // 512
        nc = self.nc = bass.Bass("TRN2", target_bir_lowering=False)
        self.gst = ExitStack()
        self.engs = [Eng(n) for n in ("tensor", "vector", "scalar", "gpsimd", "sync")]
        for e in self.engs:
            e.sem = self.gst.enter_context(nc.semaphore(f"prog_{e.name}"))
        self.dpool = []

        self._ext_shapes = {
            "x": [S, D], "mem": [256, D], "w_in": [L, D, IN_COLS], "w_out": [L, D, D],
            "b_w_uq": [L, 512, 768], "b_w_qidx": [L, 512, 1024], "b_w_uk": [L, 256, 768], "b_w_uv": [L, 256, 768],
            "cwsT": [L, 128, 512], "x_wq": [L, D, 512], "x_wkv": [L, D, 1024], "x_wo": [L, 512, D],
            "f_w_gu": [L, D, 2, FFN], "f_w_down": [L, FFN, D], "vecP": [L, 128, NVP], "vecB": [L, 128, NVB],
            "biasT": [128, 12 * 2 * 128], "b31": [128, 12], "consts": [128, 128 * 4 + 32],
        }
        self._ext = {}
        self.out = nc.dram_tensor("out", [S, D], F32, kind="ExternalOutput").ap()

        def scr(name, shape, dt):
            kind = "ExternalOutput" if debug else "Internal"
            return nc.dram_tensor(name, list(shape), dt, kind=kind).ap()

        self.h = scr("h_scr", [S, D], F32)
        self.qaT = scr("qaT", [6, 128, S], BF16)
        self.kaT = scr("kaT", [6, 128, S], BF16)
        self.va = scr("va", [S, 768], BF16)
        self.qbT = scr("qbT", [6, 128, S], BF16)
        self.kbT = scr("kbT", [6, 128, S], BF16)
        self.vb = scr("vb", [S, 768], BF16)
        self.qidxT = scr("qidxT", [8, 128, S], BF16)
        self.kidxT = scr("kidxT", [64, S], BF16)
        self.widx = scr("widx", [S, 16], F32)
        self.mixT = scr("mixT", [16, 128, S], BF16)
        self.MTd = scr("MTd", [self.ng, 128, self.nb, 512], BF16)
        self.yscr = scr("yscr", [S, D], F32)
        self.kmT = scr("kmT", [128, 4, 256], BF16)
        self.vm = scr("vm", [256, 512], BF16)

    def get_dsem(self, i):
        while len(self.dpool) <= i:
            d = _Sem()
            d.sem = self.gst.enter_context(self.nc.semaphore(f"dma_{len(self.dpool)}"))
            self.dpool.append(d)
        return self.dpool[i]

    def E(self, name):
        if name not in self._ext:
            self._ext[name] = self.nc.dram_tensor(name, list(self._ext_shapes[name]), F32, kind="ExternalInput").ap()
        return self._ext[name]

    def build(self, phases=None):
        want = lambda p: phases is None or p in phases
        for l in range(self.L):
            src = self.E("x") if l == 0 else self.h
            if want("p1a"):
                self.p1a(l, src)
            if want("p1b"):
                self.p1b(l, src)
            if want("attA"):
                self.attn(l, "A")
            if want("bidx"):
                self.bindex(l)
            if want("attB"):
                self.attn(l, "B")
            if want("p3"):
                self.memkv(l)
                self.p3(l)
            if want("ffn"):
                self.ffn(l)
                self.ffn_post(l, self.out if l == self.L - 1 else self.h)
        self.gst.close()
        return self.nc

    def begin(self, name, st, want=("ident",)):
        nc = self.nc
        ph = Phase(self, name)
        sb = lambda nm, shape, dt: st.enter_context(nc.sbuf_tensor(f"{name}_{nm}", list(shape), dt))
        pst = lambda nm, shape, dt: st.enter_context(nc.psum_tensor(f"{name}_{nm}", list(shape), dt))
        cs = self.consts_load(ph, sb, want)
        epsT = sb("epsT", [128, 1], F32); b_eps = Buf()
        ph.op(ph.dve, lambda h: h.memset(epsT[:], EPS), writes=[b_eps])
        self.eps_ap = epsT[:, 0:1]
        self.b_eps = b_eps
        return ph, sb, pst, cs

    def consts_load(self, ph, sb, want):
        res = {}
        if "ident" in want:
            cf = sb("c_idf", [128, 128], F32); bcf = Buf()
            ph.load(cf[:], self.E("consts")[:, 0:128], bcf)
            ib = sb("c_idb", [128, 128], BF16); bib = Buf()
            ph.op(ph.dve, lambda h: h.tensor_copy(out=ib[:], in_=cf[:]), reads=[bcf], writes=[bib])
            res["ident"] = (ib, bib)
        for nm, c0, n in (("mask01T", 128, 128), ("causneg", 256, 128), ("causnegT", 384, 128), ("pow2", 512, 32)):
            if nm in want:
                t = sb("c_" + nm, [128, n], F32); bt = Buf()
                ph.load(t[:], self.E("consts")[:, c0:c0 + n], bt)
                res[nm] = (t, bt)
        return res

    def rms_tile(self, ph, ht, b_ht, junk, b_junk, ss, rt, rinv, b_small, width, act_in=None):
        src = ht if act_in is None else act_in
        ph.op(ph.act, lambda h: h.activation(out=junk, in_=src, func=AF.Square, accum_out=ss), reads=[b_ht], writes=[b_junk, b_small])
        ph.op(ph.act, lambda h: h.activation(out=rt, in_=ss, func=AF.Sqrt, scale=1.0 / width, bias=self.eps_ap), reads=[b_small, self.b_eps], writes=[b_small])
        ph.op(ph.dve, lambda h: h.reciprocal(out=rinv, in_=rt), reads=[b_small], writes=[b_small])

    def p1a(self, l, src):
        nc, S, nb = self.nc, self.S, self.nb
        with ExitStack() as st:
            ph, sb, pst, cs = self.begin(f"p1a{l}", st)
            ident, b_ident = cs["ident"]
            vp = sb("vp", [128, NVP], F32); b_vp = Buf()
            ph.load(vp[:], self.E("vecP")[l], b_vp)
            NW = 2304
            win = sb("win", [128, NDC, NW], BF16); b_win = Buf()
            stg = [sb(f"stg{i}", [128, NW], F32) for i in range(2)]; b_stg = [Buf(), Buf()]
            for dc in range(NDC):
                s = dc % 2
                ph.load(stg[s][:], self.E("w_in")[l, dc * 128:(dc + 1) * 128, 0:NW], b_stg[s])
                ph.op(ph.dve, lambda h, s=s, dc=dc: h.tensor_scalar(out=win[:, dc, :], in0=stg[s][:], scalar1=vp[:, VP_GIN + dc:VP_GIN + dc + 1], scalar2=None, op0=ALU.mult),
                      reads=[b_stg[s], b_vp], writes=[b_win])
            hts = [sb(f"ht{i}", [128, D], F32) for i in range(2)]; b_ht = [Buf(), Buf()]
            junk = sb("junk", [128, D], BF16); b_junk = Buf()
            sm = sb("sm", [128, 4], F32); b_sm = Buf()
            xn = sb("xn", [128, D], BF16); b_xn = Buf()
            xnT = sb("xnT", [128, NDC, 128], BF16); b_xnT = Buf()
            fm = sb("fm", [128, 12, 128], BF16); b_fm = Buf()
            vas = sb("vas", [128, 768], BF16); b_vas = Buf()
            psB = pst("psB", [128, 2, 1024], BF16); b_psB = [Buf(), Buf()]
            psF = pst("psF", [128, 5, 512], F32); b_psF = [Buf() for _ in range(5)]

            def ld(i):
                ph.load(hts[i % 2][:], src[i * 128:(i + 1) * 128, :], b_ht[i % 2])

            ld(0)
            for i in range(nb):
                if i + 1 < nb:
                    ld(i + 1)
                ht, bh = hts[i % 2], b_ht[i % 2]
                self.rms_tile(ph, ht[:], bh, junk[:], b_junk, sm[:, 0:1], sm[:, 1:2], sm[:, 2:3], b_sm, D)
                ph.op(ph.dve, lambda h, ht=ht: h.tensor_scalar(out=xn[:], in0=ht[:], scalar1=sm[:, 2:3], scalar2=None, op0=ALU.mult),
                      reads=[bh, b_sm], writes=[b_xn])
                for dc in range(NDC):
                    ph.op(ph.pe, lambda h, dc=dc: h.transpose(out=psB[:, dc // 8, (dc % 8) * 128:(dc % 8 + 1) * 128], in_=xn[:, dc * 128:(dc + 1) * 128], identity=ident[:]),
                          reads=[b_xn, b_ident], writes=[b_psB[dc // 8]], pe_acc=True)
                for k in range(2):
                    ph.op(ph.act, lambda h, k=k: h.copy(out=xnT[:, k * 8:(k + 1) * 8, :], in_=psB[:, k, :]), reads=[b_psB[k]], writes=[b_xnT])
                for c in range(12):
                    bk = c // 4
                    for dc in range(NDC):
                        ph.op(ph.pe, lambda h, c=c, dc=dc, bk=bk: h.matmul(psF[:, bk, (c % 4) * 128:(c % 4 + 1) * 128], lhsT=win[:, dc, c * 128:(c + 1) * 128], rhs=xnT[:, dc, :], start=(dc == 0), stop=(dc == NDC - 1)),
                              reads=[b_win, b_xnT], writes=[b_psF[bk]], pe_acc=True)
                for (bk, c0, n) in ((3, 0, 512), (4, 512, 256)):
                    for dc in range(NDC):
                        ph.op(ph.pe, lambda h, bk=bk, c0=c0, n=n, dc=dc: h.matmul(psF[:, bk, 0:n], lhsT=xnT[:, dc, :], rhs=win[:, dc, 1536 + c0:1536 + c0 + n], start=(dc == 0), stop=(dc == NDC - 1)),
                              reads=[b_win, b_xnT], writes=[b_psF[bk]], pe_acc=True)
                for bk in range(3):
                    eng = ph.act if bk % 2 == 0 else ph.dve
                    if eng is ph.act:
                        ph.op(eng, lambda h, bk=bk: h.copy(out=fm[:, bk * 4:(bk + 1) * 4, :], in_=psF[:, bk, :]), reads=[b_psF[bk]], writes=[b_fm])
                    else:
                        ph.op(eng, lambda h, bk=bk: h.tensor_copy(out=fm[:, bk * 4:(bk + 1) * 4, :], in_=psF[:, bk, :]), reads=[b_psF[bk]], writes=[b_fm])
                ph.op(ph.dve, lambda h: h.tensor_copy(out=vas[:, 0:512], in_=psF[:, 3, :]), reads=[b_psF[3]], writes=[b_vas])
                ph.op(ph.act, lambda h: h.copy(out=vas[:, 512:768], in_=psF[:, 4, 0:256]), reads=[b_psF[4]], writes=[b_vas])
                tk = slice(i * 128, (i + 1) * 128)
                ph.store(self.qaT[:, :, tk].rearrange("h p t -> p h t"), fm[:, 0:6, :], b_fm)
                ph.store(self.kaT[:, :, tk].rearrange("h p t -> p h t"), fm[:, 6:12, :], b_fm)
                ph.store(self.va[tk, :], vas[:], b_vas)
            ph.emit(st)

    def p1b(self, l, src):
        nc, S, nb = self.nc, self.S, self.nb
        with ExitStack() as st:
            ph, sb, pst, cs = self.begin(f"p1b{l}", st, want=("ident", "mask01T"))
            ident, b_ident = cs["ident"]
            m01, b_m01 = cs["mask01T"]
            vp = sb("vp", [128, NVP], F32); b_vp = Buf()
            ph.load(vp[:], self.E("vecP")[l], b_vp)
            vb3 = sb("vb3", [128, 1536], F32); b_vb3 = Buf()
            ph.load(vb3[:], self.E("vecB")[l, :, VB_LNG:VB_LNG + 1536], b_vb3)
            NW = 1872
            C0 = 2304
            win = sb("win", [128, NDC, NW], BF16); b_win = Buf()
            wuq = sb("wuq", [128, 4, 768], BF16); b_wuq = Buf()
            wqi = sb("wqi", [128, 4, 1024], BF16); b_wqi = Buf()
            wuk = sb("wuk", [128, 2, 768], BF16); b_wuk = Buf()
            wuv = sb("wuv", [128, 2, 768], BF16); b_wuv = Buf()
            cws = sb("cws", [128, 512], BF16); b_cws = Buf()
            stg = [sb(f"stg{i}", [128, NW], F32) for i in range(2)]; b_stg = [Buf(), Buf()]
            cnt = [0]

            def wload(dst, b_dst, srcap, n, gcol):
                s_ = cnt[0] % 2; cnt[0] += 1
                ph.load(stg[s_][:, 0:n], srcap, b_stg[s_])
                ph.op(ph.dve, lambda h: h.tensor_scalar(out=dst, in0=stg[s_][:, 0:n], scalar1=vp[:, gcol:gcol + 1], scalar2=None, op0=ALU.mult),
                      reads=[b_stg[s_], b_vp], writes=[b_dst])

            for dc in range(NDC):
                wload(win[:, dc, :], b_win, self.E("w_in")[l, dc * 128:(dc + 1) * 128, C0:C0 + NW], NW, VP_GIN + dc)
            for rc in range(4):
                wload(wuq[:, rc, :], b_wuq, self.E("b_w_uq")[l, rc * 128:(rc + 1) * 128, :], 768, VP_CQ + rc)
                wload(wqi[:, rc, :], b_wqi, self.E("b_w_qidx")[l, rc * 128:(rc + 1) * 128, :], 1024, VP_CQ + rc)
            for rc in range(2):
                wload(wuk[:, rc, :], b_wuk, self.E("b_w_uk")[l, rc * 128:(rc + 1) * 128, :], 768, VP_CKV + rc)
                wload(wuv[:, rc, :], b_wuv, self.E("b_w_uv")[l, rc * 128:(rc + 1) * 128, :], 768, VP_CKV + rc)
            s_ = cnt[0] % 2; cnt[0] += 1
            ph.load(stg[s_][:, 0:512], self.E("cwsT")[l], b_stg[s_])
            for g in range(4):
                ph.op(ph.dve, lambda h, g=g, s_=s_: h.tensor_tensor(out=cws[:, g * 128:(g + 1) * 128], in0=stg[s_][:, g * 128:(g + 1) * 128], in1=m01[:], op=ALU.mult),
                      reads=[b_stg[s_], b_m01], writes=[b_cws])

            hts = [sb(f"ht{i}", [128, D], F32) for i in range(2)]; b_ht = [Buf(), Buf()]
            junk = sb("junk", [128, D], BF16); b_junk = Buf()
            sm = sb("sm", [128, 16], F32); b_sm = Buf()
            xn = sb("xn", [128, D], BF16); b_xn = Buf()
            xnT = sb("xnT", [128, NDC, 128], BF16); b_xnT = Buf()
            cn = sb("cn", [128, 768], BF16); b_cn = Buf()
            cT = sb("cT", [128, 6, 128], BF16); b_cT = Buf()
            gv = sb("gv", [128, 512], F32); b_gv = Buf()
            tmp = sb("tmp", [128, 512], F32); b_tmp = Buf()
            vn = sb("vn", [128, 512], BF16); b_vn = Buf()
            uT = sb("uT", [128, 4, 128], F32); b_uT = Buf()
            fm = sb("fm", [128, 24, 128], BF16); b_fm = Buf()
            kix = sb("kix", [64, 128], BF16); b_kix = Buf()
            vbs = sb("vbs", [128, 768], BF16); b_vbs = Buf()
            wix = sb("wix", [128, 16], F32); b_wix = Buf()
            psB = pst("psB", [128, 2, 1024], BF16); b_psB = [Buf(), Buf()]
            psF = pst("psF", [128, 6, 512], F32); bF = [Buf() for _ in range(6)]
            WSC = (16 ** -0.5) * (64 ** -0.5)

            def ld(i):
                ph.load(hts[i % 2][:], src[i * 128:(i + 1) * 128, :], b_ht[i % 2])

            def mm(out, lhsT, rhs, first, last, reads, wbuf):
                ph.op(ph.pe, lambda h: h.matmul(out, lhsT=lhsT, rhs=rhs, start=first, stop=last), reads=reads, writes=[wbuf], pe_acc=True)

            ld(0)
            for i in range(nb):
                if i + 1 < nb:
                    ld(i + 1)
                ht, bh = hts[i % 2], b_ht[i % 2]
                self.rms_tile(ph, ht[:], bh, junk[:], b_junk, sm[:, 0:1], sm[:, 1:2], sm[:, 2:3], b_sm, D)
                ph.op(ph.dve, lambda h, ht=ht: h.tensor_scalar(out=xn[:], in0=ht[:], scalar1=sm[:, 2:3], scalar2=None, op0=ALU.mult),
                      reads=[bh, b_sm], writes=[b_xn])
                for dc in range(NDC):
                    ph.op(ph.pe, lambda h, dc=dc: h.transpose(out=psB[:, dc // 8, (dc % 8) * 128:(dc % 8 + 1) * 128], in_=xn[:, dc * 128:(dc + 1) * 128], identity=ident[:]),
                          reads=[b_xn, b_ident], writes=[b_psB[dc // 8]], pe_acc=True)
                for k in range(2):
                    ph.op(ph.act, lambda h, k=k: h.copy(out=xnT[:, k * 8:(k + 1) * 8, :], in_=psB[:, k, :]), reads=[b_psB[k]], writes=[b_xnT])
                for dc in range(NDC):
                    mm(psF[:, 0, :], xnT[:, dc, :], win[:, dc, 0:512], dc == 0, dc == NDC - 1, [b_win, b_xnT], bF[0])
                for dc in range(NDC):
                    mm(psF[:, 1, 0:336], xnT[:, dc, :], win[:, dc, 512:848], dc == 0, dc == NDC - 1, [b_win, b_xnT], bF[1])
                for dc in range(NDC):
                    mm(psF[:, 2, :], xnT[:, dc, :], win[:, dc, 1360:1872], dc == 0, dc == NDC - 1, [b_win, b_xnT], bF[2])
                for dc in range(NDC):
                    mm(psF[0:64, 3, 0:128], win[:, dc, 768:832], xnT[:, dc, :], dc == 0, dc == NDC - 1, [b_win, b_xnT], bF[3])
                for c in range(4):
                    bk, o = (3, 128 + c * 128) if c < 3 else (4, 0)
                    for dc in range(NDC):
                        mm(psF[:, bk, o:o + 128], win[:, dc, 848 + c * 128:848 + (c + 1) * 128], xnT[:, dc, :], dc == 0, dc == NDC - 1, [b_win, b_xnT], bF[bk])
                self.rms_tile(ph, None, bF[0], junk[:, 0:512], b_junk, sm[:, 3:4], sm[:, 4:5], sm[:, 5:6], b_sm, 512, act_in=psF[:, 0, :])
                ph.op(ph.dve, lambda h: h.tensor_scalar(out=cn[:, 0:512], in0=psF[:, 0, :], scalar1=sm[:, 5:6], scalar2=None, op0=ALU.mult), reads=[bF[0], b_sm], writes=[b_cn])
                self.rms_tile(ph, None, bF[1], junk[:, 0:256], b_junk, sm[:, 6:7], sm[:, 7:8], sm[:, 8:9], b_sm, 256, act_in=psF[:, 1, 0:256])
                ph.op(ph.dve, lambda h: h.tensor_scalar(out=cn[:, 512:768], in0=psF[:, 1, 0:256], scalar1=sm[:, 8:9], scalar2=None, op0=ALU.mult), reads=[bF[1], b_sm], writes=[b_cn])
                ph.op(ph.dve, lambda h: h.tensor_scalar(out=wix[:], in0=psF[:, 1, 320:336], scalar1=WSC, scalar2=None, op0=ALU.mult), reads=[bF[1]], writes=[b_wix])
                ph.op(ph.act, lambda h: h.copy(out=kix[:], in_=psF[0:64, 3, 0:128]), reads=[bF[3]], writes=[b_kix])
                ph.op(ph.act, lambda h: h.activation(out=uT[:, 0:3, :], in_=psF[:, 3, 128:512], func=AF.Gelu_apprx_tanh), reads=[bF[3]], writes=[b_uT])
                ph.op(ph.act, lambda h: h.activation(out=uT[:, 3, :], in_=psF[:, 4, 0:128], func=AF.Gelu_apprx_tanh), reads=[bF[4]], writes=[b_uT])
                ph.op(ph.act, lambda h: h.activation(out=gv[:], in_=psF[:, 2, :], func=AF.Gelu_apprx_tanh, accum_out=sm[:, 9:10]), reads=[bF[2]], writes=[b_gv, b_sm])
                ph.op(ph.dve, lambda h: h.tensor_scalar(out=sm[:, 10:11], in0=sm[:, 9:10], scalar1=-1.0 / 512, scalar2=None, op0=ALU.mult), reads=[b_sm], writes=[b_sm])
                ph.op(ph.act, lambda h: h.activation(out=junk[:, 0:512], in_=gv[:], func=AF.Square, bias=sm[:, 10:11], accum_out=sm[:, 11:12]), reads=[b_gv, b_sm], writes=[b_junk, b_sm])
                ph.op(ph.act, lambda h: h.activation(out=sm[:, 12:13], in_=sm[:, 11:12], func=AF.Sqrt, scale=1.0 / 512, bias=self.eps_ap), reads=[b_sm, self.b_eps], writes=[b_sm])
                ph.op(ph.dve, lambda h: h.reciprocal(out=sm[:, 13:14], in_=sm[:, 12:13]), reads=[b_sm], writes=[b_sm])
                ph.op(ph.dve, lambda h: h.tensor_scalar(out=tmp[:], in0=gv[:], scalar1=sm[:, 10:11], scalar2=sm[:, 13:14], op0=ALU.add, op1=ALU.mult), reads=[b_gv, b_sm], writes=[b_tmp])
                ph.op(ph.dve, lambda h: h.tensor_tensor(out=tmp[:], in0=tmp[:], in1=vb3[:, 0:512], op=ALU.mult), reads=[b_vb3], writes=[b_tmp])
                ph.op(ph.dve, lambda h: h.tensor_tensor(out=vn[:], in0=tmp[:], in1=vb3[:, 512:1024], op=ALU.add), reads=[b_tmp, b_vb3], writes=[b_vn])
                for k in range(6):
                    ph.op(ph.pe, lambda h, k=k: h.transpose(out=psB[:, 0, k * 128:(k + 1) * 128], in_=cn[:, k * 128:(k + 1) * 128], identity=ident[:]),
                          reads=[b_cn, b_ident], writes=[b_psB[0]], pe_acc=True)
                ph.op(ph.act, lambda h: h.copy(out=cT[:], in_=psB[:, 0, 0:768]), reads=[b_psB[0]], writes=[b_cT])
                def fm_group(bk, o, wt, b_wt, nrc, rc0, col0, reads_extra=()):
                    for rc in range(nrc):
                        mm(psF[:, bk, o:o + 128], wt[:, rc, col0:col0 + 128], cT[:, rc0 + rc, :], rc == 0, rc == nrc - 1, [b_wt, b_cT], bF[bk])
                for hh in range(4):
                    fm_group(0, hh * 128, wuq, b_wuq, 4, 0, hh * 128)
                for hh in range(4, 6):
                    fm_group(1, (hh - 4) * 128, wuq, b_wuq, 4, 0, hh * 128)
                for hh in range(2):
                    fm_group(1, 256 + hh * 128, wuk, b_wuk, 2, 4, hh * 128)
                for hh in range(2, 6):
                    fm_group(2, (hh - 2) * 128, wuk, b_wuk, 2, 4, hh * 128)
                for pr in range(4):
                    fm_group(3, pr * 128, wqi, b_wqi, 4, 0, pr * 128)
                for pr in range(4, 8):
                    fm_group(4, (pr - 4) * 128, wqi, b_wqi, 4, 0, pr * 128)
                for rc in range(2):
                    mm(psF[:, 5, :], cT[:, 4 + rc, :], wuv[:, rc, 0:512], rc == 0, rc == 1, [b_wuv, b_cT], bF[5])
                ph.op(ph.act, lambda h: h.copy(out=fm[:, 0:4, :], in_=psF[:, 0, :]), reads=[bF[0]], writes=[b_fm])
                ph.op(ph.dve, lambda h: h.tensor_copy(out=fm[:, 4:6, :], in_=psF[:, 1, 0:256]), reads=[bF[1]], writes=[b_fm])
                ph.op(ph.dve, lambda h: h.tensor_copy(out=fm[:, 6:8, :], in_=psF[:, 1, 256:512]), reads=[bF[1]], writes=[b_fm])
                ph.op(ph.act, lambda h: h.copy(out=fm[:, 8:12, :], in_=psF[:, 2, :]), reads=[bF[2]], writes=[b_fm])
                ph.op(ph.dve, lambda h: h.tensor_copy(out=fm[:, 12:16, :], in_=psF[:, 3, :]), reads=[bF[3]], writes=[b_fm])
                ph.op(ph.act, lambda h: h.copy(out=fm[:, 16:20, :], in_=psF[:, 4, :]), reads=[bF[4]], writes=[b_fm])
                ph.op(ph.dve, lambda h: h.tensor_copy(out=vbs[:, 0:512], in_=psF[:, 5, :]), reads=[bF[5]], writes=[b_vbs])
                for rc in range(2):
                    mm(psF[:, 0, 0:256], cT[:, 4 + rc, :], wuv[:, rc, 512:768], rc == 0, rc == 1, [b_wuv, b_cT], bF[0])
                ph.op(ph.act, lambda h: h.copy(out=vbs[:, 512:768], in_=psF[:, 0, 0:256]), reads=[bF[0]], writes=[b_vbs])
                for g in range(4):
                    mm(psF[:, 1, g * 128:(g + 1) * 128], vn[:, g * 128:(g + 1) * 128], cws[:, g * 128:(g + 1) * 128], True, True, [b_vn, b_cws], bF[1])
                ph.op(ph.dve, lambda h: h.tensor_tensor(out=tmp[:], in0=psF[:, 1, :], in1=vb3[:, 1024:1536], op=ALU.add), reads=[bF[1], b_vb3], writes=[b_tmp])
                ph.op(ph.dve, lambda h: h.tensor_tensor(out=fm[:, 20:24, :], in0=tmp[:], in1=uT[:], op=ALU.mult), reads=[b_tmp, b_uT], writes=[b_fm])
                tk = slice(i * 128, (i + 1) * 128)
                ph.store(self.qbT[:, :, tk].rearrange("h p t -> p h t"), fm[:, 0:6, :], b_fm)
                ph.store(self.kbT[:, :, tk].rearrange("h p t -> p h t"), fm[:, 6:12, :], b_fm)
                ph.store(self.qidxT[:, :, tk].rearrange("h p t -> p h t"), fm[:, 12:20, :], b_fm)
                ph.store(self.mixT[12:16, :, tk].rearrange("h p t -> p h t"), fm[:, 20:24, :], b_fm)
                ph.store(self.vb[tk, :], vbs[:], b_vbs)
                ph.store(self.kidxT[:, tk], kix[:], b_kix)
                ph.store(self.widx[tk, :], wix[:], b_wix)
            ph.emit(st)


    def attn(self, l, kind):
        nc, S, nb, ng = self.nc, self.S, self.nb, self.ng
        isA = kind == "A"
        qT_d, kT_d, v_d = (self.qaT, self.kaT, self.va) if isA else (self.qbT, self.kbT, self.vb)
        H0 = 0 if isA else 6
        nmap = 2 if isA else 1
        scale = 64 ** -0.5 if isA else 128 ** -0.5
        lam_init = 0.8 - 0.6 * math.exp(-0.3 * l)
        with ExitStack() as st:
            ph, sb, pst, cs = self.begin(f"att{kind}{l}", st, want=("ident", "causnegT"))
            ident, b_ident = cs["ident"]
            cneg, b_cneg = cs["causnegT"]
            kT = sb("kT", [128, 6, S], BF16); b_kT = Buf()
            ph.load(kT[:], kT_d.rearrange("h p t -> p h t"), b_kT)
            v1 = sb("v1", [128, nb, 6, 129], BF16); b_v1 = Buf()
            ph.op(ph.pool, lambda h: h.memset(v1[:, :, :, 128:129], 1.0), writes=[b_v1])
            for j0 in range(nb):
                ph.dma(v1[:, j0, :, 0:128], v_d[j0 * 128:(j0 + 1) * 128, :].rearrange("p (h d) -> p h d", d=128), writes=[b_v1], sbuf=b_v1)
            b31 = sb("b31", [128, 12], F32); b_b31 = Buf()
            ph.load(b31[:], self.E("b31"), b_b31)
            braw = sb("braw", [128, 6, 2, 128], F32); b_braw = Buf()
            ph.load(braw[:], self.E("biasT")[:, H0 * 256:(H0 + 6) * 256].rearrange("p (h k q) -> p h k q", h=6, k=2), b_braw)
            bt = sb("bt", [128, 6, 2, 128], BF16); b_bt = Buf()
            for hh in range(6):
                ph.op(ph.dve, lambda h, hh=hh: h.tensor_tensor(out=braw[:, hh, 0, :], in0=braw[:, hh, 0, :], in1=cneg[:], op=ALU.add), reads=[b_cneg], writes=[b_braw])
                ph.op(ph.dve, lambda h, hh=hh: h.tensor_scalar(out=bt[:, hh, :, :], in0=braw[:, hh, :, :], scalar1=b31[:, H0 + hh:H0 + hh + 1], scalar2=1.0 / scale, op0=ALU.subtract, op1=ALU.mult),
                      reads=[b_braw, b_b31], writes=[b_bt])
            sm = sb("sm", [128, 32], F32); b_sm = Buf()
            if isA:
                vbl = sb("vbl", [128, 256], F32); b_vbl = Buf()
                ph.load(vbl[:], self.E("vecB")[l, :, VB_LAM:VB_LAM + 256], b_vbl)
                pr = sb("pr", [128, 128], F32); b_pr = Buf()
                ph.op(ph.dve, lambda h: h.tensor_tensor(out=pr[:, 0:64], in0=vbl[:, 0:64], in1=vbl[:, 64:128], op=ALU.mult), reads=[b_vbl], writes=[b_pr])
                ph.op(ph.dve, lambda h: h.tensor_tensor(out=pr[:, 64:128], in0=vbl[:, 128:192], in1=vbl[:, 192:256], op=ALU.mult), reads=[b_vbl], writes=[b_pr])
                ph.op(ph.dve, lambda h: h.reduce_sum(out=sm[:, 0:1], in_=pr[:, 0:64], axis=AX.X), reads=[b_pr], writes=[b_sm])
                ph.op(ph.dve, lambda h: h.reduce_sum(out=sm[:, 1:2], in_=pr[:, 64:128], axis=AX.X), reads=[b_pr], writes=[b_sm])
                ph.op(ph.act, lambda h: h.activation(out=sm[:, 2:4], in_=sm[:, 0:2], func=AF.Exp), reads=[b_sm], writes=[b_sm])
                ph.op(ph.dve, lambda h: h.tensor_tensor(out=sm[:, 4:5], in0=sm[:, 2:3], in1=sm[:, 3:4], op=ALU.subtract), reads=[b_sm], writes=[b_sm])
                ph.op(ph.dve, lambda h: h.tensor_scalar(out=sm[:, 5:6], in0=sm[:, 4:5], scalar1=lam_init, scalar2=-1.0, op0=ALU.add, op1=ALU.mult), reads=[b_sm], writes=[b_sm])
            qg = [sb(f"qg{i}", [128, 6, 512], BF16) for i in range(2)]; b_qg = [Buf(), Buf()]
            if not isA:
                mt = [sb(f"mt{i}", [128, nb, 512], BF16) for i in range(2)]; b_mt = [Buf(), Buf()]
            pts = [sb(f"pt{i}", [128, 512], BF16) for i in range(3)]; b_pt = [Buf() for _ in range(3)]
            o0 = sb("o0", [128, 4, 128], F32); b_o0 = Buf()
            on = sb("on", [128, 4, 128], BF16); b_on = Buf()
            junk = sb("junk", [128, 128], BF16); b_junk = Buf()
            outT = [sb(f"outT{i}", [128, 512], BF16) for i in range(2)]; b_outT = [Buf(), Buf()]
            psS = pst("psS", [128, 3, 512], F32); b_psS = [Buf() for _ in range(3)]
            psA = pst("psA", [128, 4, 512], F32); b_psA = [Buf() for _ in range(4)]
            psT = pst("psT", [128, 1024], BF16); b_psT = Buf()
            rows = lambda m: slice(m * 64, (m + 1) * 64) if isA else slice(0, 128)

            def ldg(G):
                ph.load(qg[G % 2][:], qT_d[:, :, G * 512:(G + 1) * 512].rearrange("h p t -> p h t"), b_qg[G % 2])
                if not isA:
                    nj = 4 * G + 4
                    ph.load(mt[G % 2][:, 0:nj, :], self.MTd[G, :, 0:nj, :], b_mt[G % 2])

            cst = [0]
            cacc = [0]
            cout = [0]
            ldg(0)
            for G in range(ng):
                if G + 1 < ng:
                    ldg(G + 1)
                q_, bq = qg[G % 2], b_qg[G % 2]
                nj = 4 * G + 4
                for hh in range(6):
                    for m in range(nmap):
                        aset = cacc[0] % 2; cacc[0] += 1
                        accb = (2 * aset, 2 * aset + 1)
                        for j in range(nj):
                            c0 = max(0, j - 4 * G)
                            cols = slice(c0 * 128, 512)
                            sbk = cst[0] % 3; cst[0] += 1
                            near = [(k, 4 * G + k - j) for k in range(4) if 0 <= 4 * G + k - j <= 1]
                            ph.op(ph.pe, lambda h, sbk=sbk, cols=cols, hh=hh, m=m, j=j, q_=q_, near=near: h.matmul(psS[:, sbk, cols], lhsT=kT[rows(m), hh, j * 128:(j + 1) * 128], rhs=q_[rows(m), hh, cols], start=True, stop=(len(near) == 0)),
                                  reads=[b_kT, bq], writes=[b_psS[sbk]], pe_acc=True)
                            for ii, (k, kd) in enumerate(near):
                                ph.op(ph.pe, lambda h, sbk=sbk, k=k, kd=kd, hh=hh, ii=ii, near=near: h.matmul(psS[:, sbk, k * 128:(k + 1) * 128], lhsT=ident[:], rhs=bt[:, hh, kd, :], start=False, stop=(ii == len(near) - 1)),
                                      reads=[b_ident, b_bt], writes=[b_psS[sbk]], pe_acc=True)
                            pt, bp = pts[sbk], b_pt[sbk]
                            ph.op(ph.act, lambda h, pt=pt, sbk=sbk, cols=cols, hh=hh: h.activation(out=pt[:, cols], in_=psS[:, sbk, cols], func=AF.Exp, bias=b31[:, H0 + hh:H0 + hh + 1], scale=scale),
                                  reads=[b_psS[sbk], b_b31], writes=[bp])
                            if not isA:
                                mt_ = mt[G % 2]
                                ph.op(ph.dve, lambda h, pt=pt, cols=cols, j=j, mt_=mt_: h.tensor_tensor(out=pt[:, cols], in0=pt[:, cols], in1=mt_[:, j, cols], op=ALU.mult),
                                      reads=[b_mt[G % 2]], writes=[bp])
                            for k in range(c0, 4):
                                ab = accb[k // 2]
                                ph.op(ph.pe, lambda h, ab=ab, k=k, pt=pt, j=j, hh=hh, G=G: h.matmul(psA[:, ab, (k % 2) * 129:(k % 2) * 129 + 129], lhsT=pt[:, k * 128:(k + 1) * 128], rhs=v1[:, j, hh, :], start=(j == 0 and k % 2 == 0), stop=(j == 4 * G + k), skip_group_check=True),
                                      reads=[bp, b_v1], writes=[b_psA[ab]], pe_acc=True)
                        for k in range(4):
                            ab = accb[k // 2]; o = (k % 2) * 129
                            ph.op(ph.dve, lambda h, ab=ab, o=o, k=k: h.reciprocal(out=sm[:, 8 + k:9 + k], in_=psA[:, ab, o + 128:o + 129]), reads=[b_psA[ab]], writes=[b_sm])
                            if isA and m == 0:
                                ph.op(ph.dve, lambda h, ab=ab, o=o, k=k: h.tensor_scalar(out=o0[:, k, :], in0=psA[:, ab, o:o + 128], scalar1=sm[:, 8 + k:9 + k], scalar2=None, op0=ALU.mult), reads=[b_psA[ab], b_sm], writes=[b_o0])
                            elif isA:
                                ph.op(ph.dve, lambda h, k=k: h.tensor_tensor(out=sm[:, 12 + k:13 + k], in0=sm[:, 8 + k:9 + k], in1=sm[:, 5:6], op=ALU.mult), reads=[b_sm], writes=[b_sm])
                                ph.op(ph.dve, lambda h, ab=ab, o=o, k=k: h.scalar_tensor_tensor(out=o0[:, k, :], in0=psA[:, ab, o:o + 128], scalar=sm[:, 12 + k:13 + k], in1=o0[:, k, :], op0=ALU.mult, op1=ALU.add), reads=[b_psA[ab], b_sm], writes=[b_o0])
                                ph.op(ph.act, lambda h, k=k: h.activation(out=junk[:], in_=o0[:, k, :], func=AF.Square, accum_out=sm[:, 16 + k:17 + k]), reads=[b_o0], writes=[b_junk, b_sm])
                            else:
                                ph.op(ph.dve, lambda h, ab=ab, o=o, k=k: h.tensor_scalar(out=on[:, k, :], in0=psA[:, ab, o:o + 128], scalar1=sm[:, 8 + k:9 + k], scalar2=None, op0=ALU.mult), reads=[b_psA[ab], b_sm], writes=[b_on])
                        if isA and m == 0:
                            continue
                        if isA:
                            ph.op(ph.act, lambda h: h.activation(out=sm[:, 20:24], in_=sm[:, 16:20], func=AF.Sqrt, scale=1.0 / 128, bias=self.eps_ap), reads=[b_sm, self.b_eps], writes=[b_sm])
                            ph.op(ph.dve, lambda h: h.reciprocal(out=sm[:, 24:28], in_=sm[:, 20:24]), reads=[b_sm], writes=[b_sm])
                            for k in range(4):
                                ph.op(ph.dve, lambda h, k=k: h.tensor_scalar(out=on[:, k, :], in0=o0[:, k, :], scalar1=sm[:, 24 + k:25 + k], scalar2=None, op0=ALU.mult), reads=[b_o0, b_sm], writes=[b_on])
                        for k in range(4):
                            ph.op(ph.pe, lambda h, k=k: h.transpose(out=psT[:, k * 128:(k + 1) * 128], in_=on[:, k, :], identity=ident[:]), reads=[b_on, b_ident], writes=[b_psT], pe_acc=True)
                        oi = cout[0] % 2; cout[0] += 1
                        ph.op(ph.act, lambda h, oi=oi: h.copy(out=outT[oi][:], in_=psT[:, 0:512]), reads=[b_psT], writes=[b_outT[oi]])
                        ph.store(self.mixT[H0 + hh, :, G * 512:(G + 1) * 512], outT[oi][:], b_outT[oi])
            ph.emit(st)


    def bindex(self, l):
        nc, S, nb, ng = self.nc, self.S, self.nb, self.ng
        with ExitStack() as st:
            ph, sb, pst, cs = self.begin(f"bidx{l}", st, want=("ident", "causneg", "pow2"))
            ident, b_ident = cs["ident"]
            cneg, b_cneg = cs["causneg"]
            pow2, b_pow2 = cs["pow2"]
            kx = sb("kx", [128, S], BF16); b_kx = Buf()
            ph.dma(kx[0:64, :], self.kidxT, writes=[b_kx], sbuf=b_kx)
            ph.dma(kx[64:128, :], self.kidxT, writes=[b_kx], sbuf=b_kx)
            qi = [sb(f"qi{i}", [128, 8, 128], BF16) for i in range(2)]; b_qi = [Buf(), Buf()]
            wi = [sb(f"wi{i}", [128, 16], F32) for i in range(2)]; b_wi = [Buf(), Buf()]
            sc = sb("sc", [128, S], F32); b_sc = Buf()
            rl = [sb(f"rl{i}", [128, 1024], F32) for i in range(2)]; b_rl = [Buf(), Buf()]
            junkb = sb("junkb", [128, S], BF16); b_junkb = Buf()
            msk = sb("msk", [128, S], BF16); b_msk = Buf()
            MTg = sb("MTg", [128, nb, 512], BF16); b_MTg = Buf()
            sm = sb("sm", [128, 64], F32); b_sm = Buf()
            psD = pst("psD", [128, 2, 1024], F32); b_psD = [Buf(), Buf()]
            psT = pst("psT", [128, 2, 1024], BF16); b_psT = [Buf(), Buf()]
            WH = 16

            def ldq(i):
                tk = slice(i * 128, (i + 1) * 128)
                ph.load(qi[i % 2][:], self.qidxT[:, :, tk].rearrange("h p t -> p h t"), b_qi[i % 2])
                ph.load(wi[i % 2][:], self.widx[tk, :], b_wi[i % 2])

            cd = [0]
            ct = [0]
            ldq(0)
            for i in range(nb):
                if i + 1 < nb:
                    ldq(i + 1)
                q_, bq, w_, bw = qi[i % 2], b_qi[i % 2], wi[i % 2], b_wi[i % 2]
                Si = (i + 1) * 128
                for c in range((Si + 1023) // 1024):
                    n = min(1024, Si - c * 1024)
                    for g in range(16):
                        db = cd[0] % 2; cd[0] += 1
                        r0 = (g % 2) * 64
                        for sub in range((n + 511) // 512):
                            ns = min(512, n - sub * 512)
                            k0 = c * 1024 + sub * 512
                            ph.op(ph.pe, lambda h, db=db, sub=sub, ns=ns, r0=r0, g=g, k0=k0, q_=q_: h.matmul(psD[:, db, sub * 512:sub * 512 + ns], lhsT=q_[r0:r0 + 64, g // 2, :], rhs=kx[r0:r0 + 64, k0:k0 + ns], start=True, stop=True),
                                  reads=[bq, b_kx], writes=[b_psD[db]], pe_acc=True)
                        ph.op(ph.act, lambda h, db=db, n=n: h.activation(out=rl[db][:, 0:n], in_=psD[:, db, 0:n], func=AF.Relu), reads=[b_psD[db]], writes=[b_rl[db]])
                        if g == 0:
                            ph.op(ph.dve, lambda h, db=db, n=n, c=c, w_=w_: h.tensor_scalar(out=sc[:, c * 1024:c * 1024 + n], in0=rl[db][:, 0:n], scalar1=w_[:, 0:1], scalar2=None, op0=ALU.mult),
                                  reads=[b_rl[db], bw], writes=[b_sc])
                        else:
                            ph.op(ph.dve, lambda h, db=db, n=n, c=c, g=g, w_=w_: h.scalar_tensor_tensor(out=sc[:, c * 1024:c * 1024 + n], in0=rl[db][:, 0:n], scalar=w_[:, g:g + 1], in1=sc[:, c * 1024:c * 1024 + n], op0=ALU.mult, op1=ALU.add),
                                  reads=[b_rl[db], bw], writes=[b_sc])
                ph.op(ph.dve, lambda h, i=i: h.tensor_tensor(out=sc[:, i * 128:(i + 1) * 128], in0=sc[:, i * 128:(i + 1) * 128], in1=cneg[:], op=ALU.add), reads=[b_cneg], writes=[b_sc])
                if i < 2:
                    ph.op(ph.dve, lambda h: h.memset(sm[:, 13:14], -1e29), writes=[b_sm])
                else:
                    ph.op(ph.dve, lambda h, Si=Si: h.max(out=sm[:, 0:8], in_=sc[:, 0:Si]), reads=[b_sc], writes=[b_sm])
                    ph.op(ph.dve, lambda h, i=i: h.tensor_reduce(out=sm[:, 8:9], in_=sc[:, 0:i * 128], op=ALU.min, axis=AX.X), reads=[b_sc], writes=[b_sm])
                    ph.op(ph.dve, lambda h: h.tensor_tensor(out=sm[:, 9:10], in0=sm[:, 0:1], in1=sm[:, 8:9], op=ALU.subtract), reads=[b_sm], writes=[b_sm])
                    ph.op(ph.dve, lambda h: h.tensor_scalar(out=sm[:, WH:WH + NBIS + 1], in0=pow2[:, 0:NBIS + 1], scalar1=sm[:, 9:10], scalar2=None, op0=ALU.mult), reads=[b_sm, b_pow2], writes=[b_sm])
                    ph.op(ph.dve, lambda h: h.tensor_tensor(out=sm[:, 10:11], in0=sm[:, 8:9], in1=sm[:, WH:WH + 1], op=ALU.add), reads=[b_sm], writes=[b_sm])
                    for k in range(NBIS):
                        ph.op(ph.dve, lambda h, Si=Si: h.tensor_scalar(out=junkb[:, 0:Si], in0=sc[:, 0:Si], scalar1=sm[:, 10:11], scalar2=0.0, op0=ALU.is_ge, op1=ALU.add, accum_out=sm[:, 11:12]),
                              reads=[b_sc, b_sm], writes=[b_junkb, b_sm])
                        ph.op(ph.dve, lambda h: h.tensor_scalar(out=sm[:, 12:13], in0=sm[:, 11:12], scalar1=float(TOPK), scalar2=0.5, op0=ALU.is_ge, op1=ALU.subtract), reads=[b_sm], writes=[b_sm])
                        ph.op(ph.dve, lambda h, k=k: h.scalar_tensor_tensor(out=sm[:, 10:11], in0=sm[:, WH + k:WH + k + 1], scalar=sm[:, 12:13], in1=sm[:, 10:11], op0=ALU.mult, op1=ALU.add), reads=[b_sm], writes=[b_sm])
                    ph.op(ph.dve, lambda h: h.tensor_tensor(out=sm[:, 13:14], in0=sm[:, 10:11], in1=sm[:, WH + NBIS:WH + NBIS + 1], op=ALU.subtract), reads=[b_sm], writes=[b_sm])
                ph.op(ph.dve, lambda h, Si=Si: h.tensor_scalar(out=msk[:, 0:Si], in0=sc[:, 0:Si], scalar1=sm[:, 13:14], scalar2=None, op0=ALU.is_ge), reads=[b_sc, b_sm], writes=[b_msk])
                kq = i % 4
                for j0 in range(0, i + 1, 8):
                    nj = min(8, i + 1 - j0)
                    tb = ct[0] % 2; ct[0] += 1
                    for jj in range(nj):
                        j = j0 + jj
                        ph.op(ph.pe, lambda h, tb=tb, jj=jj, j=j: h.transpose(out=psT[:, tb, jj * 128:(jj + 1) * 128], in_=msk[:, j * 128:(j + 1) * 128], identity=ident[:]),
                              reads=[b_msk, b_ident], writes=[b_psT[tb]], pe_acc=True)
                    ph.op(ph.act, lambda h, tb=tb, nj=nj, j0=j0, kq=kq: h.copy(out=MTg[:, j0:j0 + nj, kq * 128:(kq + 1) * 128], in_=psT[:, tb, 0:nj * 128].rearrange("p (j q) -> p j q", q=128)),
                          reads=[b_psT[tb]], writes=[b_MTg])
                if kq == 3:
                    G = i // 4
                    ph.store(self.MTd[G, :, 0:4 * G + 4, :], MTg[:, 0:4 * G + 4, :], b_MTg)
            ph.emit(st)


    def memkv(self, l):
        nc = self.nc
        with ExitStack() as st:
            ph, sb, pst, cs = self.begin(f"mkv{l}", st)
            ident, b_ident = cs["ident"]
            vp = sb("vp", [128, NVP], F32); b_vp = Buf()
            ph.load(vp[:], self.E("vecP")[l], b_vp)
            wkv = sb("wkv", [128, NDC, 1024], BF16); b_wkv = Buf()
            stg = [sb(f"stg{i}", [128, 1024], F32) for i in range(2)]; b_stg = [Buf(), Buf()]
            for dc in range(NDC):
                s_ = dc % 2
                ph.load(stg[s_][:], self.E("x_wkv")[l, dc * 128:(dc + 1) * 128, :], b_stg[s_])
                ph.op(ph.dve, lambda h, s_=s_, dc=dc: h.tensor_scalar(out=wkv[:, dc, :], in0=stg[s_][:], scalar1=vp[:, VP_MEMG + dc:VP_MEMG + dc + 1], scalar2=None, op0=ALU.mult),
                      reads=[b_stg[s_], b_vp], writes=[b_wkv])
            hts = [sb(f"ht{i}", [128, D], F32) for i in range(2)]; b_ht = [Buf(), Buf()]
            junk = sb("junk", [128, D], BF16); b_junk = Buf()
            sm = sb("sm", [128, 4], F32); b_sm = Buf()
            xn = sb("xn", [128, D], BF16); b_xn = Buf()
            xnT = sb("xnT", [128, NDC, 128], BF16); b_xnT = Buf()
            kst = sb("kst", [128, 4, 128], BF16); b_kst = Buf()
            vst = sb("vst", [128, 512], BF16); b_vst = Buf()
            psB = pst("psB", [128, 2, 1024], BF16); b_psB = [Buf(), Buf()]
            psF = pst("psF", [128, 2, 512], F32); bF = [Buf(), Buf()]
            for i in range(2):
                ph.load(hts[i][:], self.E("mem")[i * 128:(i + 1) * 128, :], b_ht[i])
            for i in range(2):
                ht, bh = hts[i], b_ht[i]
                self.rms_tile(ph, ht[:], bh, junk[:], b_junk, sm[:, 0:1], sm[:, 1:2], sm[:, 2:3], b_sm, D)
                ph.op(ph.dve, lambda h, ht=ht: h.tensor_scalar(out=xn[:], in0=ht[:], scalar1=sm[:, 2:3], scalar2=None, op0=ALU.mult), reads=[bh, b_sm], writes=[b_xn])
                for dc in range(NDC):
                    ph.op(ph.pe, lambda h, dc=dc: h.transpose(out=psB[:, dc // 8, (dc % 8) * 128:(dc % 8 + 1) * 128], in_=xn[:, dc * 128:(dc + 1) * 128], identity=ident[:]),
                          reads=[b_xn, b_ident], writes=[b_psB[dc // 8]], pe_acc=True)
                for k in range(2):
                    ph.op(ph.act, lambda h, k=k: h.copy(out=xnT[:, k * 8:(k + 1) * 8, :], in_=psB[:, k, :]), reads=[b_psB[k]], writes=[b_xnT])
                for hh in range(4):
                    for dc in range(NDC):
                        ph.op(ph.pe, lambda h, hh=hh, dc=dc: h.matmul(psF[:, 0, hh * 128:(hh + 1) * 128], lhsT=wkv[:, dc, hh * 128:(hh + 1) * 128], rhs=xnT[:, dc, :], start=(dc == 0), stop=(dc == NDC - 1)),
                              reads=[b_wkv, b_xnT], writes=[bF[0]], pe_acc=True)
                for dc in range(NDC):
                    ph.op(ph.pe, lambda h, dc=dc: h.matmul(psF[:, 1, :], lhsT=xnT[:, dc, :], rhs=wkv[:, dc, 512:1024], start=(dc == 0), stop=(dc == NDC - 1)),
                          reads=[b_wkv, b_xnT], writes=[bF[1]], pe_acc=True)
                ph.op(ph.act, lambda h: h.copy(out=kst[:], in_=psF[:, 0, :]), reads=[bF[0]], writes=[b_kst])
                ph.op(ph.dve, lambda h: h.tensor_copy(out=vst[:], in_=psF[:, 1, :]), reads=[bF[1]], writes=[b_vst])
                ph.store(self.kmT[:, :, i * 128:(i + 1) * 128], kst[:], b_kst)
                ph.store(self.vm[i * 128:(i + 1) * 128, :], vst[:], b_vst)
            ph.emit(st)

    def p3(self, l):
        nc, S, nb = self.nc, self.S, self.nb
        lam_init = 0.8 - 0.6 * math.exp(-0.3 * l)
        xscale = 128 ** -0.5
        hsrc = self.E("x") if l == 0 else self.h
        with ExitStack() as st:
            ph, sb, pst, cs = self.begin(f"p3{l}", st)
            ident, b_ident = cs["ident"]
            vp = sb("vp", [128, NVP], F32); b_vp = Buf()
            ph.load(vp[:], self.E("vecP")[l], b_vp)
            sg = sb("sg", [128, 1], F32); b_sg = Buf()
            ph.op(ph.dve, lambda h: h.tensor_scalar(out=sg[:], in0=vp[:, VP_SUBG:VP_SUBG + 1], scalar1=1.0 - lam_init, scalar2=None, op0=ALU.mult), reads=[b_vp], writes=[b_sg])
            pg = sb("pg", [128, 2, D], F32); b_pg = Buf()
            ph.load(pg[:, 0, :], self.E("vecB")[l, :, VB_MIXPOST:VB_MIXPOST + D], b_pg)
            ph.load(pg[:, 1, :], self.E("vecB")[l, :, VB_XPOST:VB_XPOST + D], b_pg)
            wout = sb("wout", [128, NDC, D], BF16); b_wout = Buf()
            xwq = sb("xwq", [128, NDC, 512], BF16); b_xwq = Buf()
            xwo = sb("xwo", [128, 4, D], BF16); b_xwo = Buf()
            kmT = sb("kmT", [128, 4, 256], BF16); b_kmT = Buf()
            ph.load(kmT[:], self.kmT, b_kmT)
            vm1 = sb("vm1", [128, 2, 4, 129], BF16); b_vm1 = Buf()
            ph.op(ph.pool, lambda h: h.memset(vm1[:, :, :, 128:129], 1.0), writes=[b_vm1])
            for mc in range(2):
                ph.dma(vm1[:, mc, :, 0:128], self.vm[mc * 128:(mc + 1) * 128, :].rearrange("p (h d) -> p h d", d=128), writes=[b_vm1], sbuf=b_vm1)
            stg = [sb(f"stg{i}", [128, D], F32) for i in range(2)]; b_stg = [Buf(), Buf()]
            cnt = [0]

            def wload(dst, b_dst, srcap, n, scal):
                s_ = cnt[0] % 2; cnt[0] += 1
                ph.load(stg[s_][:, 0:n], srcap, b_stg[s_])
                if scal is None:
                    ph.op(ph.dve, lambda h: h.tensor_copy(out=dst, in_=stg[s_][:, 0:n]), reads=[b_stg[s_]], writes=[b_dst])
                else:
                    ph.op(ph.dve, lambda h: h.tensor_scalar(out=dst, in0=stg[s_][:, 0:n], scalar1=scal, scalar2=None, op0=ALU.mult), reads=[b_stg[s_], b_vp, b_sg], writes=[b_dst])

            for c in range(NDC):
                wload(wout[:, c, :], b_wout, self.E("w_out")[l, c * 128:(c + 1) * 128, :], D, sg[:, 0:1] if c < 6 else None)
            for dc in range(NDC):
                wload(xwq[:, dc, :], b_xwq, self.E("x_wq")[l, dc * 128:(dc + 1) * 128, :], 512, vp[:, VP_XPRE + dc:VP_XPRE + dc + 1])
            for hh in range(4):
                wload(xwo[:, hh, :], b_xwo, self.E("x_wo")[l, hh * 128:(hh + 1) * 128, :], D, None)
            hts = [sb(f"ht{i}", [128, D], F32) for i in range(2)]; b_ht = [Buf(), Buf()]
            mxs = [sb(f"mx{i}", [128, NDC, 128], BF16) for i in range(2)]; b_mx = [Buf(), Buf()]
            junk = sb("junk", [128, D], BF16); b_junk = Buf()
            tmp = sb("tmp", [128, D], F32); b_tmp = Buf()
            sm = sb("sm", [128, 24], F32); b_sm = Buf()
            xn = sb("xn", [128, D], BF16); b_xn = Buf()
            xnT = sb("xnT", [128, NDC, 128], BF16); b_xnT = Buf()
            qx = sb("qx", [128, 4, 128], BF16); b_qx = Buf()
            pl = sb("pl", [128, 8, 128], BF16); b_pl = Buf()
            on = sb("on", [128, 4, 128], BF16); b_on = Buf()
            oT = sb("oT", [128, 4, 128], BF16); b_oT = Buf()
            psY = pst("psY", [128, 4, 512], F32); b_psY = Buf()
            psB = pst("psB", [128, 2, 1024], BF16); b_psB = [Buf(), Buf()]
            psX = pst("psX", [128, 2, 512], F32); b_psX = Buf()

            def ld(i):
                tk = slice(i * 128, (i + 1) * 128)
                ph.load(hts[i % 2][:], hsrc[tk, :], b_ht[i % 2])
                ph.load(mxs[i % 2][:], self.mixT[:, :, tk].rearrange("c p t -> p c t"), b_mx[i % 2])

            def post(which, ht, bh):
                ph.op(ph.act, lambda h: h.activation(out=junk[:], in_=psY[:].rearrange("p a b -> p (a b)"), func=AF.Square, accum_out=sm[:, 0:1]), reads=[b_psY], writes=[b_junk, b_sm])
                ph.op(ph.act, lambda h: h.activation(out=sm[:, 1:2], in_=sm[:, 0:1], func=AF.Sqrt, scale=1.0 / D, bias=self.eps_ap), reads=[b_sm, self.b_eps], writes=[b_sm])
                ph.op(ph.dve, lambda h: h.reciprocal(out=sm[:, 2:3], in_=sm[:, 1:2]), reads=[b_sm], writes=[b_sm])
                ph.op(ph.dve, lambda h: h.scalar_tensor_tensor(out=tmp[:], in0=psY[:].rearrange("p a b -> p (a b)"), scalar=sm[:, 2:3], in1=pg[:, which, :], op0=ALU.mult, op1=ALU.mult), reads=[b_psY, b_sm, b_pg], writes=[b_tmp])
                ph.op(ph.dve, lambda h: h.tensor_tensor(out=ht[:], in0=tmp[:], in1=ht[:], op=ALU.add), reads=[b_tmp], writes=[bh])

            ld(0)
            for i in range(nb):
                if i + 1 < nb:
                    ld(i + 1)
                ht, bh, mx, bm = hts[i % 2], b_ht[i % 2], mxs[i % 2], b_mx[i % 2]
                for n in range(4):
                    for c in range(NDC):
                        ph.op(ph.pe, lambda h, n=n, c=c, mx=mx: h.matmul(psY[:, n, :], lhsT=mx[:, c, :], rhs=wout[:, c, n * 512:(n + 1) * 512], start=(c == 0), stop=(c == NDC - 1)),
                              reads=[bm, b_wout], writes=[b_psY], pe_acc=True)
                post(0, ht, bh)
                self.rms_tile(ph, ht[:], bh, junk[:], b_junk, sm[:, 3:4], sm[:, 4:5], sm[:, 5:6], b_sm, D)
                ph.op(ph.dve, lambda h, ht=ht: h.tensor_scalar(out=xn[:], in0=ht[:], scalar1=sm[:, 5:6], scalar2=None, op0=ALU.mult), reads=[bh, b_sm], writes=[b_xn])
                for dc in range(NDC):
                    ph.op(ph.pe, lambda h, dc=dc: h.transpose(out=psB[:, dc // 8, (dc % 8) * 128:(dc % 8 + 1) * 128], in_=xn[:, dc * 128:(dc + 1) * 128], identity=ident[:]),
                          reads=[b_xn, b_ident], writes=[b_psB[dc // 8]], pe_acc=True)
                for k in range(2):
                    ph.op(ph.act, lambda h, k=k: h.copy(out=xnT[:, k * 8:(k + 1) * 8, :], in_=psB[:, k, :]), reads=[b_psB[k]], writes=[b_xnT])
                for hh in range(4):
                    for dc in range(NDC):
                        ph.op(ph.pe, lambda h, hh=hh, dc=dc: h.matmul(psX[:, 0, hh * 128:(hh + 1) * 128], lhsT=xwq[:, dc, hh * 128:(hh + 1) * 128], rhs=xnT[:, dc, :], start=(dc == 0), stop=(dc == NDC - 1)),
                              reads=[b_xwq, b_xnT], writes=[b_psX], pe_acc=True)
                ph.op(ph.act, lambda h: h.copy(out=qx[:], in_=psX[:, 0, :]), reads=[b_psX], writes=[b_qx])
                for hh in range(4):
                    for mc in range(2):
                        u = hh * 2 + mc
                        ph.op(ph.pe, lambda h, hh=hh, mc=mc, u=u: h.matmul(psX[:, u // 4, (u % 4) * 128:(u % 4 + 1) * 128], lhsT=kmT[:, hh, mc * 128:(mc + 1) * 128], rhs=qx[:, hh, :], start=True, stop=True),
                              reads=[b_kmT, b_qx], writes=[b_psX], pe_acc=True)
                ph.op(ph.act, lambda h: h.activation(out=pl[:], in_=psX[:].rearrange("p a b -> p (a b)"), func=AF.Exp, scale=xscale), reads=[b_psX], writes=[b_pl])
                for hh in range(4):
                    for mc in range(2):
                        ph.op(ph.pe, lambda h, hh=hh, mc=mc: h.matmul(psX[:, hh // 2, (hh % 2) * 129:(hh % 2) * 129 + 129], lhsT=pl[:, hh * 2 + mc, :], rhs=vm1[:, mc, hh, :], start=(hh % 2 == 0 and mc == 0), stop=(mc == 1), skip_group_check=True),
                              reads=[b_pl, b_vm1], writes=[b_psX], pe_acc=True)
                for hh in range(4):
                    o = (hh % 2) * 129
                    ph.op(ph.dve, lambda h, hh=hh, o=o: h.reciprocal(out=sm[:, 8 + hh:9 + hh], in_=psX[:, hh // 2, o + 128:o + 129]), reads=[b_psX], writes=[b_sm])
                    ph.op(ph.dve, lambda h, hh=hh, o=o: h.tensor_scalar(out=on[:, hh, :], in0=psX[:, hh // 2, o:o + 128], scalar1=sm[:, 8 + hh:9 + hh], scalar2=None, op0=ALU.mult), reads=[b_psX, b_sm], writes=[b_on])
                for hh in range(4):
                    ph.op(ph.pe, lambda h, hh=hh: h.transpose(out=psB[:, 0, hh * 128:(hh + 1) * 128], in_=on[:, hh, :], identity=ident[:]), reads=[b_on, b_ident], writes=[b_psB[0]], pe_acc=True)
                ph.op(ph.act, lambda h: h.copy(out=oT[:], in_=psB[:, 0, 0:512]), reads=[b_psB[0]], writes=[b_oT])
                for n in range(4):
                    for hh in range(4):
                        ph.op(ph.pe, lambda h, n=n, hh=hh: h.matmul(psY[:, n, :], lhsT=oT[:, hh, :], rhs=xwo[:, hh, n * 512:(n + 1) * 512], start=(hh == 0), stop=(hh == 3)),
                              reads=[b_oT, b_xwo], writes=[b_psY], pe_acc=True)
                post(1, ht, bh)
                ph.store(self.h[i * 128:(i + 1) * 128, :], ht[:], bh)
            ph.emit(st)


    def ffn(self, l):
        nc, S = self.nc, self.S
        TG = min(1024, S)
        ngr = S // TG
        nt = TG // 128
        nh = TG // 512
        with ExitStack() as st:
            ph, sb, pst, cs = self.begin(f"ffn{l}", st)
            ident, b_ident = cs["ident"]
            gB = sb("gB", [128, D], F32); b_gB = Buf()
            ph.load(gB[:], self.E("vecB")[l, :, VB_FPRE:VB_FPRE + D], b_gB)
            xnT = sb("xnT", [128, NDC, TG], BF16); b_xnT = Buf()
            actT = sb("actT", [128, NFC, TG], BF16); b_actT = Buf()
            gus = [sb(f"gus{i}", [128, 2, NDC, 128], F32) for i in range(2)]; b_gus = [Buf(), Buf()]
            gub = [sb(f"gub{i}", [128, 2, NDC, 128], BF16) for i in range(2)]; b_gub = [Buf(), Buf()]
            NWD = 3
            wds = [sb(f"wds{i}", [128, 256], F32) for i in range(NWD)]; b_wds = [Buf() for _ in range(NWD)]
            wdb = [sb(f"wdb{i}", [128, 256], BF16) for i in range(NWD)]; b_wdb = [Buf() for _ in range(NWD)]
            ht = sb("ht", [128, D], F32); b_ht = Buf()
            xn = sb("xn", [128, D], BF16); b_xn = Buf()
            sgt = sb("sgt", [128, 512], F32); b_sgt = Buf()
            yst = sb("yst", [128, nt, 256], F32); b_yst = Buf()
            sm = sb("sm", [128, 4], F32); b_sm = Buf()
            psB = pst("psB", [128, 1024], BF16); b_psB = Buf()
            psGU = pst("psGU", [128, 3, 512], F32); b_psGU = [Buf() for _ in range(3)]
            psD = pst("psD", [128, 4, 512], F32); b_psD = [Buf() for _ in range(4)]
            hsrc = self.h
            cgu = [0]
            cwd = [0]
            cbank = [0]
            for gr in range(ngr):
                for t in range(nt):
                    r0 = gr * TG + t * 128
                    ph.load(ht[:], hsrc[r0:r0 + 128, :], b_ht)
                    self.rms_tile(ph, ht[:], b_ht, xn[:], b_xn, sm[:, 0:1], sm[:, 1:2], sm[:, 2:3], b_sm, D)
                    ph.op(ph.dve, lambda h: h.scalar_tensor_tensor(out=xn[:], in0=ht[:], scalar=sm[:, 2:3], in1=gB[:], op0=ALU.mult, op1=ALU.mult), reads=[b_ht, b_sm, b_gB], writes=[b_xn])
                    for k in range(2):
                        for dd in range(8):
                            dc = k * 8 + dd
                            ph.op(ph.pe, lambda h, dd=dd, dc=dc: h.transpose(out=psB[:, dd * 128:(dd + 1) * 128], in_=xn[:, dc * 128:(dc + 1) * 128], identity=ident[:]),
                                  reads=[b_xn, b_ident], writes=[b_psB], pe_acc=True)
                        ph.op(ph.act, lambda h, k=k, t=t: h.copy(out=xnT[:, k * 8:(k + 1) * 8, t * 128:(t + 1) * 128], in_=psB[:].rearrange("p (a b) -> p a b", b=128)), reads=[b_psB], writes=[b_xnT])
                for fc in range(NFC):
                    s_ = cgu[0] % 2; cgu[0] += 1
                    for c in range(2):
                        ph.dma(gus[s_][:, c, :, :], self.E("f_w_gu")[l, :, c, fc * 128:(fc + 1) * 128].rearrange("(dc p) f -> p dc f", p=128), writes=[b_gus[s_]], sbuf=b_gus[s_])
                    ph.op(ph.pool, lambda h, s_=s_: h.tensor_copy(out=gub[s_][:], in_=gus[s_][:]), reads=[b_gus[s_]], writes=[b_gub[s_]])
                    for hf in range(nh):
                        bks = []
                        for c in range(2):
                            bk = cbank[0] % 3; cbank[0] += 1
                            bks.append(bk)
                            for dc in range(NDC):
                                ph.op(ph.pe, lambda h, bk=bk, c=c, dc=dc, s_=s_, hf=hf: h.matmul(psGU[:, bk, :], lhsT=gub[s_][:, c, dc, :], rhs=xnT[:, dc, hf * 512:(hf + 1) * 512], start=(dc == 0), stop=(dc == NDC - 1)),
                                      reads=[b_gub[s_], b_xnT], writes=[b_psGU[bk]], pe_acc=True)
                        ph.op(ph.act, lambda h, bk=bks[0]: h.activation(out=sgt[:], in_=psGU[:, bk, :], func=AF.Silu), reads=[b_psGU[bks[0]]], writes=[b_sgt])
                        ph.op(ph.dve, lambda h, bk=bks[1], fc=fc, hf=hf: h.tensor_tensor(out=actT[:, fc, hf * 512:(hf + 1) * 512], in0=sgt[:], in1=psGU[:, bk, :], op=ALU.mult), reads=[b_sgt, b_psGU[bks[1]]], writes=[b_actT])
                for dcol in range(8):
                    for fc in range(NFC):
                        w_ = cwd[0] % NWD; cwd[0] += 1
                        ph.load(wds[w_][:], self.E("f_w_down")[l, fc * 128:(fc + 1) * 128, dcol * 256:(dcol + 1) * 256], b_wds[w_])
                        ph.op(ph.pool, lambda h, w_=w_: h.tensor_copy(out=wdb[w_][:], in_=wds[w_][:]), reads=[b_wds[w_]], writes=[b_wdb[w_]])
                        for t in range(nt):
                            ph.op(ph.pe, lambda h, t=t, fc=fc, w_=w_: h.matmul(psD[:, t // 2, (t % 2) * 256:(t % 2) * 256 + 256], lhsT=actT[:, fc, t * 128:(t + 1) * 128], rhs=wdb[w_][:], start=(fc == 0 and t % 2 == 0), stop=(fc == NFC - 1), skip_group_check=True),
                                  reads=[b_actT, b_wdb[w_]], writes=[b_psD[t // 2]], pe_acc=True)
                    for b in range(nt // 2):
                        if b % 2 == 0:
                            ph.op(ph.dve, lambda h, b=b: h.tensor_copy(out=yst[:, 2 * b:2 * b + 2, :], in_=psD[:, b, :].rearrange("p (a c) -> p a c", c=256)), reads=[b_psD[b]], writes=[b_yst])
                        else:
                            ph.op(ph.act, lambda h, b=b: h.copy(out=yst[:, 2 * b:2 * b + 2, :], in_=psD[:, b, :].rearrange("p (a c) -> p a c", c=256)), reads=[b_psD[b]], writes=[b_yst])
                    ph.store(self.yscr[gr * TG:(gr + 1) * TG, dcol * 256:(dcol + 1) * 256].rearrange("(t p) c -> p t c", p=128), yst[:], b_yst)
            ph.emit(st)

    def ffn_post(self, l, dst):
        nc, S, nb = self.nc, self.S, self.nb
        with ExitStack() as st:
            ph, sb, pst, cs = self.begin(f"fpo{l}", st, want=())
            pg = sb("pg", [128, D], F32); b_pg = Buf()
            ph.load(pg[:], self.E("vecB")[l, :, VB_FPOST:VB_FPOST + D], b_pg)
            hts = [sb(f"ht{i}", [128, D], F32) for i in range(2)]; b_ht = [Buf(), Buf()]
            yts = [sb(f"yt{i}", [128, D], F32) for i in range(2)]; b_yt = [Buf(), Buf()]
            junk = sb("junk", [128, D], BF16); b_junk = Buf()
            sm = sb("sm", [128, 4], F32); b_sm = Buf()

            def ld(i):
                tk = slice(i * 128, (i + 1) * 128)
                ph.load(hts[i % 2][:], self.h[tk, :], b_ht[i % 2])
                ph.load(yts[i % 2][:], self.yscr[tk, :], b_yt[i % 2])

            ld(0)
            for i in range(nb):
                if i + 1 < nb:
                    ld(i + 1)
                ht, bh, yt, by = hts[i % 2], b_ht[i % 2], yts[i % 2], b_yt[i % 2]
                self.rms_tile(ph, yt[:], by, junk[:], b_junk, sm[:, 0:1], sm[:, 1:2], sm[:, 2:3], b_sm, D)
                ph.op(ph.dve, lambda h, yt=yt: h.scalar_tensor_tensor(out=yt[:], in0=yt[:], scalar=sm[:, 2:3], in1=pg[:], op0=ALU.mult, op1=ALU.mult), reads=[b_sm, b_pg], writes=[by])
                ph.op(ph.pool, lambda h, yt=yt, ht=ht: h.tensor_tensor(out=ht[:], in0=yt[:], in1=ht[:], op=ALU.add), reads=[by], writes=[bh])
                ph.store(dst[i * 128:(i + 1) * 128, :], ht[:], bh)
            ph.emit(st)


def rel_bucket_np(dist):
    n = np.maximum(dist, 0)
    nf = np.maximum(n, 1).astype(np.float32)
    large = 16 + (np.log(nf / np.float32(16)) / np.float32(math.log(128 / 16)) * np.float32(16)).astype(np.int32)
    large = np.minimum(large, 31)
    return np.where(n < 16, n, large)


def host_consts():
    c = np.zeros((128, 128 * 4 + 32), np.float32)
    c[:, 0:128] = np.eye(128, dtype=np.float32)
    s = np.arange(128)[:, None]
    t = np.arange(128)[None, :]
    c[:, 128:256] = (s <= t).astype(np.float32)
    c[:, 256:384] = np.where(t <= s, 0.0, -1e30)
    c[:, 384:512] = np.where(s <= t, 0.0, NEG)
    c[:, 512:544] = (0.5 ** (np.arange(32) + 1))[None, :]
    return c


def prep_inputs(inp, S, L):
    f = lambda a: np.ascontiguousarray(np.asarray(a, dtype=np.float32)[:L])
    shared = {}
    shared["w_in"] = f(inp["w_in"])
    shared["w_out"] = f(inp["w_out"])
    shared["b_w_uq"] = f(inp["b_w_uq"]).reshape(L, 512, 768)
    shared["b_w_qidx"] = f(inp["b_w_qidx"]).reshape(L, 512, 1024)
    shared["b_w_uk"] = f(inp["b_w_uk"]).reshape(L, 256, 768)
    shared["b_w_uv"] = f(inp["b_w_uv"]).reshape(L, 256, 768)
    shared["cwsT"] = f(np.transpose(np.asarray(inp["c_w_s"], dtype=np.float32)[:L], (0, 3, 1, 2))).reshape(L, 128, 512)
    shared["x_wq"] = f(inp["x_wq"]).reshape(L, D, 512)
    shared["x_wkv"] = f(inp["x_wkv"]).reshape(L, D, 1024)
    shared["x_wo"] = f(inp["x_wo"]).reshape(L, 512, D)
    shared["f_w_gu"] = f(inp["f_w_gu"])
    shared["f_w_down"] = f(inp["f_w_down"])
    vp = np.zeros((L, 128, NVP), np.float32)
    vb = np.zeros((L, 128, NVB), np.float32)
    pp = lambda v, n: np.asarray(v, np.float32).reshape(n, 128).T
    for l in range(L):
        vp[l, :, VP_GIN:VP_GIN + 16] = pp(inp["mix_pre_g"][l], 16)
        vp[l, :, VP_CQ:VP_CQ + 4] = pp(inp["b_cq_g"][l], 4)
        vp[l, :, VP_CKV:VP_CKV + 2] = pp(inp["b_ckv_g"][l], 2)
        vp[l, :, VP_XPRE:VP_XPRE + 16] = pp(inp["x_pre_g"][l], 16)
        vp[l, :, VP_MEMG:VP_MEMG + 16] = pp(inp["mem_g"][l], 16)
        vp[l, :, VP_FPRE:VP_FPRE + 16] = pp(inp["f_pre_g"][l], 16)
        vp[l, :, VP_SUBG] = np.asarray(inp["a_sub_g"][l], np.float32)
        vb[l, :, VB_MIXPOST:VB_MIXPOST + D] = np.asarray(inp["mix_post_g"][l])[None, :]
        vb[l, :, VB_XPOST:VB_XPOST + D] = np.asarray(inp["x_post_g"][l])[None, :]
        vb[l, :, VB_FPOST:VB_FPOST + D] = np.asarray(inp["f_post_g"][l])[None, :]
        vb[l, :, VB_LNG:VB_LNG + 512] = np.asarray(inp["c_ln_g"][l])[None, :]
        vb[l, :, VB_LNB:VB_LNB + 512] = np.asarray(inp["c_ln_b"][l])[None, :]
        vb[l, :, VB_BS:VB_BS + 512] = np.asarray(inp["c_b_s"][l]).reshape(1, 512)
        vb[l, :, VB_LAM:VB_LAM + 256] = np.asarray(inp["a_lambda"][l]).reshape(1, 256)
        vb[l, :, VB_FPRE:VB_FPRE + D] = np.asarray(inp["f_pre_g"][l])[None, :]
    shared["vecP"] = vp
    shared["vecB"] = vb
    rb = np.asarray(inp["rel_bias"], np.float32)
    s = np.arange(128)[:, None]
    q = np.arange(128)[None, :]
    bt = np.zeros((128, 12, 2, 128), np.float32)
    for kind in range(2):
        idx = rel_bucket_np(128 * kind + q - s)
        bt[:, :, kind, :] = np.transpose(rb[idx], (0, 2, 1))
    shared["biasT"] = bt.reshape(128, 12 * 2 * 128)
    shared["b31"] = np.ascontiguousarray(np.broadcast_to(rb[31][None, :], (128, 12)))
    shared["consts"] = host_consts()
    return shared


_CACHE = {}


def kernel(**inputs):
    x = np.asarray(inputs["x"], np.float32)
    B, S, _ = x.shape
    L = int(np.asarray(inputs["w_in"]).shape[0])
    key = (S, L)
    if key not in _CACHE:
        _CACHE[key] = Kern(S, L).build()
    nc = _CACHE[key]
    shared = prep_inputs(inputs, S, L)
    mem = np.asarray(inputs["mem"], np.float32)
    ncore = B
    in_maps = []
    for c in range(ncore):
        m = dict(shared)
        m["x"] = np.ascontiguousarray(x[c])
        m["mem"] = np.ascontiguousarray(mem[c])
        in_maps.append(m)
    res = run_bass_kernel_spmd(nc, in_maps, core_ids=list(range(ncore)))
    out = np.stack([np.asarray(res.results[b]["out"], np.float32) for b in range(B)], axis=0)
    return out
```

```python
import math
from contextlib import ExitStack
import numpy as np
import concourse.bass as bass
import concourse.mybir as mybir
from concourse.bass_utils import run_bass_kernel_spmd

F32 = mybir.dt.float32
BF16 = mybir.dt.bfloat16
AF = mybir.ActivationFunctionType
ALU = mybir.AluOpType
AX = mybir.AxisListType

D = 2048
NDC = 16
IN_COLS = 4176
FFN = 5632
NFC = 44
EPS = 1e-6
TOPK = 256
NBIS = 16
NEG = -30000.0

VP_GIN, VP_CQ, VP_CKV, VP_XPRE, VP_MEMG, VP_FPRE, VP_SUBG = 0, 16, 20, 22, 38, 54, 70
NVP = 71
VB_MIXPOST, VB_XPOST, VB_FPOST, VB_LNG, VB_LNB, VB_BS, VB_LAM = 0, 2048, 4096, 6144, 6656, 7168, 7680
VB_FPRE = 7936
NVB = 9984


class Buf:
    __slots__ = ("w", "r", "dsem")

    def __init__(self):
        self.w = None
        self.r = {}
        self.dsem = None


class _Sem:
    def __init__(self):
        self.sem = None
        self.n = 0


class Eng:
    def __init__(self, name):
        self.name = name
        self.sem = None
        self.n = 0
        self.prog = []
        self.waited = {}

    def wait(self, tok):
        src, val = tok
        if self.waited.get(id(src), 0) >= val:
            return
        self.waited[id(src)] = val
        self.prog.append(lambda h, src=src, val=val: h.wait_ge(src.sem, val))


class Phase:
    def __init__(self, kern, name):
        self.kern = kern
        self.nc = kern.nc
        self.name = name
        self.pe, self.dve, self.act, self.pool, self.sp = kern.engs
        self.engs = kern.engs
        for e in self.engs:
            e.prog = []
        self.dsems = []
        self.npool = 0

    @staticmethod
    def _deps(reads, writes):
        deps = []
        for b in reads:
            if b.w is not None:
                deps.append(b.w)
        for b in writes:
            if b.w is not None:
                deps.append(b.w)
            deps.extend(b.r.values())
        return deps

    @staticmethod
    def _commit(tok, reads, writes):
        k = id(tok[0])
        for b in reads:
            b.r[k] = tok
        for b in writes:
            b.w = tok
            b.r = {}

    def op(self, eng, fn, reads=(), writes=(), pe_acc=False):
        for d in self._deps(reads, writes):
            if pe_acc and d[0] is eng:
                continue
            eng.wait(d)
        eng.n += 1
        tok = (eng, eng.n)
        eng.prog.append(lambda h, fn=fn, eng=eng: fn(h).then_inc(eng.sem, 1))
        self._commit(tok, reads, writes)
        return tok

    def dma(self, out, in_, reads=(), writes=(), sbuf=None, **kw):
        eng = self.sp
        for d in self._deps(reads, writes):
            eng.wait(d)
        if sbuf.dsem is None:
            sbuf.dsem = self.kern.get_dsem(self.npool)
            self.npool += 1
            self.dsems.append(sbuf.dsem)
        ds = sbuf.dsem
        ds.n += 16
        tok = (ds, ds.n)
        eng.prog.append(lambda h, out=out, in_=in_, kw=kw, ds=ds: h.dma_start(out=out, in_=in_, **kw).then_inc(ds.sem, 16))
        self._commit(tok, reads, writes)
        return tok

    def load(self, out, in_, buf, **kw):
        return self.dma(out, in_, writes=[buf], sbuf=buf, **kw)

    def store(self, out, in_, buf, **kw):
        return self.dma(out, in_, reads=[buf], sbuf=buf, **kw)

    def emit(self, st):
        nc = self.nc
        for ds in self.dsems:
            self.sp.wait((ds, ds.n))
        block = st.enter_context(nc.Block(self.name))
        for e in self.engs:
            def body(h, e=e):
                for c in e.prog:
                    c(h)
            getattr(block, e.name)(body)


class Kern:
    def __init__(self, S, L, debug=False):
        self.S, self.L, self.debug = S, L, debug
        self.nb = S // 128
        self.ng = S // 512
        nc = self.nc = bass.Bass("TRN2", target_bir_lowering=False)
        self.gst = ExitStack()
        self.engs = [Eng(n) for n in ("tensor", "vector", "scalar", "gpsimd", "sync")]
        for e in self.engs:
            e.sem = self.gst.enter_context(nc.semaphore(f"prog_{e.name}"))
        self.dpool = []

        self._ext_shapes = {
            "x": [S, D], "mem": [256, D], "w_in": [L, D, IN_COLS], "w_out": [L, D, D],
            "b_w_uq": [L, 512, 768], "b_w_qidx": [L, 512, 1024], "b_w_uk": [L, 256, 768], "b_w_uv": [L, 256, 768],
            "cwsT": [L, 128, 512], "x_wq": [L, D, 512], "x_wkv": [L, D, 1024], "x_wo": [L, 512, D],
            "f_w_gu": [L, D, 2, FFN], "f_w_down": [L, FFN, D], "vecP": [L, 128, NVP], "vecB": [L, 128, NVB],
            "biasT": [128, 12 * 2 * 128], "b31": [128, 12], "consts": [128, 128 * 4 + 32],
        }
        self._ext = {}
        self.out = nc.dram_tensor("out", [S, D], F32, kind="ExternalOutput").ap()

        def scr(name, shape, dt):
            kind = "ExternalOutput" if debug else "Internal"
            return nc.dram_tensor(name, list(shape), dt, kind=kind).ap()

        self.h = scr("h_scr", [S, D], F32)
        self.qaT = scr("qaT", [6, 128, S], BF16)
        self.kaT = scr("kaT", [6, 128, S], BF16)
        self.va = scr("va", [S, 768], BF16)
        self.qbT = scr("qbT", [6, 128, S], BF16)
        self.kbT = scr("kbT", [6, 128, S], BF16)
        self.vb = scr("vb", [S, 768], BF16)
        self.qidxT = scr("qidxT", [8, 128, S], BF16)
        self.kidxT = scr("kidxT", [64, S], BF16)
        self.widx = scr("widx", [S, 16], F32)
        self.mixT = scr("mixT", [16, 128, S], BF16)
        self.MTd = scr("MTd", [self.ng, 128, self.nb, 512], BF16)
        self.yscr = scr("yscr", [S, D], F32)
        self.kmT = scr("kmT", [128, 4, 256], BF16)
        self.vm = scr("vm", [256, 512], BF16)

    def get_dsem(self, i):
        while len(self.dpool) <= i:
            d = _Sem()
            d.sem = self.gst.enter_context(self.nc.semaphore(f"dma_{len(self.dpool)}"))
            self.dpool.append(d)
        return self.dpool[i]

    def E(self, name):
        if name not in self._ext:
            self._ext[name] = self.nc.dram_tensor(name, list(self._ext_shapes[name]), F32, kind="ExternalInput").ap()
        return self._ext[name]

    def build(self, phases=None):
        want = lambda p: phases is None or p in phases
        for l in range(self.L):
            src = self.E("x") if l == 0 else self.h
            if want("p1a"):
                self.p1a(l, src)
            if want("p1b"):
                self.p1b(l, src)
            if want("attA"):
                self.attn(l, "A")
            if want("bidx"):
                self.bindex(l)
            if want("attB"):
                self.attn(l, "B")
            if want("p3"):
                self.memkv(l)
                self.p3(l)
            if want("ffn"):
                self.ffn(l)
                self.ffn_post(l, self.out if l == self.L - 1 else self.h)
        self.gst.close()
        return self.nc

    def begin(self, name, st, want=("ident",)):
        nc = self.nc
        ph = Phase(self, name)
        sb = lambda nm, shape, dt: st.enter_context(nc.sbuf_tensor(f"{name}_{nm}", list(shape), dt))
        pst = lambda nm, shape, dt: st.enter_context(nc.psum_tensor(f"{name}_{nm}", list(shape), dt))
        cs = self.consts_load(ph, sb, want)
        epsT = sb("epsT", [128, 1], F32); b_eps = Buf()
        ph.op(ph.dve, lambda h: h.memset(epsT[:], EPS), writes=[b_eps])
        self.eps_ap = epsT[:, 0:1]
        self.b_eps = b_eps
        return ph, sb, pst, cs

    def consts_load(self, ph, sb, want):
        res = {}
        if "ident" in want:
            cf = sb("c_idf", [128, 128], F32); bcf = Buf()
            ph.load(cf[:], self.E("consts")[:, 0:128], bcf)
            ib = sb("c_idb", [128, 128], BF16); bib = Buf()
            ph.op(ph.dve, lambda h: h.tensor_copy(out=ib[:], in_=cf[:]), reads=[bcf], writes=[bib])
            res["ident"] = (ib, bib)
        for nm, c0, n in (("mask01T", 128, 128), ("causneg", 256, 128), ("causnegT", 384, 128), ("pow2", 512, 32)):
            if nm in want:
                t = sb("c_" + nm, [128, n], F32); bt = Buf()
                ph.load(t[:], self.E("consts")[:, c0:c0 + n], bt)
                res[nm] = (t, bt)
        return res

    def rms_tile(self, ph, ht, b_ht, junk, b_junk, ss, rt, rinv, b_small, width, act_in=None):
        src = ht if act_in is None else act_in
        ph.op(ph.act, lambda h: h.activation(out=junk, in_=src, func=AF.Square, accum_out=ss), reads=[b_ht], writes=[b_junk, b_small])
        ph.op(ph.act, lambda h: h.activation(out=rt, in_=ss, func=AF.Sqrt, scale=1.0 / width, bias=self.eps_ap), reads=[b_small, self.b_eps], writes=[b_small])
        ph.op(ph.dve, lambda h: h.reciprocal(out=rinv, in_=rt), reads=[b_small], writes=[b_small])

    def p1a(self, l, src):
        nc, S, nb = self.nc, self.S, self.nb
        with ExitStack() as st:
            ph, sb, pst, cs = self.begin(f"p1a{l}", st)
            ident, b_ident = cs["ident"]
            vp = sb("vp", [128, NVP], F32); b_vp = Buf()
            ph.load(vp[:], self.E("vecP")[l], b_vp)
            NW = 2304
            win = sb("win", [128, NDC, NW], BF16); b_win = Buf()
            stg = [sb(f"stg{i}", [128, NW], F32) for i in range(2)]; b_stg = [Buf(), Buf()]
            for dc in range(NDC):
                s = dc % 2
                ph.load(stg[s][:], self.E("w_in")[l, dc * 128:(dc + 1) * 128, 0:NW], b_stg[s])
                ph.op(ph.dve, lambda h, s=s, dc=dc: h.tensor_scalar(out=win[:, dc, :], in0=stg[s][:], scalar1=vp[:, VP_GIN + dc:VP_GIN + dc + 1], scalar2=None, op0=ALU.mult),
                      reads=[b_stg[s], b_vp], writes=[b_win])
            hts = [sb(f"ht{i}", [128, D], F32) for i in range(2)]; b_ht = [Buf(), Buf()]
            junk = sb("junk", [128, D], BF16); b_junk = Buf()
            sm = sb("sm", [128, 4], F32); b_sm = Buf()
            xn = sb("xn", [128, D], BF16); b_xn = Buf()
            xnT = sb("xnT", [128, NDC, 128], BF16); b_xnT = Buf()
            fm = sb("fm", [128, 12, 128], BF16); b_fm = Buf()
            vas = sb("vas", [128, 768], BF16); b_vas = Buf()
            psB = pst("psB", [128, 2, 1024], BF16); b_psB = [Buf(), Buf()]
            psF = pst("psF", [128, 5, 512], F32); b_psF = [Buf() for _ in range(5)]

            def ld(i):
                ph.load(hts[i % 2][:], src[i * 128:(i + 1) * 128, :], b_ht[i % 2])

            ld(0)
            for i in range(nb):
                if i + 1 < nb:
                    ld(i + 1)
                ht, bh = hts[i % 2], b_ht[i % 2]
                self.rms_tile(ph, ht[:], bh, junk[:], b_junk, sm[:, 0:1], sm[:, 1:2], sm[:, 2:3], b_sm, D)
                ph.op(ph.dve, lambda h, ht=ht: h.tensor_scalar(out=xn[:], in0=ht[:], scalar1=sm[:, 2:3], scalar2=None, op0=ALU.mult),
                      reads=[bh, b_sm], writes=[b_xn])
                for dc in range(NDC):
                    ph.op(ph.pe, lambda h, dc=dc: h.transpose(out=psB[:, dc // 8, (dc % 8) * 128:(dc % 8 + 1) * 128], in_=xn[:, dc * 128:(dc + 1) * 128], identity=ident[:]),
                          reads=[b_xn, b_ident], writes=[b_psB[dc // 8]], pe_acc=True)
                for k in range(2):
                    ph.op(ph.act, lambda h, k=k: h.copy(out=xnT[:, k * 8:(k + 1) * 8, :], in_=psB[:, k, :]), reads=[b_psB[k]], writes=[b_xnT])
                for c in range(12):
                    bk = c // 4
                    for dc in range(NDC):
                        ph.op(ph.pe, lambda h, c=c, dc=dc, bk=bk: h.matmul(psF[:, bk, (c % 4) * 128:(c % 4 + 1) * 128], lhsT=win[:, dc, c * 128:(c + 1) * 128], rhs=xnT[:, dc, :], start=(dc == 0), stop=(dc == NDC - 1)),
                              reads=[b_win, b_xnT], writes=[b_psF[bk]], pe_acc=True)
                for (bk, c0, n) in ((3, 0, 512), (4, 512, 256)):
                    for dc in range(NDC):
                        ph.op(ph.pe, lambda h, bk=bk, c0=c0, n=n, dc=dc: h.matmul(psF[:, bk, 0:n], lhsT=xnT[:, dc, :], rhs=win[:, dc, 1536 + c0:1536 + c0 + n], start=(dc == 0), stop=(dc == NDC - 1)),
                              reads=[b_win, b_xnT], writes=[b_psF[bk]], pe_acc=True)
                for bk in range(3):
                    eng = ph.act if bk % 2 == 0 else ph.dve
                    if eng is ph.act:
                        ph.op(eng, lambda h, bk=bk: h.copy(out=fm[:, bk * 4:(bk + 1) * 4, :], in_=psF[:, bk, :]), reads=[b_psF[bk]], writes=[b_fm])
                    else:
                        ph.op(eng, lambda h, bk=bk: h.tensor_copy(out=fm[:, bk * 4:(bk + 1) * 4, :], in_=psF[:, bk, :]), reads=[b_psF[bk]], writes=[b_fm])
                ph.op(ph.dve, lambda h: h.tensor_copy(out=vas[:, 0:512], in_=psF[:, 3, :]), reads=[b_psF[3]], writes=[b_vas])
                ph.op(ph.act, lambda h: h.copy(out=vas[:, 512:768], in_=psF[:, 4, 0:256]), reads=[b_psF[4]], writes=[b_vas])
                tk = slice(i * 128, (i + 1) * 128)
                ph.store(self.qaT[:, :, tk].rearrange("h p t -> p h t"), fm[:, 0:6, :], b_fm)
                ph.store(self.kaT[:, :, tk].rearrange("h p t -> p h t"), fm[:, 6:12, :], b_fm)
                ph.store(self.va[tk, :], vas[:], b_vas)
            ph.emit(st)

    def p1b(self, l, src):
        nc, S, nb = self.nc, self.S, self.nb
        with ExitStack() as st:
            ph, sb, pst, cs = self.begin(f"p1b{l}", st, want=("ident", "mask01T"))
            ident, b_ident = cs["ident"]
            m01, b_m01 = cs["mask01T"]
            vp = sb("vp", [128, NVP], F32); b_vp = Buf()
            ph.load(vp[:], self.E("vecP")[l], b_vp)
            vb3 = sb("vb3", [128, 1536], F32); b_vb3 = Buf()
            ph.load(vb3[:], self.E("vecB")[l, :, VB_LNG:VB_LNG + 1536], b_vb3)
            NW = 1872
            C0 = 2304
            win = sb("win", [128, NDC, NW], BF16); b_win = Buf()
            wuq = sb("wuq", [128, 4, 768], BF16); b_wuq = Buf()
            wqi = sb("wqi", [128, 4, 1024], BF16); b_wqi = Buf()
            wuk = sb("wuk", [128, 2, 768], BF16); b_wuk = Buf()
            wuv = sb("wuv", [128, 2, 768], BF16); b_wuv = Buf()
            cws = sb("cws", [128, 512], BF16); b_cws = Buf()
            stg = [sb(f"stg{i}", [128, NW], F32) for i in range(2)]; b_stg = [Buf(), Buf()]
            cnt = [0]

            def wload(dst, b_dst, srcap, n, gcol):
                s_ = cnt[0] % 2; cnt[0] += 1
                ph.load(stg[s_][:, 0:n], srcap, b_stg[s_])
                ph.op(ph.dve, lambda h: h.tensor_scalar(out=dst, in0=stg[s_][:, 0:n], scalar1=vp[:, gcol:gcol + 1], scalar2=None, op0=ALU.mult),
                      reads=[b_stg[s_], b_vp], writes=[b_dst])

            for dc in range(NDC):
                wload(win[:, dc, :], b_win, self.E("w_in")[l, dc * 128:(dc + 1) * 128, C0:C0 + NW], NW, VP_GIN + dc)
            for rc in range(4):
                wload(wuq[:, rc, :], b_wuq, self.E("b_w_uq")[l, rc * 128:(rc + 1) * 128, :], 768, VP_CQ + rc)
                wload(wqi[:, rc, :], b_wqi, self.E("b_w_qidx")[l, rc * 128:(rc + 1) * 128, :], 1024, VP_CQ + rc)
            for rc in range(2):
                wload(wuk[:, rc, :], b_wuk, self.E("b_w_uk")[l, rc * 128:(rc + 1) * 128, :], 768, VP_CKV + rc)
                wload(wuv[:, rc, :], b_wuv, self.E("b_w_uv")[l, rc * 128:(rc + 1) * 128, :], 768, VP_CKV + rc)
            s_ = cnt[0] % 2; cnt[0] += 1
            ph.load(stg[s_][:, 0:512], self.E("cwsT")[l], b_stg[s_])
            for g in range(4):
                ph.op(ph.dve, lambda h, g=g, s_=s_: h.tensor_tensor(out=cws[:, g * 128:(g + 1) * 128], in0=stg[s_][:, g * 128:(g + 1) * 128], in1=m01[:], op=ALU.mult),
                      reads=[b_stg[s_], b_m01], writes=[b_cws])

            hts = [sb(f"ht{i}", [128, D], F32) for i in range(2)]; b_ht = [Buf(), Buf()]
            junk = sb("junk", [128, D], BF16); b_junk = Buf()
            sm = sb("sm", [128, 16], F32); b_sm = Buf()
            xn = sb("xn", [128, D], BF16); b_xn = Buf()
            xnT = sb("xnT", [128, NDC, 128], BF16); b_xnT = Buf()
            cn = sb("cn", [128, 768], BF16); b_cn = Buf()
            cT = sb("cT", [128, 6, 128], BF16); b_cT = Buf()
            gv = sb("gv", [128, 512], F32); b_gv = Buf()
            tmp = sb("tmp", [128, 512], F32); b_tmp = Buf()
            vn = sb("vn", [128, 512], BF16); b_vn = Buf()
            uT = sb("uT", [128, 4, 128], F32); b_uT = Buf()
            fm = sb("fm", [128, 24, 128], BF16); b_fm = Buf()
            kix = sb("kix", [64, 128], BF16); b_kix = Buf()
            vbs = sb("vbs", [128, 768], BF16); b_vbs = Buf()
            wix = sb("wix", [128, 16], F32); b_wix = Buf()
            psB = pst("psB", [128, 2, 1024], BF16); b_psB = [Buf(), Buf()]
            psF = pst("psF", [128, 6, 512], F32); bF = [Buf() for _ in range(6)]
            WSC = (16 ** -0.5) * (64 ** -0.5)

            def ld(i):
                ph.load(hts[i % 2][:], src[i * 128:(i + 1) * 128, :], b_ht[i % 2])

            def mm(out, lhsT, rhs, first, last, reads, wbuf):
                ph.op(ph.pe, lambda h: h.matmul(out, lhsT=lhsT, rhs=rhs, start=first, stop=last), reads=reads, writes=[wbuf], pe_acc=True)

            ld(0)
            for i in range(nb):
                if i + 1 < nb:
                    ld(i + 1)
                ht, bh = hts[i % 2], b_ht[i % 2]
                self.rms_tile(ph, ht[:], bh, junk[:], b_junk, sm[:, 0:1], sm[:, 1:2], sm[:, 2:3], b_sm, D)
                ph.op(ph.dve, lambda h, ht=ht: h.tensor_scalar(out=xn[:], in0=ht[:], scalar1=sm[:, 2:3], scalar2=None, op0=ALU.mult),
                      reads=[bh, b_sm], writes=[b_xn])
                for dc in range(NDC):
                    ph.op(ph.pe, lambda h, dc=dc: h.transpose(out=psB[:, dc // 8, (dc % 8) * 128:(dc % 8 + 1) * 128], in_=xn[:, dc * 128:(dc + 1) * 128], identity=ident[:]),
                          reads=[b_xn, b_ident], writes=[b_psB[dc // 8]], pe_acc=True)
                for k in range(2):
                    ph.op(ph.act, lambda h, k=k: h.copy(out=xnT[:, k * 8:(k + 1) * 8, :], in_=psB[:, k, :]), reads=[b_psB[k]], writes=[b_xnT])
                for dc in range(NDC):
                    mm(psF[:, 0, :], xnT[:, dc, :], win[:, dc, 0:512], dc == 0, dc == NDC - 1, [b_win, b_xnT], bF[0])
                for dc in range(NDC):
                    mm(psF[:, 1, 0:336], xnT[:, dc, :], win[:, dc, 512:848], dc == 0, dc == NDC - 1, [b_win, b_xnT], bF[1])
                for dc in range(NDC):
                    mm(psF[:, 2, :], xnT[:, dc, :], win[:, dc, 1360:1872], dc == 0, dc == NDC - 1, [b_win, b_xnT], bF[2])
                for dc in range(NDC):
                    mm(psF[0:64, 3, 0:128], win[:, dc, 768:832], xnT[:, dc, :], dc == 0, dc == NDC - 1, [b_win, b_xnT], bF[3])
                for c in range(4):
                    bk, o = (3, 128 + c * 128) if c < 3 else (4, 0)
                    for dc in range(NDC):
                        mm(psF[:, bk, o:o + 128], win[:, dc, 848 + c * 128:848 + (c + 1) * 128], xnT[:, dc, :], dc == 0, dc == NDC - 1, [b_win, b_xnT], bF[bk])
                self.rms_tile(ph, None, bF[0], junk[:, 0:512], b_junk, sm[:, 3:4], sm[:, 4:5], sm[:, 5:6], b_sm, 512, act_in=psF[:, 0, :])
                ph.op(ph.dve, lambda h: h.tensor_scalar(out=cn[:, 0:512], in0=psF[:, 0, :], scalar1=sm[:, 5:6], scalar2=None, op0=ALU.mult), reads=[bF[0], b_sm], writes=[b_cn])
                self.rms_tile(ph, None, bF[1], junk[:, 0:256], b_junk, sm[:, 6:7], sm[:, 7:8], sm[:, 8:9], b_sm, 256, act_in=psF[:, 1, 0:256])
                ph.op(ph.dve, lambda h: h.tensor_scalar(out=cn[:, 512:768], in0=psF[:, 1, 0:256], scalar1=sm[:, 8:9], scalar2=None, op0=ALU.mult), reads=[bF[1], b_sm], writes=[b_cn])
                ph.op(ph.dve, lambda h: h.tensor_scalar(out=wix[:], in0=psF[:, 1, 320:336], scalar1=WSC, scalar2=None, op0=ALU.mult), reads=[bF[1]], writes=[b_wix])
                ph.op(ph.act, lambda h: h.copy(out=kix[:], in_=psF[0:64, 3, 0:128]), reads=[bF[3]], writes=[b_kix])
                ph.op(ph.act, lambda h: h.activation(out=uT[:, 0:3, :], in_=psF[:, 3, 128:512], func=AF.Gelu_apprx_tanh), reads=[bF[3]], writes=[b_uT])
                ph.op(ph.act, lambda h: h.activation(out=uT[:, 3, :], in_=psF[:, 4, 0:128], func=AF.Gelu_apprx_tanh), reads=[bF[4]], writes=[b_uT])
                ph.op(ph.act, lambda h: h.activation(out=gv[:], in_=psF[:, 2, :], func=AF.Gelu_apprx_tanh, accum_out=sm[:, 9:10]), reads=[bF[2]], writes=[b_gv, b_sm])
                ph.op(ph.dve, lambda h: h.tensor_scalar(out=sm[:, 10:11], in0=sm[:, 9:10], scalar1=-1.0 / 512, scalar2=None, op0=ALU.mult), reads=[b_sm], writes=[b_sm])
                ph.op(ph.act, lambda h: h.activation(out=junk[:, 0:512], in_=gv[:], func=AF.Square, bias=sm[:, 10:11], accum_out=sm[:, 11:12]), reads=[b_gv, b_sm], writes=[b_junk, b_sm])
                ph.op(ph.act, lambda h: h.activation(out=sm[:, 12:13], in_=sm[:, 11:12], func=AF.Sqrt, scale=1.0 / 512, bias=self.eps_ap), reads=[b_sm, self.b_eps], writes=[b_sm])
                ph.op(ph.dve, lambda h: h.reciprocal(out=sm[:, 13:14], in_=sm[:, 12:13]), reads=[b_sm], writes=[b_sm])
                ph.op(ph.dve, lambda h: h.tensor_scalar(out=tmp[:], in0=gv[:], scalar1=sm[:, 10:11], scalar2=sm[:, 13:14], op0=ALU.add, op1=ALU.mult), reads=[b_gv, b_sm], writes=[b_tmp])
                ph.op(ph.dve, lambda h: h.tensor_tensor(out=tmp[:], in0=tmp[:], in1=vb3[:, 0:512], op=ALU.mult), reads=[b_vb3], writes=[b_tmp])
                ph.op(ph.dve, lambda h: h.tensor_tensor(out=vn[:], in0=tmp[:], in1=vb3[:, 512:1024], op=ALU.add), reads=[b_tmp, b_vb3], writes=[b_vn])
                for k in range(6):
                    ph.op(ph.pe, lambda h, k=k: h.transpose(out=psB[:, 0, k * 128:(k + 1) * 128], in_=cn[:, k * 128:(k + 1) * 128], identity=ident[:]),
                          reads=[b_cn, b_ident], writes=[b_psB[0]], pe_acc=True)
                ph.op(ph.act, lambda h: h.copy(out=cT[:], in_=psB[:, 0, 0:768]), reads=[b_psB[0]], writes=[b_cT])
                def fm_group(bk, o, wt, b_wt, nrc, rc0, col0, reads_extra=()):
                    for rc in range(nrc):
                        mm(psF[:, bk, o:o + 128], wt[:, rc, col0:col0 + 128], cT[:, rc0 + rc, :], rc == 0, rc == nrc - 1, [b_wt, b_cT], bF[bk])
                for hh in range(4):
                    fm_group(0, hh * 128, wuq, b_wuq, 4, 0, hh * 128)
                for hh in range(4, 6):
                    fm_group(1, (hh - 4) * 128, wuq, b_wuq, 4, 0, hh * 128)
                for hh in range(2):
                    fm_group(1, 256 + hh * 128, wuk, b_wuk, 2, 4, hh * 128)
                for hh in range(2, 6):
                    fm_group(2, (hh - 2) * 128, wuk, b_wuk, 2, 4, hh * 128)
                for pr in range(4):
                    fm_group(3, pr * 128, wqi, b_wqi, 4, 0, pr * 128)
                for pr in range(4, 8):
                    fm_group(4, (pr - 4) * 128, wqi, b_wqi, 4, 0, pr * 128)
                for rc in range(2):
                    mm(psF[:, 5, :], cT[:, 4 + rc, :], wuv[:, rc, 0:512], rc == 0, rc == 1, [b_wuv, b_cT], bF[5])
                ph.op(ph.act, lambda h: h.copy(out=fm[:, 0:4, :], in_=psF[:, 0, :]), reads=[bF[0]], writes=[b_fm])
                ph.op(ph.dve, lambda h: h.tensor_copy(out=fm[:, 4:6, :], in_=psF[:, 1, 0:256]), reads=[bF[1]], writes=[b_fm])
                ph.op(ph.dve, lambda h: h.tensor_copy(out=fm[:, 6:8, :], in_=psF[:, 1, 256:512]), reads=[bF[1]], writes=[b_fm])
                ph.op(ph.act, lambda h: h.copy(out=fm[:, 8:12, :], in_=psF[:, 2, :]), reads=[bF[2]], writes=[b_fm])
                ph.op(ph.dve, lambda h: h.tensor_copy(out=fm[:, 12:16, :], in_=psF[:, 3, :]), reads=[bF[3]], writes=[b_fm])
                ph.op(ph.act, lambda h: h.copy(out=fm[:, 16:20, :], in_=psF[:, 4, :]), reads=[bF[4]], writes=[b_fm])
                ph.op(ph.dve, lambda h: h.tensor_copy(out=vbs[:, 0:512], in_=psF[:, 5, :]), reads=[bF[5]], writes=[b_vbs])
                for rc in range(2):
                    mm(psF[:, 0, 0:256], cT[:, 4 + rc, :], wuv[:, rc, 512:768], rc == 0, rc == 1, [b_wuv, b_cT], bF[0])
                ph.op(ph.act, lambda h: h.copy(out=vbs[:, 512:768], in_=psF[:, 0, 0:256]), reads=[bF[0]], writes=[b_vbs])
                for g in range(4):
                    mm(psF[:, 1, g * 128:(g + 1) * 128], vn[:, g * 128:(g + 1) * 128], cws[:, g * 128:(g + 1) * 128], True, True, [b_vn, b_cws], bF[1])
                ph.op(ph.dve, lambda h: h.tensor_tensor(out=tmp[:], in0=psF[:, 1, :], in1=vb3[:, 1024:1536], op=ALU.add), reads=[bF[1], b_vb3], writes=[b_tmp])
                ph.op(ph.dve, lambda h: h.tensor_tensor(out=fm[:, 20:24, :], in0=tmp[:], in1=uT[:], op=ALU.mult), reads=[b_tmp, b_uT], writes=[b_fm])
                tk = slice(i * 128, (i + 1) * 128)
                ph.store(self.qbT[:, :, tk].rearrange("h p t -> p h t"), fm[:, 0:6, :], b_fm)
                ph.store(self.kbT[:, :, tk].rearrange("h p t -> p h t"), fm[:, 6:12, :], b_fm)
                ph.store(self.qidxT[:, :, tk].rearrange("h p t -> p h t"), fm[:, 12:20, :], b_fm)
                ph.store(self.mixT[12:16, :, tk].rearrange("h p t -> p h t"), fm[:, 20:24, :], b_fm)
                ph.store(self.vb[tk, :], vbs[:], b_vbs)
                ph.store(self.kidxT[:, tk], kix[:], b_kix)
                ph.store(self.widx[tk, :], wix[:], b_wix)
            ph.emit(st)


    def attn(self, l, kind):
        nc, S, nb, ng = self.nc, self.S, self.nb, self.ng
        isA = kind == "A"
        qT_d, kT_d, v_d = (self.qaT, self.kaT, self.va) if isA else (self.qbT, self.kbT, self.vb)
        H0 = 0 if isA else 6
        nmap = 2 if isA else 1
        scale = 64 ** -0.5 if isA else 128 ** -0.5
        lam_init = 0.8 - 0.6 * math.exp(-0.3 * l)
        with ExitStack() as st:
            ph, sb, pst, cs = self.begin(f"att{kind}{l}", st, want=("ident", "causnegT"))
            ident, b_ident = cs["ident"]
            cneg, b_cneg = cs["causnegT"]
            kT = sb("kT", [128, 6, S], BF16); b_kT = Buf()
            ph.load(kT[:], kT_d.rearrange("h p t -> p h t"), b_kT)
            v1 = sb("v1", [128, nb, 6, 129], BF16); b_v1 = Buf()
            ph.op(ph.pool, lambda h: h.memset(v1[:, :, :, 128:129], 1.0), writes=[b_v1])
            for j0 in range(nb):
                ph.dma(v1[:, j0, :, 0:128], v_d[j0 * 128:(j0 + 1) * 128, :].rearrange("p (h d) -> p h d", d=128), writes=[b_v1], sbuf=b_v1)
            b31 = sb("b31", [128, 12], F32); b_b31 = Buf()
            ph.load(b31[:], self.E("b31"), b_b31)
            braw = sb("braw", [128, 6, 2, 128], F32); b_braw = Buf()
            ph.load(braw[:], self.E("biasT")[:, H0 * 256:(H0 + 6) * 256].rearrange("p (h k q) -> p h k q", h=6, k=2), b_braw)
            bt = sb("bt", [128, 6, 2, 128], BF16); b_bt = Buf()
            for hh in range(6):
                ph.op(ph.dve, lambda h, hh=hh: h.tensor_tensor(out=braw[:, hh, 0, :], in0=braw[:, hh, 0, :], in1=cneg[:], op=ALU.add), reads=[b_cneg], writes=[b_braw])
                ph.op(ph.dve, lambda h, hh=hh: h.tensor_scalar(out=bt[:, hh, :, :], in0=braw[:, hh, :, :], scalar1=b31[:, H0 + hh:H0 + hh + 1], scalar2=1.0 / scale, op0=ALU.subtract, op1=ALU.mult),
                      reads=[b_braw, b_b31], writes=[b_bt])
            sm = sb("sm", [128, 32], F32); b_sm = Buf()
            if isA:
                vbl = sb("vbl", [128, 256], F32); b_vbl = Buf()
                ph.load(vbl[:], self.E("vecB")[l, :, VB_LAM:VB_LAM + 256], b_vbl)
                pr = sb("pr", [128, 128], F32); b_pr = Buf()
                ph.op(ph.dve, lambda h: h.tensor_tensor(out=pr[:, 0:64], in0=vbl[:, 0:64], in1=vbl[:, 64:128], op=ALU.mult), reads=[b_vbl], writes=[b_pr])
                ph.op(ph.dve, lambda h: h.tensor_tensor(out=pr[:, 64:128], in0=vbl[:, 128:192], in1=vbl[:, 192:256], op=ALU.mult), reads=[b_vbl], writes=[b_pr])
                ph.op(ph.dve, lambda h: h.reduce_sum(out=sm[:, 0:1], in_=pr[:, 0:64], axis=AX.X), reads=[b_pr], writes=[b_sm])
                ph.op(ph.dve, lambda h: h.reduce_sum(out=sm[:, 1:2], in_=pr[:, 64:128], axis=AX.X), reads=[b_pr], writes=[b_sm])
                ph.op(ph.act, lambda h: h.activation(out=sm[:, 2:4], in_=sm[:, 0:2], func=AF.Exp), reads=[b_sm], writes=[b_sm])
                ph.op(ph.dve, lambda h: h.tensor_tensor(out=sm[:, 4:5], in0=sm[:, 2:3], in1=sm[:, 3:4], op=ALU.subtract), reads=[b_sm], writes=[b_sm])
                ph.op(ph.dve, lambda h: h.tensor_scalar(out=sm[:, 5:6], in0=sm[:, 4:5], scalar1=lam_init, scalar2=-1.0, op0=ALU.add, op1=ALU.mult), reads=[b_sm], writes=[b_sm])
            b_qg = [Buf(), Buf()]
            if not isA:
                qg = [sb(f"qg{i}", [128, 6, 512], BF16) for i in range(2)]
            if not isA:
                mt = [sb(f"mt{i}", [128, nb, 512], BF16) for i in range(2)]; b_mt = [Buf(), Buf()]
            pts = [sb(f"pt{i}", [128, 512], BF16) for i in range(3)]; b_pt = [Buf() for _ in range(3)]
            o0 = sb("o0", [128, 4, 128], F32); b_o0 = Buf()
            on = sb("on", [128, 4, 128], BF16); b_on = Buf()
            junk = sb("junk", [128, 128], BF16); b_junk = Buf()
            outT = [sb(f"outT{i}", [128, 512], BF16) for i in range(2)]; b_outT = [Buf(), Buf()]
            psS = pst("psS", [128, 3, 512], F32); b_psS = [Buf() for _ in range(3)]
            psA = pst("psA", [128, 4, 512], F32); b_psA = [Buf() for _ in range(4)]
            psT = pst("psT", [128, 1024], BF16); b_psT = Buf()
            rows = lambda m: slice(m * 64, (m + 1) * 64) if isA else slice(0, 128)

            if isA:
                qz = [[sb(f"qz{m}_{i}", [128, 6, 512], BF16) for i in range(2)] for m in range(2)]
                for i in range(2):
                    ph.op(ph.pool, lambda h, i=i: h.memset(qz[0][i][64:128, :, :], 0.0), writes=[b_qg[i]])
                    ph.op(ph.pool, lambda h, i=i: h.memset(qz[1][i][0:64, :, :], 0.0), writes=[b_qg[i]])

            def ldg(G):
                if isA:
                    srcq = qT_d[:, :, G * 512:(G + 1) * 512].rearrange("h p t -> p h t")
                    ph.dma(qz[0][G % 2][0:64, :, :], srcq[0:64], writes=[b_qg[G % 2]], sbuf=b_qg[G % 2])
                    ph.dma(qz[1][G % 2][64:128, :, :], srcq[64:128], writes=[b_qg[G % 2]], sbuf=b_qg[G % 2])
                else:
                    ph.load(qg[G % 2][:], qT_d[:, :, G * 512:(G + 1) * 512].rearrange("h p t -> p h t"), b_qg[G % 2])
                if not isA:
                    nj = 4 * G + 4
                    ph.load(mt[G % 2][:, 0:nj, :], self.MTd[G, :, 0:nj, :], b_mt[G % 2])

            cst = [0]
            cout = [0]
            ldg(0)
            for G in range(ng):
                if G + 1 < ng:
                    ldg(G + 1)
                bq = b_qg[G % 2]
                nj = 4 * G + 4
                steps = [(hh, m, j) for hh in range(6) for m in range(nmap) for j in range(nj)]
                slot = {}

                def emit_qk(idx, G=G, bq=bq):
                    hh, m, j = steps[idx]
                    c0 = max(0, j - 4 * G)
                    cols = slice(c0 * 128, 512)
                    sbk = cst[0] % 3; cst[0] += 1
                    slot[idx] = sbk
                    q_ = qz[m][G % 2] if isA else qg[G % 2]
                    near = [(k, 4 * G + k - j) for k in range(4) if 0 <= 4 * G + k - j <= 1]
                    ph.op(ph.pe, lambda h: h.matmul(psS[:, sbk, cols], lhsT=kT[:, hh, j * 128:(j + 1) * 128], rhs=q_[:, hh, cols], start=True, stop=(len(near) == 0)),
                          reads=[b_kT, bq], writes=[b_psS[sbk]], pe_acc=True)
                    for ii, (k, kd) in enumerate(near):
                        ph.op(ph.pe, lambda h, k=k, kd=kd, ii=ii: h.matmul(psS[:, sbk, k * 128:(k + 1) * 128], lhsT=ident[:], rhs=bt[:, hh, kd, :], start=False, stop=(ii == len(near) - 1)),
                              reads=[b_ident, b_bt], writes=[b_psS[sbk]], pe_acc=True)

                emit_qk(0)
                for idx, (hh, m, j) in enumerate(steps):
                    if idx + 1 < len(steps):
                        emit_qk(idx + 1)
                    aset = ((G * 6 + hh) * nmap + m) % 2
                    accb = (2 * aset, 2 * aset + 1)
                    c0 = max(0, j - 4 * G)
                    cols = slice(c0 * 128, 512)
                    sbk = slot[idx]
                    pt, bp = pts[sbk], b_pt[sbk]
                    ph.op(ph.act, lambda h, pt=pt, sbk=sbk, cols=cols, hh=hh: h.activation(out=pt[:, cols], in_=psS[:, sbk, cols], func=AF.Exp, bias=b31[:, H0 + hh:H0 + hh + 1], scale=scale),
                          reads=[b_psS[sbk], b_b31], writes=[bp])
                    if not isA:
                        mt_ = mt[G % 2]
                        ph.op(ph.dve, lambda h, pt=pt, cols=cols, j=j, mt_=mt_: h.tensor_tensor(out=pt[:, cols], in0=pt[:, cols], in1=mt_[:, j, cols], op=ALU.mult),
                              reads=[b_mt[G % 2]], writes=[bp])
                    for k in range(c0, 4):
                        ab = accb[k // 2]
                        ph.op(ph.pe, lambda h, ab=ab, k=k, pt=pt, j=j, hh=hh, G=G: h.matmul(psA[:, ab, (k % 2) * 129:(k % 2) * 129 + 129], lhsT=pt[:, k * 128:(k + 1) * 128], rhs=v1[:, j, hh, :], start=(j == 0 and k % 2 == 0), stop=(j == 4 * G + k), skip_group_check=True),
                              reads=[bp, b_v1], writes=[b_psA[ab]], pe_acc=True)
                    if j == nj - 1:
                        for k in range(4):
                            ab = accb[k // 2]; o = (k % 2) * 129
                            ph.op(ph.dve, lambda h, ab=ab, o=o, k=k: h.reciprocal(out=sm[:, 8 + k:9 + k], in_=psA[:, ab, o + 128:o + 129]), reads=[b_psA[ab]], writes=[b_sm])
                            if isA and m == 0:
                                ph.op(ph.dve, lambda h, ab=ab, o=o, k=k: h.tensor_scalar(out=o0[:, k, :], in0=psA[:, ab, o:o + 128], scalar1=sm[:, 8 + k:9 + k], scalar2=None, op0=ALU.mult), reads=[b_psA[ab], b_sm], writes=[b_o0])
                            elif isA:
                                ph.op(ph.dve, lambda h, k=k: h.tensor_tensor(out=sm[:, 12 + k:13 + k], in0=sm[:, 8 + k:9 + k], in1=sm[:, 5:6], op=ALU.mult), reads=[b_sm], writes=[b_sm])
                                ph.op(ph.dve, lambda h, ab=ab, o=o, k=k: h.scalar_tensor_tensor(out=o0[:, k, :], in0=psA[:, ab, o:o + 128], scalar=sm[:, 12 + k:13 + k], in1=o0[:, k, :], op0=ALU.mult, op1=ALU.add), reads=[b_psA[ab], b_sm], writes=[b_o0])
                                ph.op(ph.act, lambda h, k=k: h.activation(out=junk[:], in_=o0[:, k, :], func=AF.Square, accum_out=sm[:, 16 + k:17 + k]), reads=[b_o0], writes=[b_junk, b_sm])
                            else:
                                ph.op(ph.dve, lambda h, ab=ab, o=o, k=k: h.tensor_scalar(out=on[:, k, :], in0=psA[:, ab, o:o + 128], scalar1=sm[:, 8 + k:9 + k], scalar2=None, op0=ALU.mult), reads=[b_psA[ab], b_sm], writes=[b_on])
                        if isA and m == 0:
                            continue
                        if isA:
                            ph.op(ph.act, lambda h: h.activation(out=sm[:, 20:24], in_=sm[:, 16:20], func=AF.Sqrt, scale=1.0 / 128, bias=self.eps_ap), reads=[b_sm, self.b_eps], writes=[b_sm])
                            ph.op(ph.dve, lambda h: h.reciprocal(out=sm[:, 24:28], in_=sm[:, 20:24]), reads=[b_sm], writes=[b_sm])
                            for k in range(4):
                                ph.op(ph.dve, lambda h, k=k: h.tensor_scalar(out=on[:, k, :], in0=o0[:, k, :], scalar1=sm[:, 24 + k:25 + k], scalar2=None, op0=ALU.mult), reads=[b_o0, b_sm], writes=[b_on])
                        for k in range(4):
                            ph.op(ph.pe, lambda h, k=k: h.transpose(out=psT[:, k * 128:(k + 1) * 128], in_=on[:, k, :], identity=ident[:]), reads=[b_on, b_ident], writes=[b_psT], pe_acc=True)
                        oi = cout[0] % 2; cout[0] += 1
                        ph.op(ph.act, lambda h, oi=oi: h.copy(out=outT[oi][:], in_=psT[:, 0:512]), reads=[b_psT], writes=[b_outT[oi]])
                        ph.store(self.mixT[H0 + hh, :, G * 512:(G + 1) * 512], outT[oi][:], b_outT[oi])
            ph.emit(st)


    def bindex(self, l):
        nc, S, nb, ng = self.nc, self.S, self.nb, self.ng
        with ExitStack() as st:
            ph, sb, pst, cs = self.begin(f"bidx{l}", st, want=("ident", "causneg", "pow2"))
            ident, b_ident = cs["ident"]
            cneg, b_cneg = cs["causneg"]
            pow2, b_pow2 = cs["pow2"]
            kx = sb("kx", [128, S], BF16); b_kx = Buf()
            ph.dma(kx[0:64, :], self.kidxT, writes=[b_kx], sbuf=b_kx)
            ph.dma(kx[64:128, :], self.kidxT, writes=[b_kx], sbuf=b_kx)
            b_qi = [Buf(), Buf()]
            qz = [[sb(f"qz{e}_{i}", [128, 8, 128], BF16) for i in range(2)] for e in range(2)]
            for i in range(2):
                ph.op(ph.pool, lambda h, i=i: h.memset(qz[0][i][64:128, :, :], 0.0), writes=[b_qi[i]])
                ph.op(ph.pool, lambda h, i=i: h.memset(qz[1][i][0:64, :, :], 0.0), writes=[b_qi[i]])
            wi = [sb(f"wi{i}", [128, 16], F32) for i in range(2)]; b_wi = [Buf(), Buf()]
            wab = [sb(f"wab{i}", [128, 16], F32) for i in range(2)]; b_wab = [Buf(), Buf()]
            wsg = [sb(f"wsg{i}", [128, 16], F32) for i in range(2)]
            dsg = [sb(f"dsg{i}", [128, 16, 128], BF16) for i in range(2)]; b_dsg = [Buf(), Buf()]
            scs = [sb(f"sc{i}", [128, S], F32) for i in range(2)]; b_scs = [Buf(), Buf()]
            NR = 4
            rl = [sb(f"rl{i}", [128, 512], BF16) for i in range(NR)]; b_rl = [Buf() for _ in range(NR)]
            junkb = sb("junkb", [128, S], BF16); b_junkb = Buf()
            msk = sb("msk", [128, S], BF16); b_msk = Buf()
            MTg = sb("MTg", [128, nb, 512], BF16); b_MTg = Buf()
            sm = sb("sm", [128, 64], F32); b_sm = Buf()
            psD = pst("psD", [128, 3, 512], F32); b_psD = [Buf() for _ in range(3)]
            psC = pst("psC", [128, 2, 512], F32); b_psC = [Buf(), Buf()]
            psT = pst("psT", [128, 2, 1024], BF16); b_psT = [Buf(), Buf()]
            WH = 16

            def ldq(i):
                tk = slice(i * 128, (i + 1) * 128)
                srcq = self.qidxT[:, :, tk].rearrange("h p t -> p h t")
                ph.dma(qz[0][i % 2][0:64, :, :], srcq[0:64], writes=[b_qi[i % 2]], sbuf=b_qi[i % 2])
                ph.dma(qz[1][i % 2][64:128, :, :], srcq[64:128], writes=[b_qi[i % 2]], sbuf=b_qi[i % 2])
                ph.load(wi[i % 2][:], self.widx[tk, :], b_wi[i % 2])

            cd = [0]
            cr = [0]
            cc = [0]
            ct = [0]

            def scores(i):
                p_ = i % 2
                bq, w_, bw = b_qi[p_], wi[p_], b_wi[p_]
                sc, b_sc = scs[p_], b_scs[p_]
                Si = (i + 1) * 128
                ph.op(ph.dve, lambda h: h.tensor_scalar(out=wab[p_][:], in0=w_[:], scalar1=-1.0, scalar2=None, op0=ALU.mult), reads=[bw], writes=[b_wab[p_]])
                ph.op(ph.dve, lambda h: h.tensor_tensor(out=wab[p_][:], in0=wab[p_][:], in1=w_[:], op=ALU.max), reads=[bw], writes=[b_wab[p_]])
                ph.op(ph.dve, lambda h: h.tensor_scalar(out=wsg[p_][:], in0=w_[:], scalar1=0.0, scalar2=2.0, op0=ALU.is_ge, op1=ALU.mult), reads=[bw], writes=[b_dsg[p_]])
                ph.op(ph.dve, lambda h: h.tensor_scalar(out=wsg[p_][:], in0=wsg[p_][:], scalar1=-1.0, scalar2=None, op0=ALU.add), writes=[b_dsg[p_]])
                for g in range(16):
                    ph.op(ph.dve, lambda h, g=g: h.tensor_scalar(out=dsg[p_][:, g, :], in0=ident[:], scalar1=wsg[p_][:, g:g + 1], scalar2=None, op0=ALU.mult), reads=[b_ident], writes=[b_dsg[p_]])
                def chunk(c):
                    n = min(512, Si - c * 512)
                    k0 = c * 512
                    cb = cc[0] % 2; cc[0] += 1
                    slots = {}

                    def dots(g):
                        db = cd[0] % 3; cd[0] += 1
                        slots[g] = db
                        ph.op(ph.pe, lambda h: h.matmul(psD[:, db, 0:n], lhsT=qz[g % 2][p_][:, g // 2, :], rhs=kx[:, k0:k0 + n], start=True, stop=True),
                              reads=[bq, b_kx], writes=[b_psD[db]], pe_acc=True)

                    dots(0)
                    for g in range(16):
                        if g + 1 < 16:
                            dots(g + 1)
                        db = slots[g]
                        r_ = cr[0] % NR; cr[0] += 1
                        ph.op(ph.act, lambda h, db=db, r_=r_, g=g: h.activation(out=rl[r_][:, 0:n], in_=psD[:, db, 0:n], func=AF.Relu, scale=wab[p_][:, g:g + 1]), reads=[b_psD[db], b_wab[p_]], writes=[b_rl[r_]])
                        ph.op(ph.pe, lambda h, r_=r_, g=g: h.matmul(psC[:, cb, 0:n], lhsT=dsg[p_][:, g, :], rhs=rl[r_][:, 0:n], start=(g == 0), stop=(g == 15)),
                              reads=[b_dsg[p_], b_rl[r_]], writes=[b_psC[cb]], pe_acc=True)
                    ph.op(ph.dve, lambda h: h.tensor_copy(out=sc[:, k0:k0 + n], in_=psC[:, cb, 0:n]), reads=[b_psC[cb]], writes=[b_sc])

                for c in range((Si + 511) // 512):
                    chunk(c)
                ph.op(ph.dve, lambda h: h.tensor_tensor(out=sc[:, i * 128:(i + 1) * 128], in0=sc[:, i * 128:(i + 1) * 128], in1=cneg[:], op=ALU.add), reads=[b_cneg], writes=[b_sc])

            def select(i):
                p_ = i % 2
                sc, b_sc = scs[p_], b_scs[p_]
                Si = (i + 1) * 128
                if i < 2:
                    ph.op(ph.dve, lambda h: h.memset(sm[:, 13:14], -1e29), writes=[b_sm])
                else:
                    ph.op(ph.dve, lambda h: h.max(out=sm[:, 0:8], in_=sc[:, 0:Si]), reads=[b_sc], writes=[b_sm])
                    ph.op(ph.dve, lambda h: h.tensor_reduce(out=sm[:, 8:9], in_=sc[:, 0:i * 128], op=ALU.min, axis=AX.X), reads=[b_sc], writes=[b_sm])
                    ph.op(ph.dve, lambda h: h.tensor_tensor(out=sm[:, 9:10], in0=sm[:, 0:1], in1=sm[:, 8:9], op=ALU.subtract), reads=[b_sm], writes=[b_sm])
                    ph.op(ph.dve, lambda h: h.tensor_scalar(out=sm[:, WH:WH + NBIS + 1], in0=pow2[:, 0:NBIS + 1], scalar1=sm[:, 9:10], scalar2=None, op0=ALU.mult), reads=[b_sm, b_pow2], writes=[b_sm])
                    ph.op(ph.dve, lambda h: h.tensor_tensor(out=sm[:, 10:11], in0=sm[:, 8:9], in1=sm[:, WH:WH + 1], op=ALU.add), reads=[b_sm], writes=[b_sm])
                    for k in range(NBIS):
                        ph.op(ph.dve, lambda h: h.tensor_scalar(out=junkb[:, 0:Si], in0=sc[:, 0:Si], scalar1=sm[:, 10:11], scalar2=0.0, op0=ALU.is_ge, op1=ALU.add, accum_out=sm[:, 11:12]),
                              reads=[b_sc, b_sm], writes=[b_junkb, b_sm])
                        ph.op(ph.dve, lambda h: h.tensor_scalar(out=sm[:, 12:13], in0=sm[:, 11:12], scalar1=float(TOPK), scalar2=0.5, op0=ALU.is_ge, op1=ALU.subtract), reads=[b_sm], writes=[b_sm])
                        ph.op(ph.dve, lambda h, k=k: h.scalar_tensor_tensor(out=sm[:, 10:11], in0=sm[:, WH + k:WH + k + 1], scalar=sm[:, 12:13], in1=sm[:, 10:11], op0=ALU.mult, op1=ALU.add), reads=[b_sm], writes=[b_sm])
                    ph.op(ph.dve, lambda h: h.tensor_tensor(out=sm[:, 13:14], in0=sm[:, 10:11], in1=sm[:, WH + NBIS:WH + NBIS + 1], op=ALU.subtract), reads=[b_sm], writes=[b_sm])
                ph.op(ph.dve, lambda h: h.tensor_scalar(out=msk[:, 0:Si], in0=sc[:, 0:Si], scalar1=sm[:, 13:14], scalar2=None, op0=ALU.is_ge), reads=[b_sc, b_sm], writes=[b_msk])

            def transposes(i):
                kq = i % 4
                for j0 in range(0, i + 1, 8):
                    nj = min(8, i + 1 - j0)
                    tb = ct[0] % 2; ct[0] += 1
                    for jj in range(nj):
                        j = j0 + jj
                        ph.op(ph.pe, lambda h, tb=tb, jj=jj, j=j: h.transpose(out=psT[:, tb, jj * 128:(jj + 1) * 128], in_=msk[:, j * 128:(j + 1) * 128], identity=ident[:]),
                              reads=[b_msk, b_ident], writes=[b_psT[tb]], pe_acc=True)
                    ph.op(ph.act, lambda h, tb=tb, nj=nj, j0=j0, kq=kq: h.copy(out=MTg[:, j0:j0 + nj, kq * 128:(kq + 1) * 128], in_=psT[:, tb, 0:nj * 128].rearrange("p (j q) -> p j q", q=128)),
                          reads=[b_psT[tb]], writes=[b_MTg])
                if kq == 3:
                    G = i // 4
                    ph.store(self.MTd[G, :, 0:4 * G + 4, :], MTg[:, 0:4 * G + 4, :], b_MTg)

            ldq(0)
            scores(0)
            for i in range(nb):
                if i + 1 < nb:
                    ldq(i + 1)
                    scores(i + 1)
                select(i)
                transposes(i)
            ph.emit(st)

    def memkv(self, l):
        nc = self.nc
        with ExitStack() as st:
            ph, sb, pst, cs = self.begin(f"mkv{l}", st)
            ident, b_ident = cs["ident"]
            vp = sb("vp", [128, NVP], F32); b_vp = Buf()
            ph.load(vp[:], self.E("vecP")[l], b_vp)
            wkv = sb("wkv", [128, NDC, 1024], BF16); b_wkv = Buf()
            stg = [sb(f"stg{i}", [128, 1024], F32) for i in range(2)]; b_stg = [Buf(), Buf()]
            for dc in range(NDC):
                s_ = dc % 2
                ph.load(stg[s_][:], self.E("x_wkv")[l, dc * 128:(dc + 1) * 128, :], b_stg[s_])
                ph.op(ph.dve, lambda h, s_=s_, dc=dc: h.tensor_scalar(out=wkv[:, dc, :], in0=stg[s_][:], scalar1=vp[:, VP_MEMG + dc:VP_MEMG + dc + 1], scalar2=None, op0=ALU.mult),
                      reads=[b_stg[s_], b_vp], writes=[b_wkv])
            hts = [sb(f"ht{i}", [128, D], F32) for i in range(2)]; b_ht = [Buf(), Buf()]
            junk = sb("junk", [128, D], BF16); b_junk = Buf()
            sm = sb("sm", [128, 4], F32); b_sm = Buf()
            xn = sb("xn", [128, D], BF16); b_xn = Buf()
            xnT = sb("xnT", [128, NDC, 128], BF16); b_xnT = Buf()
            kst = sb("kst", [128, 4, 128], BF16); b_kst = Buf()
            vst = sb("vst", [128, 512], BF16); b_vst = Buf()
            psB = pst("psB", [128, 2, 1024], BF16); b_psB = [Buf(), Buf()]
            psF = pst("psF", [128, 2, 512], F32); bF = [Buf(), Buf()]
            for i in range(2):
                ph.load(hts[i][:], self.E("mem")[i * 128:(i + 1) * 128, :], b_ht[i])
            for i in range(2):
                ht, bh = hts[i], b_ht[i]
                self.rms_tile(ph, ht[:], bh, junk[:], b_junk, sm[:, 0:1], sm[:, 1:2], sm[:, 2:3], b_sm, D)
                ph.op(ph.dve, lambda h, ht=ht: h.tensor_scalar(out=xn[:], in0=ht[:], scalar1=sm[:, 2:3], scalar2=None, op0=ALU.mult), reads=[bh, b_sm], writes=[b_xn])
                for dc in range(NDC):
                    ph.op(ph.pe, lambda h, dc=dc: h.transpose(out=psB[:, dc // 8, (dc % 8) * 128:(dc % 8 + 1) * 128], in_=xn[:, dc * 128:(dc + 1) * 128], identity=ident[:]),
                          reads=[b_xn, b_ident], writes=[b_psB[dc // 8]], pe_acc=True)
                for k in range(2):
                    ph.op(ph.act, lambda h, k=k: h.copy(out=xnT[:, k * 8:(k + 1) * 8, :], in_=psB[:, k, :]), reads=[b_psB[k]], writes=[b_xnT])
                for hh in range(4):
                    for dc in range(NDC):
                        ph.op(ph.pe, lambda h, hh=hh, dc=dc: h.matmul(psF[:, 0, hh * 128:(hh + 1) * 128], lhsT=wkv[:, dc, hh * 128:(hh + 1) * 128], rhs=xnT[:, dc, :], start=(dc == 0), stop=(dc == NDC - 1)),
                              reads=[b_wkv, b_xnT], writes=[bF[0]], pe_acc=True)
                for dc in range(NDC):
                    ph.op(ph.pe, lambda h, dc=dc: h.matmul(psF[:, 1, :], lhsT=xnT[:, dc, :], rhs=wkv[:, dc, 512:1024], start=(dc == 0), stop=(dc == NDC - 1)),
                          reads=[b_wkv, b_xnT], writes=[bF[1]], pe_acc=True)
                ph.op(ph.act, lambda h: h.copy(out=kst[:], in_=psF[:, 0, :]), reads=[bF[0]], writes=[b_kst])
                ph.op(ph.dve, lambda h: h.tensor_copy(out=vst[:], in_=psF[:, 1, :]), reads=[bF[1]], writes=[b_vst])
                ph.store(self.kmT[:, :, i * 128:(i + 1) * 128], kst[:], b_kst)
                ph.store(self.vm[i * 128:(i + 1) * 128, :], vst[:], b_vst)
            ph.emit(st)

    def p3(self, l):
        nc, S, nb = self.nc, self.S, self.nb
        lam_init = 0.8 - 0.6 * math.exp(-0.3 * l)
        xscale = 128 ** -0.5
        hsrc = self.E("x") if l == 0 else self.h
        with ExitStack() as st:
            ph, sb, pst, cs = self.begin(f"p3{l}", st)
            ident, b_ident = cs["ident"]
            vp = sb("vp", [128, NVP], F32); b_vp = Buf()
            ph.load(vp[:], self.E("vecP")[l], b_vp)
            sg = sb("sg", [128, 1], F32); b_sg = Buf()
            ph.op(ph.dve, lambda h: h.tensor_scalar(out=sg[:], in0=vp[:, VP_SUBG:VP_SUBG + 1], scalar1=1.0 - lam_init, scalar2=None, op0=ALU.mult), reads=[b_vp], writes=[b_sg])
            pg = sb("pg", [128, 2, D], F32); b_pg = Buf()
            ph.load(pg[:, 0, :], self.E("vecB")[l, :, VB_MIXPOST:VB_MIXPOST + D], b_pg)
            ph.load(pg[:, 1, :], self.E("vecB")[l, :, VB_XPOST:VB_XPOST + D], b_pg)
            wout = sb("wout", [128, NDC, D], BF16); b_wout = Buf()
            xwq = sb("xwq", [128, NDC, 512], BF16); b_xwq = Buf()
            xwo = sb("xwo", [128, 4, D], BF16); b_xwo = Buf()
            kmT = sb("kmT", [128, 4, 256], BF16); b_kmT = Buf()
            ph.load(kmT[:], self.kmT, b_kmT)
            vm1 = sb("vm1", [128, 2, 4, 129], BF16); b_vm1 = Buf()
            ph.op(ph.pool, lambda h: h.memset(vm1[:, :, :, 128:129], 1.0), writes=[b_vm1])
            for mc in range(2):
                ph.dma(vm1[:, mc, :, 0:128], self.vm[mc * 128:(mc + 1) * 128, :].rearrange("p (h d) -> p h d", d=128), writes=[b_vm1], sbuf=b_vm1)
            stg = [sb(f"stg{i}", [128, D], F32) for i in range(2)]; b_stg = [Buf(), Buf()]
            cnt = [0]

            def wload(dst, b_dst, srcap, n, scal):
                s_ = cnt[0] % 2; cnt[0] += 1
                ph.load(stg[s_][:, 0:n], srcap, b_stg[s_])
                if scal is None:
                    ph.op(ph.dve, lambda h: h.tensor_copy(out=dst, in_=stg[s_][:, 0:n]), reads=[b_stg[s_]], writes=[b_dst])
                else:
                    ph.op(ph.dve, lambda h: h.tensor_scalar(out=dst, in0=stg[s_][:, 0:n], scalar1=scal, scalar2=None, op0=ALU.mult), reads=[b_stg[s_], b_vp, b_sg], writes=[b_dst])

            for c in range(NDC):
                wload(wout[:, c, :], b_wout, self.E("w_out")[l, c * 128:(c + 1) * 128, :], D, sg[:, 0:1] if c < 6 else None)
            for dc in range(NDC):
                wload(xwq[:, dc, :], b_xwq, self.E("x_wq")[l, dc * 128:(dc + 1) * 128, :], 512, vp[:, VP_XPRE + dc:VP_XPRE + dc + 1])
            for hh in range(4):
                wload(xwo[:, hh, :], b_xwo, self.E("x_wo")[l, hh * 128:(hh + 1) * 128, :], D, None)
            hts = [sb(f"ht{i}", [128, D], F32) for i in range(2)]; b_ht = [Buf(), Buf()]
            mxs = [sb(f"mx{i}", [128, NDC, 128], BF16) for i in range(2)]; b_mx = [Buf(), Buf()]
            junk = sb("junk", [128, D], BF16); b_junk = Buf()
            tmp = sb("tmp", [128, D], F32); b_tmp = Buf()
            sm = sb("sm", [128, 24], F32); b_sm = Buf()
            xn = sb("xn", [128, D], BF16); b_xn = Buf()
            xnT = sb("xnT", [128, NDC, 128], BF16); b_xnT = Buf()
            qx = sb("qx", [128, 4, 128], BF16); b_qx = Buf()
            pl = sb("pl", [128, 8, 128], BF16); b_pl = Buf()
            on = sb("on", [128, 4, 128], BF16); b_on = Buf()
            oT = sb("oT", [128, 4, 128], BF16); b_oT = Buf()
            psY = pst("psY", [128, 4, 512], F32); b_psY = Buf()
            psB = pst("psB", [128, 2, 1024], BF16); b_psB = [Buf(), Buf()]
            psX = pst("psX", [128, 2, 512], F32); b_psX = Buf()

            def ld(i):
                tk = slice(i * 128, (i + 1) * 128)
                ph.load(hts[i % 2][:], hsrc[tk, :], b_ht[i % 2])
                ph.load(mxs[i % 2][:], self.mixT[:, :, tk].rearrange("c p t -> p c t"), b_mx[i % 2])

            def post(which, ht, bh):
                ph.op(ph.act, lambda h: h.activation(out=junk[:], in_=psY[:].rearrange("p a b -> p (a b)"), func=AF.Square, accum_out=sm[:, 0:1]), reads=[b_psY], writes=[b_junk, b_sm])
                ph.op(ph.act, lambda h: h.activation(out=sm[:, 1:2], in_=sm[:, 0:1], func=AF.Sqrt, scale=1.0 / D, bias=self.eps_ap), reads=[b_sm, self.b_eps], writes=[b_sm])
                ph.op(ph.dve, lambda h: h.reciprocal(out=sm[:, 2:3], in_=sm[:, 1:2]), reads=[b_sm], writes=[b_sm])
                ph.op(ph.dve, lambda h: h.scalar_tensor_tensor(out=tmp[:], in0=psY[:].rearrange("p a b -> p (a b)"), scalar=sm[:, 2:3], in1=pg[:, which, :], op0=ALU.mult, op1=ALU.mult), reads=[b_psY, b_sm, b_pg], writes=[b_tmp])
                ph.op(ph.dve, lambda h: h.tensor_tensor(out=ht[:], in0=tmp[:], in1=ht[:], op=ALU.add), reads=[b_tmp], writes=[bh])

            ld(0)
            for i in range(nb):
                if i + 1 < nb:
                    ld(i + 1)
                ht, bh, mx, bm = hts[i % 2], b_ht[i % 2], mxs[i % 2], b_mx[i % 2]
                for n in range(4):
                    for c in range(NDC):
                        ph.op(ph.pe, lambda h, n=n, c=c, mx=mx: h.matmul(psY[:, n, :], lhsT=mx[:, c, :], rhs=wout[:, c, n * 512:(n + 1) * 512], start=(c == 0), stop=(c == NDC - 1)),
                              reads=[bm, b_wout], writes=[b_psY], pe_acc=True)
                post(0, ht, bh)
                self.rms_tile(ph, ht[:], bh, junk[:], b_junk, sm[:, 3:4], sm[:, 4:5], sm[:, 5:6], b_sm, D)
                ph.op(ph.dve, lambda h, ht=ht: h.tensor_scalar(out=xn[:], in0=ht[:], scalar1=sm[:, 5:6], scalar2=None, op0=ALU.mult), reads=[bh, b_sm], writes=[b_xn])
                for dc in range(NDC):
                    ph.op(ph.pe, lambda h, dc=dc: h.transpose(out=psB[:, dc // 8, (dc % 8) * 128:(dc % 8 + 1) * 128], in_=xn[:, dc * 128:(dc + 1) * 128], identity=ident[:]),
                          reads=[b_xn, b_ident], writes=[b_psB[dc // 8]], pe_acc=True)
                for k in range(2):
                    ph.op(ph.act, lambda h, k=k: h.copy(out=xnT[:, k * 8:(k + 1) * 8, :], in_=psB[:, k, :]), reads=[b_psB[k]], writes=[b_xnT])
                for hh in range(4):
                    for dc in range(NDC):
                        ph.op(ph.pe, lambda h, hh=hh, dc=dc: h.matmul(psX[:, 0, hh * 128:(hh + 1) * 128], lhsT=xwq[:, dc, hh * 128:(hh + 1) * 128], rhs=xnT[:, dc, :], start=(dc == 0), stop=(dc == NDC - 1)),
                              reads=[b_xwq, b_xnT], writes=[b_psX], pe_acc=True)
                ph.op(ph.act, lambda h: h.copy(out=qx[:], in_=psX[:, 0, :]), reads=[b_psX], writes=[b_qx])
                for hh in range(4):
                    for mc in range(2):
                        u = hh * 2 + mc
                        ph.op(ph.pe, lambda h, hh=hh, mc=mc, u=u: h.matmul(psX[:, u // 4, (u % 4) * 128:(u % 4 + 1) * 128], lhsT=kmT[:, hh, mc * 128:(mc + 1) * 128], rhs=qx[:, hh, :], start=True, stop=True),
                              reads=[b_kmT, b_qx], writes=[b_psX], pe_acc=True)
                ph.op(ph.act, lambda h: h.activation(out=pl[:], in_=psX[:].rearrange("p a b -> p (a b)"), func=AF.Exp, scale=xscale), reads=[b_psX], writes=[b_pl])
                for hh in range(4):
                    for mc in range(2):
                        ph.op(ph.pe, lambda h, hh=hh, mc=mc: h.matmul(psX[:, hh // 2, (hh % 2) * 129:(hh % 2) * 129 + 129], lhsT=pl[:, hh * 2 + mc, :], rhs=vm1[:, mc, hh, :], start=(hh % 2 == 0 and mc == 0), stop=(mc == 1), skip_group_check=True),
                              reads=[b_pl, b_vm1], writes=[b_psX], pe_acc=True)
                for hh in range(4):
                    o = (hh % 2) * 129
                    ph.op(ph.dve, lambda h, hh=hh, o=o: h.reciprocal(out=sm[:, 8 + hh:9 + hh], in_=psX[:, hh // 2, o + 128:o + 129]), reads=[b_psX], writes=[b_sm])
                    ph.op(ph.dve, lambda h, hh=hh, o=o: h.tensor_scalar(out=on[:, hh, :], in0=psX[:, hh // 2, o:o + 128], scalar1=sm[:, 8 + hh:9 + hh], scalar2=None, op0=ALU.mult), reads=[b_psX, b_sm], writes=[b_on])
                for hh in range(4):
                    ph.op(ph.pe, lambda h, hh=hh: h.transpose(out=psB[:, 0, hh * 128:(hh + 1) * 128], in_=on[:, hh, :], identity=ident[:]), reads=[b_on, b_ident], writes=[b_psB[0]], pe_acc=True)
                ph.op(ph.act, lambda h: h.copy(out=oT[:], in_=psB[:, 0, 0:512]), reads=[b_psB[0]], writes=[b_oT])
                for n in range(4):
                    for hh in range(4):
                        ph.op(ph.pe, lambda h, n=n, hh=hh: h.matmul(psY[:, n, :], lhsT=oT[:, hh, :], rhs=xwo[:, hh, n * 512:(n + 1) * 512], start=(hh == 0), stop=(hh == 3)),
                              reads=[b_oT, b_xwo], writes=[b_psY], pe_acc=True)
                post(1, ht, bh)
                ph.store(self.h[i * 128:(i + 1) * 128, :], ht[:], bh)
            ph.emit(st)


    def ffn(self, l):
        nc, S = self.nc, self.S
        TG = min(1024, S)
        ngr = S // TG
        nt = TG // 128
        nh = TG // 512
        with ExitStack() as st:
            ph, sb, pst, cs = self.begin(f"ffn{l}", st)
            ident, b_ident = cs["ident"]
            gB = sb("gB", [128, D], F32); b_gB = Buf()
            ph.load(gB[:], self.E("vecB")[l, :, VB_FPRE:VB_FPRE + D], b_gB)
            xnT = sb("xnT", [128, NDC, TG], BF16); b_xnT = Buf()
            actT = sb("actT", [128, NFC, TG], BF16); b_actT = Buf()
            gus = [sb(f"gus{i}", [128, 2, NDC, 128], F32) for i in range(2)]; b_gus = [Buf(), Buf()]
            gub = [sb(f"gub{i}", [128, 2, NDC, 128], BF16) for i in range(2)]; b_gub = [Buf(), Buf()]
            NWD = 6
            wds = [sb(f"wds{i}", [128, 256], F32) for i in range(NWD)]; b_wds = [Buf() for _ in range(NWD)]
            wdb = [sb(f"wdb{i}", [128, 256], BF16) for i in range(NWD)]; b_wdb = [Buf() for _ in range(NWD)]
            ht = sb("ht", [128, D], F32); b_ht = Buf()
            xn = sb("xn", [128, D], BF16); b_xn = Buf()
            sgt = sb("sgt", [128, 512], F32); b_sgt = Buf()
            yst = sb("yst", [128, nt, 256], F32); b_yst = Buf()
            sm = sb("sm", [128, 4], F32); b_sm = Buf()
            psB = pst("psB", [128, 1024], BF16); b_psB = Buf()
            psGU = pst("psGU", [128, 3, 512], F32); b_psGU = [Buf() for _ in range(3)]
            psD = pst("psD", [128, 4, 512], F32); b_psD = [Buf() for _ in range(4)]
            hsrc = self.h
            cgu = [0]
            cwd = [0]
            cbank = [0]
            for gr in range(ngr):
                for t in range(nt):
                    r0 = gr * TG + t * 128
                    ph.load(ht[:], hsrc[r0:r0 + 128, :], b_ht)
                    self.rms_tile(ph, ht[:], b_ht, xn[:], b_xn, sm[:, 0:1], sm[:, 1:2], sm[:, 2:3], b_sm, D)
                    ph.op(ph.dve, lambda h: h.scalar_tensor_tensor(out=xn[:], in0=ht[:], scalar=sm[:, 2:3], in1=gB[:], op0=ALU.mult, op1=ALU.mult), reads=[b_ht, b_sm, b_gB], writes=[b_xn])
                    for k in range(2):
                        for dd in range(8):
                            dc = k * 8 + dd
                            ph.op(ph.pe, lambda h, dd=dd, dc=dc: h.transpose(out=psB[:, dd * 128:(dd + 1) * 128], in_=xn[:, dc * 128:(dc + 1) * 128], identity=ident[:]),
                                  reads=[b_xn, b_ident], writes=[b_psB], pe_acc=True)
                        ph.op(ph.act, lambda h, k=k, t=t: h.copy(out=xnT[:, k * 8:(k + 1) * 8, t * 128:(t + 1) * 128], in_=psB[:].rearrange("p (a b) -> p a b", b=128)), reads=[b_psB], writes=[b_xnT])
                for fc in range(NFC):
                    s_ = cgu[0] % 2; cgu[0] += 1
                    for c in range(2):
                        ph.dma(gus[s_][:, c, :, :], self.E("f_w_gu")[l, :, c, fc * 128:(fc + 1) * 128].rearrange("(dc p) f -> p dc f", p=128), writes=[b_gus[s_]], sbuf=b_gus[s_])
                    ph.op(ph.pool, lambda h, s_=s_: h.tensor_copy(out=gub[s_][:], in_=gus[s_][:]), reads=[b_gus[s_]], writes=[b_gub[s_]])
                    for hf in range(nh):
                        bks = []
                        for c in range(2):
                            bk = cbank[0] % 3; cbank[0] += 1
                            bks.append(bk)
                            for dc in range(NDC):
                                ph.op(ph.pe, lambda h, bk=bk, c=c, dc=dc, s_=s_, hf=hf: h.matmul(psGU[:, bk, :], lhsT=gub[s_][:, c, dc, :], rhs=xnT[:, dc, hf * 512:(hf + 1) * 512], start=(dc == 0), stop=(dc == NDC - 1)),
                                      reads=[b_gub[s_], b_xnT], writes=[b_psGU[bk]], pe_acc=True)
                        ph.op(ph.act, lambda h, bk=bks[0]: h.activation(out=sgt[:], in_=psGU[:, bk, :], func=AF.Silu), reads=[b_psGU[bks[0]]], writes=[b_sgt])
                        ph.op(ph.dve, lambda h, bk=bks[1], fc=fc, hf=hf: h.tensor_tensor(out=actT[:, fc, hf * 512:(hf + 1) * 512], in0=sgt[:], in1=psGU[:, bk, :], op=ALU.mult), reads=[b_sgt, b_psGU[bks[1]]], writes=[b_actT])
                for dcol in range(8):
                    for fc in range(NFC):
                        w_ = cwd[0] % NWD; cwd[0] += 1
                        ph.load(wds[w_][:], self.E("f_w_down")[l, fc * 128:(fc + 1) * 128, dcol * 256:(dcol + 1) * 256], b_wds[w_])
                        ph.op(ph.pool, lambda h, w_=w_: h.tensor_copy(out=wdb[w_][:], in_=wds[w_][:]), reads=[b_wds[w_]], writes=[b_wdb[w_]])
                        for t in range(nt):
                            ph.op(ph.pe, lambda h, t=t, fc=fc, w_=w_: h.matmul(psD[:, t // 2, (t % 2) * 256:(t % 2) * 256 + 256], lhsT=actT[:, fc, t * 128:(t + 1) * 128], rhs=wdb[w_][:], start=(fc == 0 and t % 2 == 0), stop=(fc == NFC - 1), skip_group_check=True),
                                  reads=[b_actT, b_wdb[w_]], writes=[b_psD[t // 2]], pe_acc=True)
                    for b in range(nt // 2):
                        if b % 2 == 0:
                            ph.op(ph.dve, lambda h, b=b: h.tensor_copy(out=yst[:, 2 * b:2 * b + 2, :], in_=psD[:, b, :].rearrange("p (a c) -> p a c", c=256)), reads=[b_psD[b]], writes=[b_yst])
                        else:
                            ph.op(ph.act, lambda h, b=b: h.copy(out=yst[:, 2 * b:2 * b + 2, :], in_=psD[:, b, :].rearrange("p (a c) -> p a c", c=256)), reads=[b_psD[b]], writes=[b_yst])
                    ph.store(self.yscr[gr * TG:(gr + 1) * TG, dcol * 256:(dcol + 1) * 256].rearrange("(t p) c -> p t c", p=128), yst[:], b_yst)
            ph.emit(st)

    def ffn_post(self, l, dst):
        nc, S, nb = self.nc, self.S, self.nb
        with ExitStack() as st:
            ph, sb, pst, cs = self.begin(f"fpo{l}", st, want=())
            pg = sb("pg", [128, D], F32); b_pg = Buf()
            ph.load(pg[:], self.E("vecB")[l, :, VB_FPOST:VB_FPOST + D], b_pg)
            hts = [sb(f"ht{i}", [128, D], F32) for i in range(2)]; b_ht = [Buf(), Buf()]
            yts = [sb(f"yt{i}", [128, D], F32) for i in range(2)]; b_yt = [Buf(), Buf()]
            junk = sb("junk", [128, D], BF16); b_junk = Buf()
            sm = sb("sm", [128, 4], F32); b_sm = Buf()

            def ld(i):
                tk = slice(i * 128, (i + 1) * 128)
                ph.load(hts[i % 2][:], self.h[tk, :], b_ht[i % 2])
                ph.load(yts[i % 2][:], self.yscr[tk, :], b_yt[i % 2])

            ld(0)
            for i in range(nb):
                if i + 1 < nb:
                    ld(i + 1)
                ht, bh, yt, by = hts[i % 2], b_ht[i % 2], yts[i % 2], b_yt[i % 2]
                self.rms_tile(ph, yt[:], by, junk[:], b_junk, sm[:, 0:1], sm[:, 1:2], sm[:, 2:3], b_sm, D)
                ph.op(ph.dve, lambda h, yt=yt: h.scalar_tensor_tensor(out=yt[:], in0=yt[:], scalar=sm[:, 2:3], in1=pg[:], op0=ALU.mult, op1=ALU.mult), reads=[b_sm, b_pg], writes=[by])
                ph.op(ph.pool, lambda h, yt=yt, ht=ht: h.tensor_tensor(out=ht[:], in0=yt[:], in1=ht[:], op=ALU.add), reads=[by], writes=[bh])
                ph.store(dst[i * 128:(i + 1) * 128, :], ht[:], bh)
            ph.emit(st)


def rel_bucket_np(dist):
    n = np.maximum(dist, 0)
    nf = np.maximum(n, 1).astype(np.float32)
    large = 16 + (np.log(nf / np.float32(16)) / np.float32(math.log(128 / 16)) * np.float32(16)).astype(np.int32)
    large = np.minimum(large, 31)
    return np.where(n < 16, n, large)


def host_consts():
    c = np.zeros((128, 128 * 4 + 32), np.float32)
    c[:, 0:128] = np.eye(128, dtype=np.float32)
    s = np.arange(128)[:, None]
    t = np.arange(128)[None, :]
    c[:, 128:256] = (s <= t).astype(np.float32)
    c[:, 256:384] = np.where(t <= s, 0.0, -1e30)
    c[:, 384:512] = np.where(s <= t, 0.0, NEG)
    c[:, 512:544] = (0.5 ** (np.arange(32) + 1))[None, :]
    return c


def prep_inputs(inp, S, L):
    f = lambda a: np.ascontiguousarray(np.asarray(a, dtype=np.float32)[:L])
    shared = {}
    shared["w_in"] = f(inp["w_in"])
    shared["w_out"] = f(inp["w_out"])
    shared["b_w_uq"] = f(inp["b_w_uq"]).reshape(L, 512, 768)
    shared["b_w_qidx"] = f(inp["b_w_qidx"]).reshape(L, 512, 1024)
    shared["b_w_uk"] = f(inp["b_w_uk"]).reshape(L, 256, 768)
    shared["b_w_uv"] = f(inp["b_w_uv"]).reshape(L, 256, 768)
    shared["cwsT"] = f(np.transpose(np.asarray(inp["c_w_s"], dtype=np.float32)[:L], (0, 3, 1, 2))).reshape(L, 128, 512)
    shared["x_wq"] = f(inp["x_wq"]).reshape(L, D, 512)
    shared["x_wkv"] = f(inp["x_wkv"]).reshape(L, D, 1024)
    shared["x_wo"] = f(inp["x_wo"]).reshape(L, 512, D)
    shared["f_w_gu"] = f(inp["f_w_gu"])
    shared["f_w_down"] = f(inp["f_w_down"])
    vp = np.zeros((L, 128, NVP), np.float32)
    vb = np.zeros((L, 128, NVB), np.float32)
    pp = lambda v, n: np.asarray(v, np.float32).reshape(n, 128).T
    for l in range(L):
        vp[l, :, VP_GIN:VP_GIN + 16] = pp(inp["mix_pre_g"][l], 16)
        vp[l, :, VP_CQ:VP_CQ + 4] = pp(inp["b_cq_g"][l], 4)
        vp[l, :, VP_CKV:VP_CKV + 2] = pp(inp["b_ckv_g"][l], 2)
        vp[l, :, VP_XPRE:VP_XPRE + 16] = pp(inp["x_pre_g"][l], 16)
        vp[l, :, VP_MEMG:VP_MEMG + 16] = pp(inp["mem_g"][l], 16)
        vp[l, :, VP_FPRE:VP_FPRE + 16] = pp(inp["f_pre_g"][l], 16)
        vp[l, :, VP_SUBG] = np.asarray(inp["a_sub_g"][l], np.float32)
        vb[l, :, VB_MIXPOST:VB_MIXPOST + D] = np.asarray(inp["mix_post_g"][l])[None, :]
        vb[l, :, VB_XPOST:VB_XPOST + D] = np.asarray(inp["x_post_g"][l])[None, :]
        vb[l, :, VB_FPOST:VB_FPOST + D] = np.asarray(inp["f_post_g"][l])[None, :]
        vb[l, :, VB_LNG:VB_LNG + 512] = np.asarray(inp["c_ln_g"][l])[None, :]
        vb[l, :, VB_LNB:VB_LNB + 512] = np.asarray(inp["c_ln_b"][l])[None, :]
        vb[l, :, VB_BS:VB_BS + 512] = np.asarray(inp["c_b_s"][l]).reshape(1, 512)
        vb[l, :, VB_LAM:VB_LAM + 256] = np.asarray(inp["a_lambda"][l]).reshape(1, 256)
        vb[l, :, VB_FPRE:VB_FPRE + D] = np.asarray(inp["f_pre_g"][l])[None, :]
    shared["vecP"] = vp
    shared["vecB"] = vb
    rb = np.asarray(inp["rel_bias"], np.float32)
    s = np.arange(128)[:, None]
    q = np.arange(128)[None, :]
    bt = np.zeros((128, 12, 2, 128), np.float32)
    for kind in range(2):
        idx = rel_bucket_np(128 * kind + q - s)
        bt[:, :, kind, :] = np.transpose(rb[idx], (0, 2, 1))
    shared["biasT"] = bt.reshape(128, 12 * 2 * 128)
    shared["b31"] = np.ascontiguousarray(np.broadcast_to(rb[31][None, :], (128, 12)))
    shared["consts"] = host_consts()
    return shared


_CACHE = {}


def kernel(**inputs):
    x = np.asarray(inputs["x"], np.float32)
    B, S, _ = x.shape
    L = int(np.asarray(inputs["w_in"]).shape[0])
    key = (S, L)
    if key not in _CACHE:
        _CACHE[key] = Kern(S, L).build()
    nc = _CACHE[key]
    shared = prep_inputs(inputs, S, L)
    mem = np.asarray(inputs["mem"], np.float32)
    ncore = B
    in_maps = []
    for c in range(ncore):
        m = dict(shared)
        m["x"] = np.ascontiguousarray(x[c])
        m["mem"] = np.ascontiguousarray(mem[c])
        in_maps.append(m)
    res = run_bass_kernel_spmd(nc, in_maps, core_ids=list(range(ncore)))
    out = np.stack([np.asarray(res.results[b]["out"], np.float32) for b in range(B)], axis=0)
    return out
```

```python
import math
from contextlib import ExitStack
import numpy as np
import concourse.bass as bass
import concourse.mybir as mybir
from concourse.bass_utils import run_bass_kernel_spmd

F32 = mybir.dt.float32
BF16 = mybir.dt.bfloat16
AF = mybir.ActivationFunctionType
ALU = mybir.AluOpType
AX = mybir.AxisListType

D = 2048
NDC = 16
IN_COLS = 4176
FFN = 5632
NFC = 44
EPS = 1e-6
TOPK = 256
NBIS = 14
NEG = -30000.0

VP_GIN, VP_CQ, VP_CKV, VP_XPRE, VP_MEMG, VP_FPRE, VP_SUBG = 0, 16, 20, 22, 38, 54, 70
NVP = 71
VB_MIXPOST, VB_XPOST, VB_FPOST, VB_LNG, VB_LNB, VB_BS, VB_LAM = 0, 2048, 4096, 6144, 6656, 7168, 7680
VB_FPRE = 7936
NVB = 9984


class Buf:
    __slots__ = ("w", "r", "dsem")

    def __init__(self):
        self.w = None
        self.r = {}
        self.dsem = None


class _Sem:
    def __init__(self):
        self.sem = None
        self.n = 0


class Eng:
    def __init__(self, name):
        self.name = name
        self.sem = None
        self.n = 0
        self.prog = []
        self.waited = {}

    def wait(self, tok):
        src, val = tok
        if self.waited.get(id(src), 0) >= val:
            return
        self.waited[id(src)] = val
        self.prog.append(lambda h, src=src, val=val: h.wait_ge(src.sem, val))


class Phase:
    def __init__(self, kern, name):
        self.kern = kern
        self.nc = kern.nc
        self.name = name
        self.pe, self.dve, self.act, self.pool, self.sp = kern.engs
        self.engs = kern.engs
        for e in self.engs:
            e.prog = []
        self.dsems = []
        self.npool = 0

    @staticmethod
    def _deps(reads, writes):
        deps = []
        for b in reads:
            if b.w is not None:
                deps.append(b.w)
        for b in writes:
            if b.w is not None:
                deps.append(b.w)
            deps.extend(b.r.values())
        return deps

    @staticmethod
    def _commit(tok, reads, writes):
        k = id(tok[0])
        for b in reads:
            b.r[k] = tok
        for b in writes:
            b.w = tok
            b.r = {}

    def op(self, eng, fn, reads=(), writes=(), pe_acc=False):
        for d in self._deps(reads, writes):
            if pe_acc and d[0] is eng:
                continue
            eng.wait(d)
        eng.n += 1
        tok = (eng, eng.n)
        eng.prog.append(lambda h, fn=fn, eng=eng: fn(h).then_inc(eng.sem, 1))
        self._commit(tok, reads, writes)
        return tok

    def dma(self, out, in_, reads=(), writes=(), sbuf=None, **kw):
        eng = self.sp
        for d in self._deps(reads, writes):
            eng.wait(d)
        if sbuf.dsem is None:
            sbuf.dsem = self.kern.get_dsem(self.npool)
            self.npool += 1
            self.dsems.append(sbuf.dsem)
        ds = sbuf.dsem
        ds.n += 16
        tok = (ds, ds.n)
        eng.prog.append(lambda h, out=out, in_=in_, kw=kw, ds=ds: h.dma_start(out=out, in_=in_, **kw).then_inc(ds.sem, 16))
        self._commit(tok, reads, writes)
        return tok

    def load(self, out, in_, buf, **kw):
        return self.dma(out, in_, writes=[buf], sbuf=buf, **kw)

    def store(self, out, in_, buf, **kw):
        return self.dma(out, in_, reads=[buf], sbuf=buf, **kw)

    def emit(self, st):
        nc = self.nc
        for ds in self.dsems:
            self.sp.wait((ds, ds.n))
        block = st.enter_context(nc.Block(self.name))
        for e in self.engs:
            def body(h, e=e):
                for c in e.prog:
                    c(h)
            getattr(block, e.name)(body)


class Kern:
    def __init__(self, S, L, debug=False):
        self.S, self.L, self.debug = S, L, debug
        self.nb = S // 128
        self.ng = S // 512
        nc = self.nc = bass.Bass("TRN2", target_bir_lowering=False)
        self.gst = ExitStack()
        self.engs = [Eng(n) for n in ("tensor", "vector", "scalar", "gpsimd", "sync")]
        for e in self.engs:
            e.sem = self.gst.enter_context(nc.semaphore(f"prog_{e.name}"))
        self.dpool = []

        self._ext_shapes = {
            "x": [S, D], "mem": [256, D], "w_in": [L, D, IN_COLS], "w_out": [L, D, D],
            "b_w_uq": [L, 512, 768], "b_w_qidx": [L, 512, 1024], "b_w_uk": [L, 256, 768], "b_w_uv": [L, 256, 768],
            "cwsT": [L, 128, 512], "x_wq": [L, D, 512], "x_wkv": [L, D, 1024], "x_wo": [L, 512, D],
            "f_w_gu": [L, D, 2, FFN], "f_w_down": [L, FFN, D], "vecP": [L, 128, NVP], "vecB": [L, 128, NVB],
            "biasT": [128, 12 * 2 * 128], "b31": [128, 12], "consts": [128, 128 * 4 + 32],
        }
        self._ext = {}
        self.out = nc.dram_tensor("out", [S, D], F32, kind="ExternalOutput").ap()

        def scr(name, shape, dt):
            kind = "ExternalOutput" if debug else "Internal"
            return nc.dram_tensor(name, list(shape), dt, kind=kind).ap()

        self.h = scr("h_scr", [S, D], F32)
        self.qaT = scr("qaT", [6, 128, S], BF16)
        self.kaT = scr("kaT", [6, 128, S], BF16)
        self.va = scr("va", [S, 768], BF16)
        self.qbT = scr("qbT", [6, 128, S], BF16)
        self.kbT = scr("kbT", [6, 128, S], BF16)
        self.vb = scr("vb", [S, 768], BF16)
        self.qidxT = scr("qidxT", [8, 128, S], BF16)
        self.kidxT = scr("kidxT", [64, S], BF16)
        self.widx = scr("widx", [S, 16], F32)
        self.mixT = scr("mixT", [16, 128, S], BF16)
        self.MTd = scr("MTd", [self.ng, 128, self.nb, 512], BF16)
        self.yscr = scr("yscr", [S, D], F32)
        self.kmT = scr("kmT", [128, 4, 256], BF16)
        self.vm = scr("vm", [256, 512], BF16)

    def get_dsem(self, i):
        while len(self.dpool) <= i:
            d = _Sem()
            d.sem = self.gst.enter_context(self.nc.semaphore(f"dma_{len(self.dpool)}"))
            self.dpool.append(d)
        return self.dpool[i]

    def E(self, name):
        if name not in self._ext:
            self._ext[name] = self.nc.dram_tensor(name, list(self._ext_shapes[name]), F32, kind="ExternalInput").ap()
        return self._ext[name]

    def build(self, phases=None):
        want = lambda p: phases is None or p in phases
        for l in range(self.L):
            src = self.E("x") if l == 0 else self.h
            if want("p1a"):
                self.p1a(l, src)
            if want("p1b"):
                self.p1b(l, src)
            if want("attA"):
                self.attn(l, "A")
            if want("bidx"):
                self.bindex(l)
            if want("attB"):
                self.attn(l, "B")
            if want("p3"):
                self.memkv(l)
                self.p3(l)
            if want("ffn"):
                self.ffn(l)
                self.ffn_post(l, self.out if l == self.L - 1 else self.h)
        self.gst.close()
        return self.nc

    def begin(self, name, st, want=("ident",)):
        nc = self.nc
        ph = Phase(self, name)
        sb = lambda nm, shape, dt: st.enter_context(nc.sbuf_tensor(f"{name}_{nm}", list(shape), dt))
        pst = lambda nm, shape, dt: st.enter_context(nc.psum_tensor(f"{name}_{nm}", list(shape), dt))
        cs = self.consts_load(ph, sb, want)
        epsT = sb("epsT", [128, 1], F32); b_eps = Buf()
        ph.op(ph.dve, lambda h: h.memset(epsT[:], EPS), writes=[b_eps])
        self.eps_ap = epsT[:, 0:1]
        self.b_eps = b_eps
        return ph, sb, pst, cs

    def consts_load(self, ph, sb, want):
        res = {}
        if "ident" in want:
            cf = sb("c_idf", [128, 128], F32); bcf = Buf()
            ph.load(cf[:], self.E("consts")[:, 0:128], bcf)
            ib = sb("c_idb", [128, 128], BF16); bib = Buf()
            ph.op(ph.dve, lambda h: h.tensor_copy(out=ib[:], in_=cf[:]), reads=[bcf], writes=[bib])
            res["ident"] = (ib, bib)
        for nm, c0, n in (("mask01T", 128, 128), ("causneg", 256, 128), ("causnegT", 384, 128), ("pow2", 512, 32)):
            if nm in want:
                t = sb("c_" + nm, [128, n], F32); bt = Buf()
                ph.load(t[:], self.E("consts")[:, c0:c0 + n], bt)
                res[nm] = (t, bt)
        return res

    def rms_tile(self, ph, ht, b_ht, junk, b_junk, ss, rt, rinv, b_small, width, act_in=None):
        src = ht if act_in is None else act_in
        ph.op(ph.act, lambda h: h.activation(out=junk, in_=src, func=AF.Square, accum_out=ss), reads=[b_ht], writes=[b_junk, b_small])
        ph.op(ph.act, lambda h: h.activation(out=rt, in_=ss, func=AF.Sqrt, scale=1.0 / width, bias=self.eps_ap), reads=[b_small, self.b_eps], writes=[b_small])
        ph.op(ph.dve, lambda h: h.reciprocal(out=rinv, in_=rt), reads=[b_small], writes=[b_small])

    def p1a(self, l, src):
        nc, S, nb = self.nc, self.S, self.nb
        with ExitStack() as st:
            ph, sb, pst, cs = self.begin(f"p1a{l}", st)
            ident, b_ident = cs["ident"]
            vp = sb("vp", [128, NVP], F32); b_vp = Buf()
            ph.load(vp[:], self.E("vecP")[l], b_vp)
            NW = 2304
            win = sb("win", [128, NDC, NW], BF16); b_win = Buf()
            stg = [sb(f"stg{i}", [128, NW], F32) for i in range(2)]; b_stg = [Buf(), Buf()]
            for dc in range(NDC):
                s = dc % 2
                ph.load(stg[s][:], self.E("w_in")[l, dc * 128:(dc + 1) * 128, 0:NW], b_stg[s])
                ph.op(ph.dve, lambda h, s=s, dc=dc: h.tensor_scalar(out=win[:, dc, :], in0=stg[s][:], scalar1=vp[:, VP_GIN + dc:VP_GIN + dc + 1], scalar2=None, op0=ALU.mult),
                      reads=[b_stg[s], b_vp], writes=[b_win])
            hts = [sb(f"ht{i}", [128, D], F32) for i in range(2)]; b_ht = [Buf(), Buf()]
            junk = sb("junk", [128, D], BF16); b_junk = Buf()
            sm = sb("sm", [128, 4], F32); b_sm = Buf()
            xn = sb("xn", [128, D], BF16); b_xn = Buf()
            xnT = sb("xnT", [128, NDC, 128], BF16); b_xnT = Buf()
            fm = sb("fm", [128, 12, 128], BF16); b_fm = Buf()
            vas = sb("vas", [128, 768], BF16); b_vas = Buf()
            psB = pst("psB", [128, 2, 1024], BF16); b_psB = [Buf(), Buf()]
            psF = pst("psF", [128, 5, 512], F32); b_psF = [Buf() for _ in range(5)]

            def ld(i):
                ph.load(hts[i % 2][:], src[i * 128:(i + 1) * 128, :], b_ht[i % 2])

            ld(0)
            for i in range(nb):
                if i + 1 < nb:
                    ld(i + 1)
                ht, bh = hts[i % 2], b_ht[i % 2]
                self.rms_tile(ph, ht[:], bh, junk[:], b_junk, sm[:, 0:1], sm[:, 1:2], sm[:, 2:3], b_sm, D)
                ph.op(ph.dve, lambda h, ht=ht: h.tensor_scalar(out=xn[:], in0=ht[:], scalar1=sm[:, 2:3], scalar2=None, op0=ALU.mult),
                      reads=[bh, b_sm], writes=[b_xn])
                for dc in range(NDC):
                    ph.op(ph.pe, lambda h, dc=dc: h.transpose(out=psB[:, dc // 8, (dc % 8) * 128:(dc % 8 + 1) * 128], in_=xn[:, dc * 128:(dc + 1) * 128], identity=ident[:]),
                          reads=[b_xn, b_ident], writes=[b_psB[dc // 8]], pe_acc=True)
                for k in range(2):
                    ph.op(ph.act, lambda h, k=k: h.copy(out=xnT[:, k * 8:(k + 1) * 8, :], in_=psB[:, k, :]), reads=[b_psB[k]], writes=[b_xnT])
                for c in range(12):
                    bk = c // 4
                    for dc in range(NDC):
                        ph.op(ph.pe, lambda h, c=c, dc=dc, bk=bk: h.matmul(psF[:, bk, (c % 4) * 128:(c % 4 + 1) * 128], lhsT=win[:, dc, c * 128:(c + 1) * 128], rhs=xnT[:, dc, :], start=(dc == 0), stop=(dc == NDC - 1)),
                              reads=[b_win, b_xnT], writes=[b_psF[bk]], pe_acc=True)
                for (bk, c0, n) in ((3, 0, 512), (4, 512, 256)):
                    for dc in range(NDC):
                        ph.op(ph.pe, lambda h, bk=bk, c0=c0, n=n, dc=dc: h.matmul(psF[:, bk, 0:n], lhsT=xnT[:, dc, :], rhs=win[:, dc, 1536 + c0:1536 + c0 + n], start=(dc == 0), stop=(dc == NDC - 1)),
                              reads=[b_win, b_xnT], writes=[b_psF[bk]], pe_acc=True)
                for bk in range(3):
                    eng = ph.act if bk % 2 == 0 else ph.dve
                    if eng is ph.act:
                        ph.op(eng, lambda h, bk=bk: h.copy(out=fm[:, bk * 4:(bk + 1) * 4, :], in_=psF[:, bk, :]), reads=[b_psF[bk]], writes=[b_fm])
                    else:
                        ph.op(eng, lambda h, bk=bk: h.tensor_copy(out=fm[:, bk * 4:(bk + 1) * 4, :], in_=psF[:, bk, :]), reads=[b_psF[bk]], writes=[b_fm])
                ph.op(ph.dve, lambda h: h.tensor_copy(out=vas[:, 0:512], in_=psF[:, 3, :]), reads=[b_psF[3]], writes=[b_vas])
                ph.op(ph.act, lambda h: h.copy(out=vas[:, 512:768], in_=psF[:, 4, 0:256]), reads=[b_psF[4]], writes=[b_vas])
                tk = slice(i * 128, (i + 1) * 128)
                ph.store(self.qaT[:, :, tk].rearrange("h p t -> p h t"), fm[:, 0:6, :], b_fm)
                ph.store(self.kaT[:, :, tk].rearrange("h p t -> p h t"), fm[:, 6:12, :], b_fm)
                ph.store(self.va[tk, :], vas[:], b_vas)
            ph.emit(st)

    def p1b(self, l, src):
        nc, S, nb = self.nc, self.S, self.nb
        with ExitStack() as st:
            ph, sb, pst, cs = self.begin(f"p1b{l}", st, want=("ident", "mask01T"))
            ident, b_ident = cs["ident"]
            m01, b_m01 = cs["mask01T"]
            vp = sb("vp", [128, NVP], F32); b_vp = Buf()
            ph.load(vp[:], self.E("vecP")[l], b_vp)
            vb3 = sb("vb3", [128, 1536], F32); b_vb3 = Buf()
            ph.load(vb3[:], self.E("vecB")[l, :, VB_LNG:VB_LNG + 1536], b_vb3)
            NW = 1872
            C0 = 2304
            win = sb("win", [128, NDC, NW], BF16); b_win = Buf()
            wuq = sb("wuq", [128, 4, 768], BF16); b_wuq = Buf()
            wqi = sb("wqi", [128, 4, 1024], BF16); b_wqi = Buf()
            wuk = sb("wuk", [128, 2, 768], BF16); b_wuk = Buf()
            wuv = sb("wuv", [128, 2, 768], BF16); b_wuv = Buf()
            cws = sb("cws", [128, 512], BF16); b_cws = Buf()
            stg = [sb(f"stg{i}", [128, NW], F32) for i in range(2)]; b_stg = [Buf(), Buf()]
            cnt = [0]

            def wload(dst, b_dst, srcap, n, gcol):
                s_ = cnt[0] % 2; cnt[0] += 1
                ph.load(stg[s_][:, 0:n], srcap, b_stg[s_])
                ph.op(ph.dve, lambda h: h.tensor_scalar(out=dst, in0=stg[s_][:, 0:n], scalar1=vp[:, gcol:gcol + 1], scalar2=None, op0=ALU.mult),
                      reads=[b_stg[s_], b_vp], writes=[b_dst])

            for dc in range(NDC):
                wload(win[:, dc, :], b_win, self.E("w_in")[l, dc * 128:(dc + 1) * 128, C0:C0 + NW], NW, VP_GIN + dc)
            for rc in range(4):
                wload(wuq[:, rc, :], b_wuq, self.E("b_w_uq")[l, rc * 128:(rc + 1) * 128, :], 768, VP_CQ + rc)
                wload(wqi[:, rc, :], b_wqi, self.E("b_w_qidx")[l, rc * 128:(rc + 1) * 128, :], 1024, VP_CQ + rc)
            for rc in range(2):
                wload(wuk[:, rc, :], b_wuk, self.E("b_w_uk")[l, rc * 128:(rc + 1) * 128, :], 768, VP_CKV + rc)
                wload(wuv[:, rc, :], b_wuv, self.E("b_w_uv")[l, rc * 128:(rc + 1) * 128, :], 768, VP_CKV + rc)
            s_ = cnt[0] % 2; cnt[0] += 1
            ph.load(stg[s_][:, 0:512], self.E("cwsT")[l], b_stg[s_])
            for g in range(4):
                ph.op(ph.dve, lambda h, g=g, s_=s_: h.tensor_tensor(out=cws[:, g * 128:(g + 1) * 128], in0=stg[s_][:, g * 128:(g + 1) * 128], in1=m01[:], op=ALU.mult),
                      reads=[b_stg[s_], b_m01], writes=[b_cws])

            hts = [sb(f"ht{i}", [128, D], F32) for i in range(2)]; b_ht = [Buf(), Buf()]
            junk = sb("junk", [128, D], BF16); b_junk = Buf()
            sm = sb("sm", [128, 16], F32); b_sm = Buf()
            xn = sb("xn", [128, D], BF16); b_xn = Buf()
            xnT = sb("xnT", [128, NDC, 128], BF16); b_xnT = Buf()
            cn = sb("cn", [128, 768], BF16); b_cn = Buf()
            cT = sb("cT", [128, 6, 128], BF16); b_cT = Buf()
            gv = sb("gv", [128, 512], F32); b_gv = Buf()
            tmp = sb("tmp", [128, 512], F32); b_tmp = Buf()
            vn = sb("vn", [128, 512], BF16); b_vn = Buf()
            uT = sb("uT", [128, 4, 128], F32); b_uT = Buf()
            fm = sb("fm", [128, 24, 128], BF16); b_fm = Buf()
            kix = sb("kix", [64, 128], BF16); b_kix = Buf()
            vbs = sb("vbs", [128, 768], BF16); b_vbs = Buf()
            wix = sb("wix", [128, 16], F32); b_wix = Buf()
            psB = pst("psB", [128, 2, 1024], BF16); b_psB = [Buf(), Buf()]
            psF = pst("psF", [128, 6, 512], F32); bF = [Buf() for _ in range(6)]
            WSC = (16 ** -0.5) * (64 ** -0.5)

            def ld(i):
                ph.load(hts[i % 2][:], src[i * 128:(i + 1) * 128, :], b_ht[i % 2])

            def mm(out, lhsT, rhs, first, last, reads, wbuf):
                ph.op(ph.pe, lambda h: h.matmul(out, lhsT=lhsT, rhs=rhs, start=first, stop=last), reads=reads, writes=[wbuf], pe_acc=True)

            ld(0)
            for i in range(nb):
                if i + 1 < nb:
                    ld(i + 1)
                ht, bh = hts[i % 2], b_ht[i % 2]
                self.rms_tile(ph, ht[:], bh, junk[:], b_junk, sm[:, 0:1], sm[:, 1:2], sm[:, 2:3], b_sm, D)
                ph.op(ph.dve, lambda h, ht=ht: h.tensor_scalar(out=xn[:], in0=ht[:], scalar1=sm[:, 2:3], scalar2=None, op0=ALU.mult),
                      reads=[bh, b_sm], writes=[b_xn])
                for dc in range(NDC):
                    ph.op(ph.pe, lambda h, dc=dc: h.transpose(out=psB[:, dc // 8, (dc % 8) * 128:(dc % 8 + 1) * 128], in_=xn[:, dc * 128:(dc + 1) * 128], identity=ident[:]),
                          reads=[b_xn, b_ident], writes=[b_psB[dc // 8]], pe_acc=True)
                for k in range(2):
                    ph.op(ph.act, lambda h, k=k: h.copy(out=xnT[:, k * 8:(k + 1) * 8, :], in_=psB[:, k, :]), reads=[b_psB[k]], writes=[b_xnT])
                for dc in range(NDC):
                    mm(psF[:, 0, :], xnT[:, dc, :], win[:, dc, 0:512], dc == 0, dc == NDC - 1, [b_win, b_xnT], bF[0])
                for dc in range(NDC):
                    mm(psF[:, 1, 0:336], xnT[:, dc, :], win[:, dc, 512:848], dc == 0, dc == NDC - 1, [b_win, b_xnT], bF[1])
                for dc in range(NDC):
                    mm(psF[:, 2, :], xnT[:, dc, :], win[:, dc, 1360:1872], dc == 0, dc == NDC - 1, [b_win, b_xnT], bF[2])
                for dc in range(NDC):
                    mm(psF[0:64, 3, 0:128], win[:, dc, 768:832], xnT[:, dc, :], dc == 0, dc == NDC - 1, [b_win, b_xnT], bF[3])
                for c in range(4):
                    bk, o = (3, 128 + c * 128) if c < 3 else (4, 0)
                    for dc in range(NDC):
                        mm(psF[:, bk, o:o + 128], win[:, dc, 848 + c * 128:848 + (c + 1) * 128], xnT[:, dc, :], dc == 0, dc == NDC - 1, [b_win, b_xnT], bF[bk])
                self.rms_tile(ph, None, bF[0], junk[:, 0:512], b_junk, sm[:, 3:4], sm[:, 4:5], sm[:, 5:6], b_sm, 512, act_in=psF[:, 0, :])
                ph.op(ph.dve, lambda h: h.tensor_scalar(out=cn[:, 0:512], in0=psF[:, 0, :], scalar1=sm[:, 5:6], scalar2=None, op0=ALU.mult), reads=[bF[0], b_sm], writes=[b_cn])
                self.rms_tile(ph, None, bF[1], junk[:, 0:256], b_junk, sm[:, 6:7], sm[:, 7:8], sm[:, 8:9], b_sm, 256, act_in=psF[:, 1, 0:256])
                ph.op(ph.dve, lambda h: h.tensor_scalar(out=cn[:, 512:768], in0=psF[:, 1, 0:256], scalar1=sm[:, 8:9], scalar2=None, op0=ALU.mult), reads=[bF[1], b_sm], writes=[b_cn])
                ph.op(ph.dve, lambda h: h.tensor_scalar(out=wix[:], in0=psF[:, 1, 320:336], scalar1=WSC, scalar2=None, op0=ALU.mult), reads=[bF[1]], writes=[b_wix])
                ph.op(ph.act, lambda h: h.copy(out=kix[:], in_=psF[0:64, 3, 0:128]), reads=[bF[3]], writes=[b_kix])
                ph.op(ph.act, lambda h: h.activation(out=uT[:, 0:3, :], in_=psF[:, 3, 128:512], func=AF.Gelu_apprx_tanh), reads=[bF[3]], writes=[b_uT])
                ph.op(ph.act, lambda h: h.activation(out=uT[:, 3, :], in_=psF[:, 4, 0:128], func=AF.Gelu_apprx_tanh), reads=[bF[4]], writes=[b_uT])
                ph.op(ph.act, lambda h: h.activation(out=gv[:], in_=psF[:, 2, :], func=AF.Gelu_apprx_tanh, accum_out=sm[:, 9:10]), reads=[bF[2]], writes=[b_gv, b_sm])
                ph.op(ph.dve, lambda h: h.tensor_scalar(out=sm[:, 10:11], in0=sm[:, 9:10], scalar1=-1.0 / 512, scalar2=None, op0=ALU.mult), reads=[b_sm], writes=[b_sm])
                ph.op(ph.act, lambda h: h.activation(out=junk[:, 0:512], in_=gv[:], func=AF.Square, bias=sm[:, 10:11], accum_out=sm[:, 11:12]), reads=[b_gv, b_sm], writes=[b_junk, b_sm])
                ph.op(ph.act, lambda h: h.activation(out=sm[:, 12:13], in_=sm[:, 11:12], func=AF.Sqrt, scale=1.0 / 512, bias=self.eps_ap), reads=[b_sm, self.b_eps], writes=[b_sm])
                ph.op(ph.dve, lambda h: h.reciprocal(out=sm[:, 13:14], in_=sm[:, 12:13]), reads=[b_sm], writes=[b_sm])
                ph.op(ph.dve, lambda h: h.tensor_scalar(out=tmp[:], in0=gv[:], scalar1=sm[:, 10:11], scalar2=sm[:, 13:14], op0=ALU.add, op1=ALU.mult), reads=[b_gv, b_sm], writes=[b_tmp])
                ph.op(ph.dve, lambda h: h.tensor_tensor(out=tmp[:], in0=tmp[:], in1=vb3[:, 0:512], op=ALU.mult), reads=[b_vb3], writes=[b_tmp])
                ph.op(ph.dve, lambda h: h.tensor_tensor(out=vn[:], in0=tmp[:], in1=vb3[:, 512:1024], op=ALU.add), reads=[b_tmp, b_vb3], writes=[b_vn])
                for k in range(6):
                    ph.op(ph.pe, lambda h, k=k: h.transpose(out=psB[:, 0, k * 128:(k + 1) * 128], in_=cn[:, k * 128:(k + 1) * 128], identity=ident[:]),
                          reads=[b_cn, b_ident], writes=[b_psB[0]], pe_acc=True)
                ph.op(ph.act, lambda h: h.copy(out=cT[:], in_=psB[:, 0, 0:768]), reads=[b_psB[0]], writes=[b_cT])
                def fm_group(bk, o, wt, b_wt, nrc, rc0, col0, reads_extra=()):
                    for rc in range(nrc):
                        mm(psF[:, bk, o:o + 128], wt[:, rc, col0:col0 + 128], cT[:, rc0 + rc, :], rc == 0, rc == nrc - 1, [b_wt, b_cT], bF[bk])
                for hh in range(4):
                    fm_group(0, hh * 128, wuq, b_wuq, 4, 0, hh * 128)
                for hh in range(4, 6):
                    fm_group(1, (hh - 4) * 128, wuq, b_wuq, 4, 0, hh * 128)
                for hh in range(2):
                    fm_group(1, 256 + hh * 128, wuk, b_wuk, 2, 4, hh * 128)
                for hh in range(2, 6):
                    fm_group(2, (hh - 2) * 128, wuk, b_wuk, 2, 4, hh * 128)
                for pr in range(4):
                    fm_group(3, pr * 128, wqi, b_wqi, 4, 0, pr * 128)
                for pr in range(4, 8):
                    fm_group(4, (pr - 4) * 128, wqi, b_wqi, 4, 0, pr * 128)
                for rc in range(2):
                    mm(psF[:, 5, :], cT[:, 4 + rc, :], wuv[:, rc, 0:512], rc == 0, rc == 1, [b_wuv, b_cT], bF[5])
                ph.op(ph.act, lambda h: h.copy(out=fm[:, 0:4, :], in_=psF[:, 0, :]), reads=[bF[0]], writes=[b_fm])
                ph.op(ph.dve, lambda h: h.tensor_copy(out=fm[:, 4:6, :], in_=psF[:, 1, 0:256]), reads=[bF[1]], writes=[b_fm])
                ph.op(ph.dve, lambda h: h.tensor_copy(out=fm[:, 6:8, :], in_=psF[:, 1, 256:512]), reads=[bF[1]], writes=[b_fm])
                ph.op(ph.act, lambda h: h.copy(out=fm[:, 8:12, :], in_=psF[:, 2, :]), reads=[bF[2]], writes=[b_fm])
                ph.op(ph.dve, lambda h: h.tensor_copy(out=fm[:, 12:16, :], in_=psF[:, 3, :]), reads=[bF[3]], writes=[b_fm])
                ph.op(ph.act, lambda h: h.copy(out=fm[:, 16:20, :], in_=psF[:, 4, :]), reads=[bF[4]], writes=[b_fm])
                ph.op(ph.dve, lambda h: h.tensor_copy(out=vbs[:, 0:512], in_=psF[:, 5, :]), reads=[bF[5]], writes=[b_vbs])
                for rc in range(2):
                    mm(psF[:, 0, 0:256], cT[:, 4 + rc, :], wuv[:, rc, 512:768], rc == 0, rc == 1, [b_wuv, b_cT], bF[0])
                ph.op(ph.act, lambda h: h.copy(out=vbs[:, 512:768], in_=psF[:, 0, 0:256]), reads=[bF[0]], writes=[b_vbs])
                for g in range(4):
                    mm(psF[:, 1, g * 128:(g + 1) * 128], vn[:, g * 128:(g + 1) * 128], cws[:, g * 128:(g + 1) * 128], True, True, [b_vn, b_cws], bF[1])
                ph.op(ph.dve, lambda h: h.tensor_tensor(out=tmp[:], in0=psF[:, 1, :], in1=vb3[:, 1024:1536], op=ALU.add), reads=[bF[1], b_vb3], writes=[b_tmp])
                ph.op(ph.dve, lambda h: h.tensor_tensor(out=fm[:, 20:24, :], in0=tmp[:], in1=uT[:], op=ALU.mult), reads=[b_tmp, b_uT], writes=[b_fm])
                tk = slice(i * 128, (i + 1) * 128)
                ph.store(self.qbT[:, :, tk].rearrange("h p t -> p h t"), fm[:, 0:6, :], b_fm)
                ph.store(self.kbT[:, :, tk].rearrange("h p t -> p h t"), fm[:, 6:12, :], b_fm)
                ph.store(self.qidxT[:, :, tk].rearrange("h p t -> p h t"), fm[:, 12:20, :], b_fm)
                ph.store(self.mixT[12:16, :, tk].rearrange("h p t -> p h t"), fm[:, 20:24, :], b_fm)
                ph.store(self.vb[tk, :], vbs[:], b_vbs)
                ph.store(self.kidxT[:, tk], kix[:], b_kix)
                ph.store(self.widx[tk, :], wix[:], b_wix)
            ph.emit(st)


    def attn(self, l, kind):
        nc, S, nb, ng = self.nc, self.S, self.nb, self.ng
        isA = kind == "A"
        qT_d, kT_d, v_d = (self.qaT, self.kaT, self.va) if isA else (self.qbT, self.kbT, self.vb)
        H0 = 0 if isA else 6
        nmap = 2 if isA else 1
        scale = 64 ** -0.5 if isA else 128 ** -0.5
        lam_init = 0.8 - 0.6 * math.exp(-0.3 * l)
        with ExitStack() as st:
            ph, sb, pst, cs = self.begin(f"att{kind}{l}", st, want=("ident", "causnegT"))
            ident, b_ident = cs["ident"]
            cneg, b_cneg = cs["causnegT"]
            kT = sb("kT", [128, 6, S], BF16); b_kT = Buf()
            ph.load(kT[:], kT_d.rearrange("h p t -> p h t"), b_kT)
            v1 = sb("v1", [128, nb, 6, 129], BF16); b_v1 = Buf()
            ph.op(ph.pool, lambda h: h.memset(v1[:, :, :, 128:129], 1.0), writes=[b_v1])
            for j0 in range(nb):
                ph.dma(v1[:, j0, :, 0:128], v_d[j0 * 128:(j0 + 1) * 128, :].rearrange("p (h d) -> p h d", d=128), writes=[b_v1], sbuf=b_v1)
            b31 = sb("b31", [128, 12], F32); b_b31 = Buf()
            ph.load(b31[:], self.E("b31"), b_b31)
            braw = sb("braw", [128, 6, 2, 128], F32); b_braw = Buf()
            ph.load(braw[:], self.E("biasT")[:, H0 * 256:(H0 + 6) * 256].rearrange("p (h k q) -> p h k q", h=6, k=2), b_braw)
            bt = sb("bt", [128, 6, 2, 128], BF16); b_bt = Buf()
            for hh in range(6):
                ph.op(ph.dve, lambda h, hh=hh: h.tensor_tensor(out=braw[:, hh, 0, :], in0=braw[:, hh, 0, :], in1=cneg[:], op=ALU.add), reads=[b_cneg], writes=[b_braw])
                ph.op(ph.dve, lambda h, hh=hh: h.tensor_scalar(out=bt[:, hh, :, :], in0=braw[:, hh, :, :], scalar1=b31[:, H0 + hh:H0 + hh + 1], scalar2=1.0 / scale, op0=ALU.subtract, op1=ALU.mult),
                      reads=[b_braw, b_b31], writes=[b_bt])
            sm = sb("sm", [128, 32], F32); b_sm = Buf()
            if isA:
                vbl = sb("vbl", [128, 256], F32); b_vbl = Buf()
                ph.load(vbl[:], self.E("vecB")[l, :, VB_LAM:VB_LAM + 256], b_vbl)
                pr = sb("pr", [128, 128], F32); b_pr = Buf()
                ph.op(ph.dve, lambda h: h.tensor_tensor(out=pr[:, 0:64], in0=vbl[:, 0:64], in1=vbl[:, 64:128], op=ALU.mult), reads=[b_vbl], writes=[b_pr])
                ph.op(ph.dve, lambda h: h.tensor_tensor(out=pr[:, 64:128], in0=vbl[:, 128:192], in1=vbl[:, 192:256], op=ALU.mult), reads=[b_vbl], writes=[b_pr])
                ph.op(ph.dve, lambda h: h.reduce_sum(out=sm[:, 0:1], in_=pr[:, 0:64], axis=AX.X), reads=[b_pr], writes=[b_sm])
                ph.op(ph.dve, lambda h: h.reduce_sum(out=sm[:, 1:2], in_=pr[:, 64:128], axis=AX.X), reads=[b_pr], writes=[b_sm])
                ph.op(ph.act, lambda h: h.activation(out=sm[:, 2:4], in_=sm[:, 0:2], func=AF.Exp), reads=[b_sm], writes=[b_sm])
                ph.op(ph.dve, lambda h: h.tensor_tensor(out=sm[:, 4:5], in0=sm[:, 2:3], in1=sm[:, 3:4], op=ALU.subtract), reads=[b_sm], writes=[b_sm])
                ph.op(ph.dve, lambda h: h.tensor_scalar(out=sm[:, 5:6], in0=sm[:, 4:5], scalar1=lam_init, scalar2=-1.0, op0=ALU.add, op1=ALU.mult), reads=[b_sm], writes=[b_sm])
            b_qg = [Buf(), Buf()]
            if not isA:
                qg = [sb(f"qg{i}", [128, 6, 512], BF16) for i in range(2)]
            if not isA:
                mt = [sb(f"mt{i}", [128, nb, 512], BF16) for i in range(2)]; b_mt = [Buf(), Buf()]
            pts = [sb(f"pt{i}", [128, 512], BF16) for i in range(3)]; b_pt = [Buf() for _ in range(3)]
            o0 = sb("o0", [128, 4, 128], F32); b_o0 = Buf()
            on = sb("on", [128, 4, 128], BF16); b_on = Buf()
            junkf = sb("junkf", [128, 128], F32); b_junk = Buf()
            outT = [sb(f"outT{i}", [128, 512], BF16) for i in range(2)]; b_outT = [Buf(), Buf()]
            psS = pst("psS", [128, 3, 512], F32); b_psS = [Buf() for _ in range(3)]
            psA = pst("psA", [128, 4, 512], F32); b_psA = [Buf() for _ in range(4)]
            psT = pst("psT", [128, 1024], BF16); b_psT = Buf()
            rows = lambda m: slice(m * 64, (m + 1) * 64) if isA else slice(0, 128)

            if isA:
                qz = [[sb(f"qz{m}_{i}", [128, 6, 512], BF16) for i in range(2)] for m in range(2)]
                for i in range(2):
                    ph.op(ph.pool, lambda h, i=i: h.memset(qz[0][i][64:128, :, :], 0.0), writes=[b_qg[i]])
                    ph.op(ph.pool, lambda h, i=i: h.memset(qz[1][i][0:64, :, :], 0.0), writes=[b_qg[i]])

            def ldg(G):
                if isA:
                    srcq = qT_d[:, :, G * 512:(G + 1) * 512].rearrange("h p t -> p h t")
                    ph.dma(qz[0][G % 2][0:64, :, :], srcq[0:64], writes=[b_qg[G % 2]], sbuf=b_qg[G % 2])
                    ph.dma(qz[1][G % 2][64:128, :, :], srcq[64:128], writes=[b_qg[G % 2]], sbuf=b_qg[G % 2])
                else:
                    ph.load(qg[G % 2][:], qT_d[:, :, G * 512:(G + 1) * 512].rearrange("h p t -> p h t"), b_qg[G % 2])
                if not isA:
                    nj = 4 * G + 4
                    ph.load(mt[G % 2][:, 0:nj, :], self.MTd[G, :, 0:nj, :], b_mt[G % 2])

            cst = [0]
            cout = [0]
            ldg(0)
            for G in range(ng):
                if G + 1 < ng:
                    ldg(G + 1)
                bq = b_qg[G % 2]
                nj = 4 * G + 4
                steps = [(hh, m, j) for hh in range(6) for m in range(nmap) for j in range(nj)]
                slot = {}

                def emit_qk(idx, G=G, bq=bq):
                    hh, m, j = steps[idx]
                    c0 = max(0, j - 4 * G)
                    cols = slice(c0 * 128, 512)
                    sbk = cst[0] % 3; cst[0] += 1
                    slot[idx] = sbk
                    q_ = qz[m][G % 2] if isA else qg[G % 2]
                    near = [(k, 4 * G + k - j) for k in range(4) if 0 <= 4 * G + k - j <= 1]
                    ph.op(ph.pe, lambda h: h.matmul(psS[:, sbk, cols], lhsT=kT[:, hh, j * 128:(j + 1) * 128], rhs=q_[:, hh, cols], start=True, stop=(len(near) == 0)),
                          reads=[b_kT, bq], writes=[b_psS[sbk]], pe_acc=True)
                    for ii, (k, kd) in enumerate(near):
                        ph.op(ph.pe, lambda h, k=k, kd=kd, ii=ii: h.matmul(psS[:, sbk, k * 128:(k + 1) * 128], lhsT=ident[:], rhs=bt[:, hh, kd, :], start=False, stop=(ii == len(near) - 1)),
                              reads=[b_ident, b_bt], writes=[b_psS[sbk]], pe_acc=True)

                pending = []
                emit_qk(0)
                for idx, (hh, m, j) in enumerate(steps):
                    if idx + 1 < len(steps):
                        emit_qk(idx + 1)
                    aset = ((G * 6 + hh) * nmap + m) % 2
                    accb = (2 * aset, 2 * aset + 1)
                    c0 = max(0, j - 4 * G)
                    cols = slice(c0 * 128, 512)
                    sbk = slot[idx]
                    pt, bp = pts[sbk], b_pt[sbk]
                    ph.op(ph.act, lambda h, pt=pt, sbk=sbk, cols=cols, hh=hh: h.activation(out=pt[:, cols], in_=psS[:, sbk, cols], func=AF.Exp, bias=b31[:, H0 + hh:H0 + hh + 1], scale=scale),
                          reads=[b_psS[sbk], b_b31], writes=[bp])
                    if not isA:
                        mt_ = mt[G % 2]
                        ph.op(ph.dve, lambda h, pt=pt, cols=cols, j=j, mt_=mt_: h.tensor_tensor(out=pt[:, cols], in0=pt[:, cols], in1=mt_[:, j, cols], op=ALU.mult),
                              reads=[b_mt[G % 2]], writes=[bp])
                    for k in range(c0, 4):
                        ab = accb[k // 2]
                        ph.op(ph.pe, lambda h, ab=ab, k=k, pt=pt, j=j, hh=hh, G=G: h.matmul(psA[:, ab, (k % 2) * 129:(k % 2) * 129 + 129], lhsT=pt[:, k * 128:(k + 1) * 128], rhs=v1[:, j, hh, :], start=(j == 0 and k % 2 == 0), stop=(j == 4 * G + k), skip_group_check=True),
                              reads=[bp, b_v1], writes=[b_psA[ab]], pe_acc=True)
                    if pending and idx - pending[0][0] >= 3:
                        pending.pop(0)[1]()
                    if j == nj - 1:
                        for k in range(4):
                            ab = accb[k // 2]; o = (k % 2) * 129
                            ph.op(ph.dve, lambda h, ab=ab, o=o, k=k: h.reciprocal(out=sm[:, 8 + k:9 + k], in_=psA[:, ab, o + 128:o + 129]), reads=[b_psA[ab]], writes=[b_sm])
                            if isA and m == 0:
                                ph.op(ph.dve, lambda h, ab=ab, o=o, k=k: h.tensor_scalar(out=o0[:, k, :], in0=psA[:, ab, o:o + 128], scalar1=sm[:, 8 + k:9 + k], scalar2=None, op0=ALU.mult), reads=[b_psA[ab], b_sm], writes=[b_o0])
                            elif isA:
                                ph.op(ph.dve, lambda h, k=k: h.tensor_tensor(out=sm[:, 12 + k:13 + k], in0=sm[:, 8 + k:9 + k], in1=sm[:, 5:6], op=ALU.mult), reads=[b_sm], writes=[b_sm])
                                ph.op(ph.dve, lambda h, ab=ab, o=o, k=k: h.scalar_tensor_tensor(out=o0[:, k, :], in0=psA[:, ab, o:o + 128], scalar=sm[:, 12 + k:13 + k], in1=o0[:, k, :], op0=ALU.mult, op1=ALU.add), reads=[b_psA[ab], b_sm], writes=[b_o0])
                                ph.op(ph.dve, lambda h, k=k: h.tensor_tensor(out=junkf[:], in0=o0[:, k, :], in1=o0[:, k, :], op=ALU.mult), reads=[b_o0], writes=[b_junk])
                                ph.op(ph.dve, lambda h, k=k: h.reduce_sum(out=sm[:, 16 + k:17 + k], in_=junkf[:], axis=AX.X), reads=[b_junk], writes=[b_sm])
                            else:
                                ph.op(ph.dve, lambda h, ab=ab, o=o, k=k: h.tensor_scalar(out=on[:, k, :], in0=psA[:, ab, o:o + 128], scalar1=sm[:, 8 + k:9 + k], scalar2=None, op0=ALU.mult), reads=[b_psA[ab], b_sm], writes=[b_on])
                        if isA and m == 0:
                            continue

                        def epi2(hh=hh, G=G):
                            if isA:
                                ph.op(ph.act, lambda h: h.activation(out=sm[:, 20:24], in_=sm[:, 16:20], func=AF.Ln, scale=1.0 / 128, bias=self.eps_ap), reads=[b_sm, self.b_eps], writes=[b_sm])
                                ph.op(ph.act, lambda h: h.activation(out=sm[:, 24:28], in_=sm[:, 20:24], func=AF.Exp, scale=-0.5), reads=[b_sm], writes=[b_sm])
                                for k in range(4):
                                    ph.op(ph.dve, lambda h, k=k: h.tensor_scalar(out=on[:, k, :], in0=o0[:, k, :], scalar1=sm[:, 24 + k:25 + k], scalar2=None, op0=ALU.mult), reads=[b_o0, b_sm], writes=[b_on])
                            for k in range(4):
                                ph.op(ph.pe, lambda h, k=k: h.transpose(out=psT[:, k * 128:(k + 1) * 128], in_=on[:, k, :], identity=ident[:]), reads=[b_on, b_ident], writes=[b_psT], pe_acc=True)
                            oi = cout[0] % 2; cout[0] += 1
                            ph.op(ph.dve, lambda h, oi=oi: h.tensor_copy(out=outT[oi][:], in_=psT[:, 0:512]), reads=[b_psT], writes=[b_outT[oi]])
                            ph.store(self.mixT[H0 + hh, :, G * 512:(G + 1) * 512], outT[oi][:], b_outT[oi])

                        pending.append((idx, epi2))
                while pending:
                    pending.pop(0)[1]()
            ph.emit(st)


    def bindex(self, l):
        nc, S, nb, ng = self.nc, self.S, self.nb, self.ng
        with ExitStack() as st:
            ph, sb, pst, cs = self.begin(f"bidx{l}", st, want=("ident", "causneg", "pow2"))
            ident, b_ident = cs["ident"]
            cneg, b_cneg = cs["causneg"]
            pow2, b_pow2 = cs["pow2"]
            kx = sb("kx", [128, S], BF16); b_kx = Buf()
            ph.dma(kx[0:64, :], self.kidxT, writes=[b_kx], sbuf=b_kx)
            ph.dma(kx[64:128, :], self.kidxT, writes=[b_kx], sbuf=b_kx)
            b_qi = [Buf(), Buf()]
            qz = [[sb(f"qz{e}_{i}", [128, 8, 128], BF16) for i in range(2)] for e in range(2)]
            for i in range(2):
                ph.op(ph.pool, lambda h, i=i: h.memset(qz[0][i][64:128, :, :], 0.0), writes=[b_qi[i]])
                ph.op(ph.pool, lambda h, i=i: h.memset(qz[1][i][0:64, :, :], 0.0), writes=[b_qi[i]])
            wi = [sb(f"wi{i}", [128, 16], F32) for i in range(2)]; b_wi = [Buf(), Buf()]
            wab = [sb(f"wab{i}", [128, 16], F32) for i in range(2)]; b_wab = [Buf(), Buf()]
            wsg = [sb(f"wsg{i}", [128, 16], F32) for i in range(2)]
            dsg = [sb(f"dsg{i}", [128, 16, 128], BF16) for i in range(2)]; b_dsg = [Buf(), Buf()]
            scs = [sb(f"sc{i}", [128, S], F32) for i in range(2)]; b_scs = [Buf(), Buf()]
            NR = 4
            rl = [sb(f"rl{i}", [128, 512], BF16) for i in range(NR)]; b_rl = [Buf() for _ in range(NR)]
            junkb = sb("junkb", [128, S], BF16); b_junkb = Buf()
            msk = sb("msk", [128, S], BF16); b_msk = Buf()
            MTg = sb("MTg", [128, nb, 512], BF16); b_MTg = Buf()
            ph.op(ph.pool, lambda h: h.memset(MTg[:], 0.0), writes=[b_MTg])
            sm = sb("sm", [128, 64], F32); b_sm = Buf()
            psD = pst("psD", [128, 3, 512], F32); b_psD = [Buf() for _ in range(3)]
            psC = pst("psC", [128, 2, 512], F32); b_psC = [Buf(), Buf()]
            psT = pst("psT", [128, 2, 1024], BF16); b_psT = [Buf(), Buf()]
            WH = 16

            def ldq(i):
                tk = slice(i * 128, (i + 1) * 128)
                srcq = self.qidxT[:, :, tk].rearrange("h p t -> p h t")
                ph.dma(qz[0][i % 2][0:64, :, :], srcq[0:64], writes=[b_qi[i % 2]], sbuf=b_qi[i % 2])
                ph.dma(qz[1][i % 2][64:128, :, :], srcq[64:128], writes=[b_qi[i % 2]], sbuf=b_qi[i % 2])
                ph.load(wi[i % 2][:], self.widx[tk, :], b_wi[i % 2])

            cd = [0]
            cr = [0]
            cc = [0]
            ct = [0]

            def scores(i):
                p_ = i % 2
                bq, w_, bw = b_qi[p_], wi[p_], b_wi[p_]
                sc, b_sc = scs[p_], b_scs[p_]
                Si = (i + 1) * 128
                ph.op(ph.dve, lambda h: h.tensor_scalar(out=wab[p_][:], in0=w_[:], scalar1=-1.0, scalar2=None, op0=ALU.mult), reads=[bw], writes=[b_wab[p_]])
                ph.op(ph.dve, lambda h: h.tensor_tensor(out=wab[p_][:], in0=wab[p_][:], in1=w_[:], op=ALU.max), reads=[bw], writes=[b_wab[p_]])
                ph.op(ph.dve, lambda h: h.tensor_scalar(out=wsg[p_][:], in0=w_[:], scalar1=0.0, scalar2=2.0, op0=ALU.is_ge, op1=ALU.mult), reads=[bw], writes=[b_dsg[p_]])
                ph.op(ph.dve, lambda h: h.tensor_scalar(out=wsg[p_][:], in0=wsg[p_][:], scalar1=-1.0, scalar2=None, op0=ALU.add), writes=[b_dsg[p_]])
                for g in range(16):
                    ph.op(ph.dve, lambda h, g=g: h.tensor_scalar(out=dsg[p_][:, g, :], in0=ident[:], scalar1=wsg[p_][:, g:g + 1], scalar2=None, op0=ALU.mult), reads=[b_ident], writes=[b_dsg[p_]])
                def chunk(c):
                    n = min(512, Si - c * 512)
                    k0 = c * 512
                    cb = cc[0] % 2; cc[0] += 1
                    slots = {}

                    def dots(g):
                        db = cd[0] % 3; cd[0] += 1
                        slots[g] = db
                        ph.op(ph.pe, lambda h: h.matmul(psD[:, db, 0:n], lhsT=qz[g % 2][p_][:, g // 2, :], rhs=kx[:, k0:k0 + n], start=True, stop=True),
                              reads=[bq, b_kx], writes=[b_psD[db]], pe_acc=True)

                    dots(0)
                    for g in range(16):
                        if g + 1 < 16:
                            dots(g + 1)
                        db = slots[g]
                        r_ = cr[0] % NR; cr[0] += 1
                        ph.op(ph.act, lambda h, db=db, r_=r_, g=g: h.activation(out=rl[r_][:, 0:n], in_=psD[:, db, 0:n], func=AF.Relu, scale=wab[p_][:, g:g + 1]), reads=[b_psD[db], b_wab[p_]], writes=[b_rl[r_]])
                        ph.op(ph.pe, lambda h, r_=r_, g=g: h.matmul(psC[:, cb, 0:n], lhsT=dsg[p_][:, g, :], rhs=rl[r_][:, 0:n], start=(g == 0), stop=(g == 15)),
                              reads=[b_dsg[p_], b_rl[r_]], writes=[b_psC[cb]], pe_acc=True)
                    ph.op(ph.act, lambda h: h.copy(out=sc[:, k0:k0 + n], in_=psC[:, cb, 0:n]), reads=[b_psC[cb]], writes=[b_sc])

                for c in range((Si + 511) // 512):
                    chunk(c)

            def select(i):
                p_ = i % 2
                sc, b_sc = scs[p_], b_scs[p_]
                Si = (i + 1) * 128
                ph.op(ph.dve, lambda h: h.tensor_tensor(out=sc[:, i * 128:(i + 1) * 128], in0=sc[:, i * 128:(i + 1) * 128], in1=cneg[:], op=ALU.add), reads=[b_cneg], writes=[b_sc])
                if i < 2:
                    ph.op(ph.dve, lambda h: h.memset(sm[:, 13:14], -1e29), writes=[b_sm])
                else:
                    ph.op(ph.dve, lambda h: h.max(out=sm[:, 0:8], in_=sc[:, 0:Si]), reads=[b_sc], writes=[b_sm])
                    ph.op(ph.dve, lambda h: h.tensor_reduce(out=sm[:, 8:9], in_=sc[:, 0:i * 128], op=ALU.min, axis=AX.X), reads=[b_sc], writes=[b_sm])
                    ph.op(ph.dve, lambda h: h.tensor_tensor(out=sm[:, 9:10], in0=sm[:, 0:1], in1=sm[:, 8:9], op=ALU.subtract), reads=[b_sm], writes=[b_sm])
                    ph.op(ph.dve, lambda h: h.tensor_scalar(out=sm[:, WH:WH + NBIS + 1], in0=pow2[:, 0:NBIS + 1], scalar1=sm[:, 9:10], scalar2=None, op0=ALU.mult), reads=[b_sm, b_pow2], writes=[b_sm])
                    ph.op(ph.dve, lambda h: h.tensor_tensor(out=sm[:, 10:11], in0=sm[:, 8:9], in1=sm[:, WH:WH + 1], op=ALU.add), reads=[b_sm], writes=[b_sm])
                    for k in range(NBIS):
                        ph.op(ph.dve, lambda h: h.tensor_scalar(out=junkb[:, 0:Si], in0=sc[:, 0:Si], scalar1=sm[:, 10:11], scalar2=0.0, op0=ALU.is_ge, op1=ALU.add, accum_out=sm[:, 11:12]),
                              reads=[b_sc, b_sm], writes=[b_junkb, b_sm])
                        ph.op(ph.dve, lambda h: h.tensor_scalar(out=sm[:, 12:13], in0=sm[:, 11:12], scalar1=float(TOPK), scalar2=0.5, op0=ALU.is_ge, op1=ALU.subtract), reads=[b_sm], writes=[b_sm])
                        ph.op(ph.dve, lambda h, k=k: h.scalar_tensor_tensor(out=sm[:, 10:11], in0=sm[:, WH + k:WH + k + 1], scalar=sm[:, 12:13], in1=sm[:, 10:11], op0=ALU.mult, op1=ALU.add), reads=[b_sm], writes=[b_sm])
                    ph.op(ph.dve, lambda h: h.tensor_tensor(out=sm[:, 13:14], in0=sm[:, 10:11], in1=sm[:, WH + NBIS:WH + NBIS + 1], op=ALU.subtract), reads=[b_sm], writes=[b_sm])
                ph.op(ph.dve, lambda h: h.tensor_scalar(out=msk[:, 0:Si], in0=sc[:, 0:Si], scalar1=sm[:, 13:14], scalar2=None, op0=ALU.is_ge), reads=[b_sc, b_sm], writes=[b_msk])

            def transposes(i):
                kq = i % 4
                for j0 in range(0, i + 1, 8):
                    nj = min(8, i + 1 - j0)
                    tb = ct[0] % 2; ct[0] += 1
                    for jj in range(nj):
                        j = j0 + jj
                        ph.op(ph.pe, lambda h, tb=tb, jj=jj, j=j: h.transpose(out=psT[:, tb, jj * 128:(jj + 1) * 128], in_=msk[:, j * 128:(j + 1) * 128], identity=ident[:]),
                              reads=[b_msk, b_ident], writes=[b_psT[tb]], pe_acc=True)
                    ph.op(ph.dve, lambda h, tb=tb, nj=nj, j0=j0, kq=kq: h.tensor_copy(out=MTg[:, j0:j0 + nj, kq * 128:(kq + 1) * 128], in_=psT[:, tb, 0:nj * 128].rearrange("p (j q) -> p j q", q=128)),
                          reads=[b_psT[tb]], writes=[b_MTg])
                if kq == 3:
                    G = i // 4
                    ph.store(self.MTd[G, :, 0:4 * G + 4, :], MTg[:, 0:4 * G + 4, :], b_MTg)

            ldq(0)
            scores(0)
            for i in range(nb):
                if i + 1 < nb:
                    ldq(i + 1)
                    scores(i + 1)
                select(i)
                transposes(i)
            ph.emit(st)

    def memkv(self, l):
        nc = self.nc
        with ExitStack() as st:
            ph, sb, pst, cs = self.begin(f"mkv{l}", st)
            ident, b_ident = cs["ident"]
            vp = sb("vp", [128, NVP], F32); b_vp = Buf()
            ph.load(vp[:], self.E("vecP")[l], b_vp)
            wkv = sb("wkv", [128, NDC, 1024], BF16); b_wkv = Buf()
            stg = [sb(f"stg{i}", [128, 1024], F32) for i in range(2)]; b_stg = [Buf(), Buf()]
            for dc in range(NDC):
                s_ = dc % 2
                ph.load(stg[s_][:], self.E("x_wkv")[l, dc * 128:(dc + 1) * 128, :], b_stg[s_])
                ph.op(ph.dve, lambda h, s_=s_, dc=dc: h.tensor_scalar(out=wkv[:, dc, :], in0=stg[s_][:], scalar1=vp[:, VP_MEMG + dc:VP_MEMG + dc + 1], scalar2=None, op0=ALU.mult),
                      reads=[b_stg[s_], b_vp], writes=[b_wkv])
            hts = [sb(f"ht{i}", [128, D], F32) for i in range(2)]; b_ht = [Buf(), Buf()]
            junk = sb("junk", [128, D], BF16); b_junk = Buf()
            sm = sb("sm", [128, 4], F32); b_sm = Buf()
            xn = sb("xn", [128, D], BF16); b_xn = Buf()
            xnT = sb("xnT", [128, NDC, 128], BF16); b_xnT = Buf()
            kst = sb("kst", [128, 4, 128], BF16); b_kst = Buf()
            vst = sb("vst", [128, 512], BF16); b_vst = Buf()
            psB = pst("psB", [128, 2, 1024], BF16); b_psB = [Buf(), Buf()]
            psF = pst("psF", [128, 2, 512], F32); bF = [Buf(), Buf()]
            for i in range(2):
                ph.load(hts[i][:], self.E("mem")[i * 128:(i + 1) * 128, :], b_ht[i])
            for i in range(2):
                ht, bh = hts[i], b_ht[i]
                self.rms_tile(ph, ht[:], bh, junk[:], b_junk, sm[:, 0:1], sm[:, 1:2], sm[:, 2:3], b_sm, D)
                ph.op(ph.dve, lambda h, ht=ht: h.tensor_scalar(out=xn[:], in0=ht[:], scalar1=sm[:, 2:3], scalar2=None, op0=ALU.mult), reads=[bh, b_sm], writes=[b_xn])
                for dc in range(NDC):
                    ph.op(ph.pe, lambda h, dc=dc: h.transpose(out=psB[:, dc // 8, (dc % 8) * 128:(dc % 8 + 1) * 128], in_=xn[:, dc * 128:(dc + 1) * 128], identity=ident[:]),
                          reads=[b_xn, b_ident], writes=[b_psB[dc // 8]], pe_acc=True)
                for k in range(2):
                    ph.op(ph.act, lambda h, k=k: h.copy(out=xnT[:, k * 8:(k + 1) * 8, :], in_=psB[:, k, :]), reads=[b_psB[k]], writes=[b_xnT])
                for hh in range(4):
                    for dc in range(NDC):
                        ph.op(ph.pe, lambda h, hh=hh, dc=dc: h.matmul(psF[:, 0, hh * 128:(hh + 1) * 128], lhsT=wkv[:, dc, hh * 128:(hh + 1) * 128], rhs=xnT[:, dc, :], start=(dc == 0), stop=(dc == NDC - 1)),
                              reads=[b_wkv, b_xnT], writes=[bF[0]], pe_acc=True)
                for dc in range(NDC):
                    ph.op(ph.pe, lambda h, dc=dc: h.matmul(psF[:, 1, :], lhsT=xnT[:, dc, :], rhs=wkv[:, dc, 512:1024], start=(dc == 0), stop=(dc == NDC - 1)),
                          reads=[b_wkv, b_xnT], writes=[bF[1]], pe_acc=True)
                ph.op(ph.act, lambda h: h.copy(out=kst[:], in_=psF[:, 0, :]), reads=[bF[0]], writes=[b_kst])
                ph.op(ph.dve, lambda h: h.tensor_copy(out=vst[:], in_=psF[:, 1, :]), reads=[bF[1]], writes=[b_vst])
                ph.store(self.kmT[:, :, i * 128:(i + 1) * 128], kst[:], b_kst)
                ph.store(self.vm[i * 128:(i + 1) * 128, :], vst[:], b_vst)
            ph.emit(st)

    def p3(self, l):
        nc, S, nb = self.nc, self.S, self.nb
        lam_init = 0.8 - 0.6 * math.exp(-0.3 * l)
        xscale = 128 ** -0.5
        hsrc = self.E("x") if l == 0 else self.h
        with ExitStack() as st:
            ph, sb, pst, cs = self.begin(f"p3{l}", st)
            ident, b_ident = cs["ident"]
            vp = sb("vp", [128, NVP], F32); b_vp = Buf()
            ph.load(vp[:], self.E("vecP")[l], b_vp)
            sg = sb("sg", [128, 1], F32); b_sg = Buf()
            ph.op(ph.dve, lambda h: h.tensor_scalar(out=sg[:], in0=vp[:, VP_SUBG:VP_SUBG + 1], scalar1=1.0 - lam_init, scalar2=None, op0=ALU.mult), reads=[b_vp], writes=[b_sg])
            pg = sb("pg", [128, 2, D], F32); b_pg = Buf()
            ph.load(pg[:, 0, :], self.E("vecB")[l, :, VB_MIXPOST:VB_MIXPOST + D], b_pg)
            ph.load(pg[:, 1, :], self.E("vecB")[l, :, VB_XPOST:VB_XPOST + D], b_pg)
            wout = sb("wout", [128, NDC, D], BF16); b_wout = Buf()
            xwq = sb("xwq", [128, NDC, 512], BF16); b_xwq = Buf()
            xwo = sb("xwo", [128, 4, D], BF16); b_xwo = Buf()
            kmT = sb("kmT", [128, 4, 256], BF16); b_kmT = Buf()
            ph.load(kmT[:], self.kmT, b_kmT)
            vm1 = sb("vm1", [128, 2, 4, 129], BF16); b_vm1 = Buf()
            ph.op(ph.pool, lambda h: h.memset(vm1[:, :, :, 128:129], 1.0), writes=[b_vm1])
            for mc in range(2):
                ph.dma(vm1[:, mc, :, 0:128], self.vm[mc * 128:(mc + 1) * 128, :].rearrange("p (h d) -> p h d", d=128), writes=[b_vm1], sbuf=b_vm1)
            stg = [sb(f"stg{i}", [128, D], F32) for i in range(2)]; b_stg = [Buf(), Buf()]
            cnt = [0]

            def wload(dst, b_dst, srcap, n, scal):
                s_ = cnt[0] % 2; cnt[0] += 1
                ph.load(stg[s_][:, 0:n], srcap, b_stg[s_])
                if scal is None:
                    ph.op(ph.dve, lambda h: h.tensor_copy(out=dst, in_=stg[s_][:, 0:n]), reads=[b_stg[s_]], writes=[b_dst])
                else:
                    ph.op(ph.dve, lambda h: h.tensor_scalar(out=dst, in0=stg[s_][:, 0:n], scalar1=scal, scalar2=None, op0=ALU.mult), reads=[b_stg[s_], b_vp, b_sg], writes=[b_dst])

            for c in range(NDC):
                wload(wout[:, c, :], b_wout, self.E("w_out")[l, c * 128:(c + 1) * 128, :], D, sg[:, 0:1] if c < 6 else None)
            for dc in range(NDC):
                wload(xwq[:, dc, :], b_xwq, self.E("x_wq")[l, dc * 128:(dc + 1) * 128, :], 512, vp[:, VP_XPRE + dc:VP_XPRE + dc + 1])
            for hh in range(4):
                wload(xwo[:, hh, :], b_xwo, self.E("x_wo")[l, hh * 128:(hh + 1) * 128, :], D, None)
            hts = [sb(f"ht{i}", [128, D], F32) for i in range(2)]; b_ht = [Buf(), Buf()]
            mxs = [sb(f"mx{i}", [128, NDC, 128], BF16) for i in range(2)]; b_mx = [Buf(), Buf()]
            junk = sb("junk", [128, D], BF16); b_junk = Buf()
            tmp = sb("tmp", [128, D], F32); b_tmp = Buf()
            sm = sb("sm", [128, 24], F32); b_sm = Buf()
            xn = sb("xn", [128, D], BF16); b_xn = Buf()
            xnT = sb("xnT", [128, NDC, 128], BF16); b_xnT = Buf()
            qx = sb("qx", [128, 4, 128], BF16); b_qx = Buf()
            pl = sb("pl", [128, 8, 128], BF16); b_pl = Buf()
            on = sb("on", [128, 4, 128], BF16); b_on = Buf()
            oT = sb("oT", [128, 4, 128], BF16); b_oT = Buf()
            psY = pst("psY", [128, 4, 512], F32); b_psY = Buf()
            psB = pst("psB", [128, 2, 1024], BF16); b_psB = [Buf(), Buf()]
            psX = pst("psX", [128, 2, 512], F32); b_psX = Buf()

            def ld(i):
                tk = slice(i * 128, (i + 1) * 128)
                ph.load(hts[i % 2][:], hsrc[tk, :], b_ht[i % 2])
                ph.load(mxs[i % 2][:], self.mixT[:, :, tk].rearrange("c p t -> p c t"), b_mx[i % 2])

            def post(which, ht, bh):
                ph.op(ph.act, lambda h: h.activation(out=junk[:], in_=psY[:].rearrange("p a b -> p (a b)"), func=AF.Square, accum_out=sm[:, 0:1]), reads=[b_psY], writes=[b_junk, b_sm])
                ph.op(ph.act, lambda h: h.activation(out=sm[:, 1:2], in_=sm[:, 0:1], func=AF.Sqrt, scale=1.0 / D, bias=self.eps_ap), reads=[b_sm, self.b_eps], writes=[b_sm])
                ph.op(ph.dve, lambda h: h.reciprocal(out=sm[:, 2:3], in_=sm[:, 1:2]), reads=[b_sm], writes=[b_sm])
                ph.op(ph.dve, lambda h: h.scalar_tensor_tensor(out=tmp[:], in0=psY[:].rearrange("p a b -> p (a b)"), scalar=sm[:, 2:3], in1=pg[:, which, :], op0=ALU.mult, op1=ALU.mult), reads=[b_psY, b_sm, b_pg], writes=[b_tmp])
                ph.op(ph.dve, lambda h: h.tensor_tensor(out=ht[:], in0=tmp[:], in1=ht[:], op=ALU.add), reads=[b_tmp], writes=[bh])

            ld(0)
            for i in range(nb):
                if i + 1 < nb:
                    ld(i + 1)
                ht, bh, mx, bm = hts[i % 2], b_ht[i % 2], mxs[i % 2], b_mx[i % 2]
                for n in range(4):
                    for c in range(NDC):
                        ph.op(ph.pe, lambda h, n=n, c=c, mx=mx: h.matmul(psY[:, n, :], lhsT=mx[:, c, :], rhs=wout[:, c, n * 512:(n + 1) * 512], start=(c == 0), stop=(c == NDC - 1)),
                              reads=[bm, b_wout], writes=[b_psY], pe_acc=True)
                post(0, ht, bh)
                self.rms_tile(ph, ht[:], bh, junk[:], b_junk, sm[:, 3:4], sm[:, 4:5], sm[:, 5:6], b_sm, D)
                ph.op(ph.dve, lambda h, ht=ht: h.tensor_scalar(out=xn[:], in0=ht[:], scalar1=sm[:, 5:6], scalar2=None, op0=ALU.mult), reads=[bh, b_sm], writes=[b_xn])
                for dc in range(NDC):
                    ph.op(ph.pe, lambda h, dc=dc: h.transpose(out=psB[:, dc // 8, (dc % 8) * 128:(dc % 8 + 1) * 128], in_=xn[:, dc * 128:(dc + 1) * 128], identity=ident[:]),
                          reads=[b_xn, b_ident], writes=[b_psB[dc // 8]], pe_acc=True)
                for k in range(2):
                    ph.op(ph.act, lambda h, k=k: h.copy(out=xnT[:, k * 8:(k + 1) * 8, :], in_=psB[:, k, :]), reads=[b_psB[k]], writes=[b_xnT])
                for hh in range(4):
                    for dc in range(NDC):
                        ph.op(ph.pe, lambda h, hh=hh, dc=dc: h.matmul(psX[:, 0, hh * 128:(hh + 1) * 128], lhsT=xwq[:, dc, hh * 128:(hh + 1) * 128], rhs=xnT[:, dc, :], start=(dc == 0), stop=(dc == NDC - 1)),
                              reads=[b_xwq, b_xnT], writes=[b_psX], pe_acc=True)
                ph.op(ph.act, lambda h: h.copy(out=qx[:], in_=psX[:, 0, :]), reads=[b_psX], writes=[b_qx])
                for hh in range(4):
                    for mc in range(2):
                        u = hh * 2 + mc
                        ph.op(ph.pe, lambda h, hh=hh, mc=mc, u=u: h.matmul(psX[:, u // 4, (u % 4) * 128:(u % 4 + 1) * 128], lhsT=kmT[:, hh, mc * 128:(mc + 1) * 128], rhs=qx[:, hh, :], start=True, stop=True),
                              reads=[b_kmT, b_qx], writes=[b_psX], pe_acc=True)
                ph.op(ph.act, lambda h: h.activation(out=pl[:], in_=psX[:].rearrange("p a b -> p (a b)"), func=AF.Exp, scale=xscale), reads=[b_psX], writes=[b_pl])
                for hh in range(4):
                    for mc in range(2):
                        ph.op(ph.pe, lambda h, hh=hh, mc=mc: h.matmul(psX[:, hh // 2, (hh % 2) * 129:(hh % 2) * 129 + 129], lhsT=pl[:, hh * 2 + mc, :], rhs=vm1[:, mc, hh, :], start=(hh % 2 == 0 and mc == 0), stop=(mc == 1), skip_group_check=True),
                              reads=[b_pl, b_vm1], writes=[b_psX], pe_acc=True)
                for hh in range(4):
                    o = (hh % 2) * 129
                    ph.op(ph.dve, lambda h, hh=hh, o=o: h.reciprocal(out=sm[:, 8 + hh:9 + hh], in_=psX[:, hh // 2, o + 128:o + 129]), reads=[b_psX], writes=[b_sm])
                    ph.op(ph.dve, lambda h, hh=hh, o=o: h.tensor_scalar(out=on[:, hh, :], in0=psX[:, hh // 2, o:o + 128], scalar1=sm[:, 8 + hh:9 + hh], scalar2=None, op0=ALU.mult), reads=[b_psX, b_sm], writes=[b_on])
                for hh in range(4):
                    ph.op(ph.pe, lambda h, hh=hh: h.transpose(out=psB[:, 0, hh * 128:(hh + 1) * 128], in_=on[:, hh, :], identity=ident[:]), reads=[b_on, b_ident], writes=[b_psB[0]], pe_acc=True)
                ph.op(ph.act, lambda h: h.copy(out=oT[:], in_=psB[:, 0, 0:512]), reads=[b_psB[0]], writes=[b_oT])
                for n in range(4):
                    for hh in range(4):
                        ph.op(ph.pe, lambda h, n=n, hh=hh: h.matmul(psY[:, n, :], lhsT=oT[:, hh, :], rhs=xwo[:, hh, n * 512:(n + 1) * 512], start=(hh == 0), stop=(hh == 3)),
                              reads=[b_oT, b_xwo], writes=[b_psY], pe_acc=True)
                post(1, ht, bh)
                ph.store(self.h[i * 128:(i + 1) * 128, :], ht[:], bh)
            ph.emit(st)


    def ffn(self, l):
        nc, S = self.nc, self.S
        TG = min(1024, S)
        ngr = S // TG
        nt = TG // 128
        nh = TG // 512
        with ExitStack() as st:
            ph, sb, pst, cs = self.begin(f"ffn{l}", st)
            ident, b_ident = cs["ident"]
            gB = sb("gB", [128, D], F32); b_gB = Buf()
            ph.load(gB[:], self.E("vecB")[l, :, VB_FPRE:VB_FPRE + D], b_gB)
            xnT = sb("xnT", [128, NDC, TG], BF16); b_xnT = Buf()
            actT = sb("actT", [128, NFC, TG], BF16); b_actT = Buf()
            gus = [sb(f"gus{i}", [128, 2, NDC, 128], F32) for i in range(2)]; b_gus = [Buf(), Buf()]
            gub = [sb(f"gub{i}", [128, 2, NDC, 128], BF16) for i in range(2)]; b_gub = [Buf(), Buf()]
            NWD = 6
            wds = [sb(f"wds{i}", [128, 256], F32) for i in range(NWD)]; b_wds = [Buf() for _ in range(NWD)]
            wdb = [sb(f"wdb{i}", [128, 256], BF16) for i in range(NWD)]; b_wdb = [Buf() for _ in range(NWD)]
            ht = sb("ht", [128, D], F32); b_ht = Buf()
            xn = sb("xn", [128, D], BF16); b_xn = Buf()
            sgt = sb("sgt", [128, 512], F32); b_sgt = Buf()
            yst = sb("yst", [128, nt, 256], F32); b_yst = Buf()
            sm = sb("sm", [128, 4], F32); b_sm = Buf()
            psB = pst("psB", [128, 1024], BF16); b_psB = Buf()
            psGU = pst("psGU", [128, 3, 512], F32); b_psGU = [Buf() for _ in range(3)]
            psD = pst("psD", [128, 4, 512], F32); b_psD = [Buf() for _ in range(4)]
            hsrc = self.h
            cgu = [0]
            cwd = [0]
            cbank = [0]
            for gr in range(ngr):
                hring = [(ht[:], b_ht), (yst[:].rearrange("p t c -> p (t c)"), b_yst)]

                def lda(t, gr=gr):
                    r0 = gr * TG + t * 128
                    ph.load(hring[t % 2][0], hsrc[r0:r0 + 128, :], hring[t % 2][1])

                lda(0)
                for t in range(nt):
                    if t + 1 < nt:
                        lda(t + 1)
                    hta, bha = hring[t % 2]
                    self.rms_tile(ph, hta, bha, xn[:], b_xn, sm[:, 0:1], sm[:, 1:2], sm[:, 2:3], b_sm, D)
                    ph.op(ph.dve, lambda h, hta=hta: h.scalar_tensor_tensor(out=xn[:], in0=hta, scalar=sm[:, 2:3], in1=gB[:], op0=ALU.mult, op1=ALU.mult), reads=[bha, b_sm, b_gB], writes=[b_xn])
                    for k in range(2):
                        for dd in range(8):
                            dc = k * 8 + dd
                            ph.op(ph.pe, lambda h, dd=dd, dc=dc: h.transpose(out=psB[:, dd * 128:(dd + 1) * 128], in_=xn[:, dc * 128:(dc + 1) * 128], identity=ident[:]),
                                  reads=[b_xn, b_ident], writes=[b_psB], pe_acc=True)
                        ph.op(ph.act, lambda h, k=k, t=t: h.copy(out=xnT[:, k * 8:(k + 1) * 8, t * 128:(t + 1) * 128], in_=psB[:].rearrange("p (a b) -> p a b", b=128)), reads=[b_psB], writes=[b_xnT])
                for fc in range(NFC):
                    s_ = cgu[0] % 2; cgu[0] += 1
                    for c in range(2):
                        ph.dma(gus[s_][:, c, :, :], self.E("f_w_gu")[l, :, c, fc * 128:(fc + 1) * 128].rearrange("(dc p) f -> p dc f", p=128), writes=[b_gus[s_]], sbuf=b_gus[s_])
                    ph.op(ph.pool, lambda h, s_=s_: h.tensor_copy(out=gub[s_][:], in_=gus[s_][:]), reads=[b_gus[s_]], writes=[b_gub[s_]])
                    for hf in range(nh):
                        bks = []
                        for c in range(2):
                            bk = cbank[0] % 3; cbank[0] += 1
                            bks.append(bk)
                            for dc in range(NDC):
                                ph.op(ph.pe, lambda h, bk=bk, c=c, dc=dc, s_=s_, hf=hf: h.matmul(psGU[:, bk, :], lhsT=gub[s_][:, c, dc, :], rhs=xnT[:, dc, hf * 512:(hf + 1) * 512], start=(dc == 0), stop=(dc == NDC - 1)),
                                      reads=[b_gub[s_], b_xnT], writes=[b_psGU[bk]], pe_acc=True)
                        ph.op(ph.act, lambda h, bk=bks[0]: h.activation(out=sgt[:], in_=psGU[:, bk, :], func=AF.Silu), reads=[b_psGU[bks[0]]], writes=[b_sgt])
                        ph.op(ph.dve, lambda h, bk=bks[1], fc=fc, hf=hf: h.tensor_tensor(out=actT[:, fc, hf * 512:(hf + 1) * 512], in0=sgt[:], in1=psGU[:, bk, :], op=ALU.mult), reads=[b_sgt, b_psGU[bks[1]]], writes=[b_actT])
                for dcol in range(8):
                    for fc in range(NFC):
                        w_ = cwd[0] % NWD; cwd[0] += 1
                        ph.load(wds[w_][:], self.E("f_w_down")[l, fc * 128:(fc + 1) * 128, dcol * 256:(dcol + 1) * 256], b_wds[w_])
                        if cwd[0] % 2 == 0:
                            ph.op(ph.act, lambda h, w_=w_: h.copy(out=wdb[w_][:], in_=wds[w_][:]), reads=[b_wds[w_]], writes=[b_wdb[w_]])
                        else:
                            ph.op(ph.dve, lambda h, w_=w_: h.tensor_copy(out=wdb[w_][:], in_=wds[w_][:]), reads=[b_wds[w_]], writes=[b_wdb[w_]])
                        for t in range(nt):
                            ph.op(ph.pe, lambda h, t=t, fc=fc, w_=w_: h.matmul(psD[:, t // 2, (t % 2) * 256:(t % 2) * 256 + 256], lhsT=actT[:, fc, t * 128:(t + 1) * 128], rhs=wdb[w_][:], start=(fc == 0 and t % 2 == 0), stop=(fc == NFC - 1), skip_group_check=True),
                                  reads=[b_actT, b_wdb[w_]], writes=[b_psD[t // 2]], pe_acc=True)
                    for b in range(nt // 2):
                        if b % 2 == 0:
                            ph.op(ph.dve, lambda h, b=b: h.tensor_copy(out=yst[:, 2 * b:2 * b + 2, :], in_=psD[:, b, :].rearrange("p (a c) -> p a c", c=256)), reads=[b_psD[b]], writes=[b_yst])
                        else:
                            ph.op(ph.act, lambda h, b=b: h.copy(out=yst[:, 2 * b:2 * b + 2, :], in_=psD[:, b, :].rearrange("p (a c) -> p a c", c=256)), reads=[b_psD[b]], writes=[b_yst])
                    ph.store(self.yscr[gr * TG:(gr + 1) * TG, dcol * 256:(dcol + 1) * 256].rearrange("(t p) c -> p t c", p=128), yst[:], b_yst)
            ph.emit(st)

    def ffn_post(self, l, dst):
        nc, S, nb = self.nc, self.S, self.nb
        with ExitStack() as st:
            ph, sb, pst, cs = self.begin(f"fpo{l}", st, want=())
            pg = sb("pg", [128, D], F32); b_pg = Buf()
            ph.load(pg[:], self.E("vecB")[l, :, VB_FPOST:VB_FPOST + D], b_pg)
            hts = [sb(f"ht{i}", [128, D], F32) for i in range(2)]; b_ht = [Buf(), Buf()]
            yts = [sb(f"yt{i}", [128, D], F32) for i in range(2)]; b_yt = [Buf(), Buf()]
            junk = sb("junk", [128, D], BF16); b_junk = Buf()
            sm = sb("sm", [128, 4], F32); b_sm = Buf()

            def ld(i):
                tk = slice(i * 128, (i + 1) * 128)
                ph.load(hts[i % 2][:], self.h[tk, :], b_ht[i % 2])
                ph.load(yts[i % 2][:], self.yscr[tk, :], b_yt[i % 2])

            ld(0)
            for i in range(nb):
                if i + 1 < nb:
                    ld(i + 1)
                ht, bh, yt, by = hts[i % 2], b_ht[i % 2], yts[i % 2], b_yt[i % 2]
                self.rms_tile(ph, yt[:], by, junk[:], b_junk, sm[:, 0:1], sm[:, 1:2], sm[:, 2:3], b_sm, D)
                ph.op(ph.dve, lambda h, yt=yt: h.scalar_tensor_tensor(out=yt[:], in0=yt[:], scalar=sm[:, 2:3], in1=pg[:], op0=ALU.mult, op1=ALU.mult), reads=[b_sm, b_pg], writes=[by])
                ph.op(ph.pool, lambda h, yt=yt, ht=ht: h.tensor_tensor(out=ht[:], in0=yt[:], in1=ht[:], op=ALU.add), reads=[by], writes=[bh])
                ph.store(dst[i * 128:(i + 1) * 128, :], ht[:], bh)
            ph.emit(st)


def rel_bucket_np(dist):
    n = np.maximum(dist, 0)
    nf = np.maximum(n, 1).astype(np.float32)
    large = 16 + (np.log(nf / np.float32(16)) / np.float32(math.log(128 / 16)) * np.float32(16)).astype(np.int32)
    large = np.minimum(large, 31)
    return np.where(n < 16, n, large)


def host_consts():
    c = np.zeros((128, 128 * 4 + 32), np.float32)
    c[:, 0:128] = np.eye(128, dtype=np.float32)
    s = np.arange(128)[:, None]
    t = np.arange(128)[None, :]
    c[:, 128:256] = (s <= t).astype(np.float32)
    c[:, 256:384] = np.where(t <= s, 0.0, -1e30)
    c[:, 384:512] = np.where(s <= t, 0.0, NEG)
    c[:, 512:544] = (0.5 ** (np.arange(32) + 1))[None, :]
    return c


def prep_inputs(inp, S, L):
    f = lambda a: np.ascontiguousarray(np.asarray(a, dtype=np.float32)[:L])
    shared = {}
    shared["w_in"] = f(inp["w_in"])
    shared["w_out"] = f(inp["w_out"])
    shared["b_w_uq"] = f(inp["b_w_uq"]).reshape(L, 512, 768)
    shared["b_w_qidx"] = f(inp["b_w_qidx"]).reshape(L, 512, 1024)
    shared["b_w_uk"] = f(inp["b_w_uk"]).reshape(L, 256, 768)
    shared["b_w_uv"] = f(inp["b_w_uv"]).reshape(L, 256, 768)
    shared["cwsT"] = f(np.transpose(np.asarray(inp["c_w_s"], dtype=np.float32)[:L], (0, 3, 1, 2))).reshape(L, 128, 512)
    shared["x_wq"] = f(inp["x_wq"]).reshape(L, D, 512)
    shared["x_wkv"] = f(inp["x_wkv"]).reshape(L, D, 1024)
    shared["x_wo"] = f(inp["x_wo"]).reshape(L, 512, D)
    shared["f_w_gu"] = f(inp["f_w_gu"])
    shared["f_w_down"] = f(inp["f_w_down"])
    vp = np.zeros((L, 128, NVP), np.float32)
    vb = np.zeros((L, 128, NVB), np.float32)
    pp = lambda v, n: np.asarray(v, np.float32).reshape(n, 128).T
    for l in range(L):
        vp[l, :, VP_GIN:VP_GIN + 16] = pp(inp["mix_pre_g"][l], 16)
        vp[l, :, VP_CQ:VP_CQ + 4] = pp(inp["b_cq_g"][l], 4)
        vp[l, :, VP_CKV:VP_CKV + 2] = pp(inp["b_ckv_g"][l], 2)
        vp[l, :, VP_XPRE:VP_XPRE + 16] = pp(inp["x_pre_g"][l], 16)
        vp[l, :, VP_MEMG:VP_MEMG + 16] = pp(inp["mem_g"][l], 16)
        vp[l, :, VP_FPRE:VP_FPRE + 16] = pp(inp["f_pre_g"][l], 16)
        vp[l, :, VP_SUBG] = np.asarray(inp["a_sub_g"][l], np.float32)
        vb[l, :, VB_MIXPOST:VB_MIXPOST + D] = np.asarray(inp["mix_post_g"][l])[None, :]
        vb[l, :, VB_XPOST:VB_XPOST + D] = np.asarray(inp["x_post_g"][l])[None, :]
        vb[l, :, VB_FPOST:VB_FPOST + D] = np.asarray(inp["f_post_g"][l])[None, :]
        vb[l, :, VB_LNG:VB_LNG + 512] = np.asarray(inp["c_ln_g"][l])[None, :]
        vb[l, :, VB_LNB:VB_LNB + 512] = np.asarray(inp["c_ln_b"][l])[None, :]
        vb[l, :, VB_BS:VB_BS + 512] = np.asarray(inp["c_b_s"][l]).reshape(1, 512)
        vb[l, :, VB_LAM:VB_LAM + 256] = np.asarray(inp["a_lambda"][l]).reshape(1, 256)
        vb[l, :, VB_FPRE:VB_FPRE + D] = np.asarray(inp["f_pre_g"][l])[None, :]
    shared["vecP"] = vp
    shared["vecB"] = vb
    rb = np.asarray(inp["rel_bias"], np.float32)
    s = np.arange(128)[:, None]
    q = np.arange(128)[None, :]
    bt = np.zeros((128, 12, 2, 128), np.float32)
    for kind in range(2):
        idx = rel_bucket_np(128 * kind + q - s)
        bt[:, :, kind, :] = np.transpose(rb[idx], (0, 2, 1))
    shared["biasT"] = bt.reshape(128, 12 * 2 * 128)
    shared["b31"] = np.ascontiguousarray(np.broadcast_to(rb[31][None, :], (128, 12)))
    shared["consts"] = host_consts()
    return shared


_CACHE = {}


def kernel(**inputs):
    x = np.asarray(inputs["x"], np.float32)
    B, S, _ = x.shape
    L = int(np.asarray(inputs["w_in"]).shape[0])
    key = (S, L)
    if key not in _CACHE:
        _CACHE[key] = Kern(S, L).build()
    nc = _CACHE[key]
    shared = prep_inputs(inputs, S, L)
    mem = np.asarray(inputs["mem"], np.float32)
    ncore = B
    in_maps = []
    for c in range(ncore):
        m = dict(shared)
        m["x"] = np.ascontiguousarray(x[c])
        m["mem"] = np.ascontiguousarray(mem[c])
        in_maps.append(m)
    res = run_bass_kernel_spmd(nc, in_maps, core_ids=list(range(ncore)))
    out = np.stack([np.asarray(res.results[b]["out"], np.float32) for b in range(B)], axis=0)
    return out
```

```python
import math
from contextlib import ExitStack
import numpy as np
import concourse.bass as bass
import concourse.mybir as mybir
from concourse.bass_utils import run_bass_kernel_spmd

F32 = mybir.dt.float32
BF16 = mybir.dt.bfloat16
AF = mybir.ActivationFunctionType
ALU = mybir.AluOpType
AX = mybir.AxisListType

D = 2048
NDC = 16
IN_COLS = 4176
FFN = 5632
NFC = 44
EPS = 1e-6
TOPK = 256
NBIS = 14
NEG = -30000.0

VP_GIN, VP_CQ, VP_CKV, VP_XPRE, VP_MEMG, VP_FPRE, VP_SUBG = 0, 16, 20, 22, 38, 54, 70
NVP = 71
VB_MIXPOST, VB_XPOST, VB_FPOST, VB_LNG, VB_LNB, VB_BS, VB_LAM = 0, 2048, 4096, 6144, 6656, 7168, 7680
VB_FPRE = 7936
NVB = 9984


class Buf:
    __slots__ = ("w", "r", "dsem")

    def __init__(self):
        self.w = None
        self.r = {}
        self.dsem = None


class _Sem:
    def __init__(self):
        self.sem = None
        self.n = 0


class Eng:
    def __init__(self, name):
        self.name = name
        self.sem = None
        self.n = 0
        self.prog = []
        self.waited = {}

    def wait(self, tok):
        src, val = tok
        if self.waited.get(id(src), 0) >= val:
            return
        self.waited[id(src)] = val
        self.prog.append(lambda h, src=src, val=val: h.wait_ge(src.sem, val))


class Phase:
    def __init__(self, kern, name):
        self.kern = kern
        self.nc = kern.nc
        self.name = name
        self.pe, self.dve, self.act, self.pool, self.sp = kern.engs
        self.engs = kern.engs
        for e in self.engs:
            e.prog = []
        self.dsems = []
        self.npool = 0

    @staticmethod
    def _deps(reads, writes):
        deps = []
        for b in reads:
            if b.w is not None:
                deps.append(b.w)
        for b in writes:
            if b.w is not None:
                deps.append(b.w)
            deps.extend(b.r.values())
        return deps

    @staticmethod
    def _commit(tok, reads, writes):
        k = id(tok[0])
        for b in reads:
            b.r[k] = tok
        for b in writes:
            b.w = tok
            b.r = {}

    def op(self, eng, fn, reads=(), writes=(), pe_acc=False):
        for d in self._deps(reads, writes):
            if pe_acc and d[0] is eng:
                continue
            eng.wait(d)
        eng.n += 1
        tok = (eng, eng.n)
        eng.prog.append(lambda h, fn=fn, eng=eng: fn(h).then_inc(eng.sem, 1))
        self._commit(tok, reads, writes)
        return tok

    def dma(self, out, in_, reads=(), writes=(), sbuf=None, **kw):
        eng = self.sp
        for d in self._deps(reads, writes):
            eng.wait(d)
        if sbuf.dsem is None:
            sbuf.dsem = self.kern.get_dsem(self.npool)
            self.npool += 1
            self.dsems.append(sbuf.dsem)
        ds = sbuf.dsem
        ds.n += 16
        tok = (ds, ds.n)
        eng.prog.append(lambda h, out=out, in_=in_, kw=kw, ds=ds: h.dma_start(out=out, in_=in_, **kw).then_inc(ds.sem, 16))
        self._commit(tok, reads, writes)
        return tok

    def load(self, out, in_, buf, **kw):
        return self.dma(out, in_, writes=[buf], sbuf=buf, **kw)

    def store(self, out, in_, buf, **kw):
        return self.dma(out, in_, reads=[buf], sbuf=buf, **kw)

    def emit(self, st):
        nc = self.nc
        for ds in self.dsems:
            self.sp.wait((ds, ds.n))
        block = st.enter_context(nc.Block(self.name))
        for e in self.engs:
            def body(h, e=e):
                for c in e.prog:
                    c(h)
            getattr(block, e.name)(body)


class Kern:
    def __init__(self, S, L, debug=False):
        self.S, self.L, self.debug = S, L, debug
        self.nb = S // 128
        self.ng = S // 512
        nc = self.nc = bass.Bass("TRN2", target_bir_lowering=False)
        self.gst = ExitStack()
        self.engs = [Eng(n) for n in ("tensor", "vector", "scalar", "gpsimd", "sync")]
        for e in self.engs:
            e.sem = self.gst.enter_context(nc.semaphore(f"prog_{e.name}"))
        self.dpool = []

        self._ext_shapes = {
            "x": [S, D], "mem": [256, D], "w_in": [L, D, IN_COLS], "w_out": [L, D, D],
            "b_w_uq": [L, 512, 768], "b_w_qidx": [L, 512, 1024], "b_w_uk": [L, 256, 768], "b_w_uv": [L, 256, 768],
            "cwsT": [L, 128, 512], "x_wq": [L, D, 512], "x_wkv": [L, D, 1024], "x_wo": [L, 512, D],
            "f_w_gu": [L, D, 2, FFN], "f_w_down": [L, FFN, D], "vecP": [L, 128, NVP], "vecB": [L, 128, NVB],
            "biasT": [128, 12 * 2 * 128], "b31": [128, 12], "consts": [128, 128 * 4 + 32],
        }
        self._ext = {}
        self.out = nc.dram_tensor("out", [S, D], F32, kind="ExternalOutput").ap()

        def scr(name, shape, dt):
            kind = "ExternalOutput" if debug else "Internal"
            return nc.dram_tensor(name, list(shape), dt, kind=kind).ap()

        self.h = scr("h_scr", [S, D], F32)
        self.qaT = scr("qaT", [6, 128, S], BF16)
        self.kaT = scr("kaT", [6, 128, S], BF16)
        self.va = scr("va", [S, 768], BF16)
        self.qbT = scr("qbT", [6, 128, S], BF16)
        self.kbT = scr("kbT", [6, 128, S], BF16)
        self.vb = scr("vb", [S, 768], BF16)
        self.qidxT = scr("qidxT", [8, 128, S], BF16)
        self.kidxT = scr("kidxT", [64, S], BF16)
        self.widx = scr("widx", [S, 16], F32)
        self.mixT = scr("mixT", [16, 128, S], BF16)
        self.MTd = scr("MTd", [self.ng, 128, self.nb, 512], BF16)
        self.yscr = scr("yscr", [S, D], F32)
        self.kmT = scr("kmT", [128, 4, 256], BF16)
        self.vm = scr("vm", [256, 512], BF16)

    def get_dsem(self, i):
        while len(self.dpool) <= i:
            d = _Sem()
            d.sem = self.gst.enter_context(self.nc.semaphore(f"dma_{len(self.dpool)}"))
            self.dpool.append(d)
        return self.dpool[i]

    def E(self, name):
        if name not in self._ext:
            self._ext[name] = self.nc.dram_tensor(name, list(self._ext_shapes[name]), F32, kind="ExternalInput").ap()
        return self._ext[name]

    def build(self, phases=None):
        want = lambda p: phases is None or p in phases
        for l in range(self.L):
            src = self.E("x") if l == 0 else self.h
            if want("p1a"):
                self.p1a(l, src)
            if want("p1b"):
                self.p1b(l, src)
            if want("attA"):
                self.attn(l, "A")
            if want("bidx"):
                self.bindex(l)
            if want("attB"):
                self.attn(l, "B")
            if want("p3"):
                self.memkv(l)
                self.p3a(l)
                self.p3b(l)
            if want("ffn"):
                self.ffn(l)
                self.ffn_post(l, self.out if l == self.L - 1 else self.h)
        self.gst.close()
        return self.nc

    def begin(self, name, st, want=("ident",)):
        nc = self.nc
        ph = Phase(self, name)
        sb = lambda nm, shape, dt: st.enter_context(nc.sbuf_tensor(f"{name}_{nm}", list(shape), dt))
        pst = lambda nm, shape, dt: st.enter_context(nc.psum_tensor(f"{name}_{nm}", list(shape), dt))
        cs = self.consts_load(ph, sb, want)
        epsT = sb("epsT", [128, 1], F32); b_eps = Buf()
        ph.op(ph.dve, lambda h: h.memset(epsT[:], EPS), writes=[b_eps])
        self.eps_ap = epsT[:, 0:1]
        self.b_eps = b_eps
        return ph, sb, pst, cs

    def consts_load(self, ph, sb, want):
        res = {}
        if "ident" in want:
            cf = sb("c_idf", [128, 128], F32); bcf = Buf()
            ph.load(cf[:], self.E("consts")[:, 0:128], bcf)
            ib = sb("c_idb", [128, 128], BF16); bib = Buf()
            ph.op(ph.dve, lambda h: h.tensor_copy(out=ib[:], in_=cf[:]), reads=[bcf], writes=[bib])
            res["ident"] = (ib, bib)
        for nm, c0, n in (("mask01T", 128, 128), ("causneg", 256, 128), ("causnegT", 384, 128), ("pow2", 512, 32)):
            if nm in want:
                t = sb("c_" + nm, [128, n], F32); bt = Buf()
                ph.load(t[:], self.E("consts")[:, c0:c0 + n], bt)
                res[nm] = (t, bt)
        return res

    def rms_tile(self, ph, ht, b_ht, junk, b_junk, ss, rt, rinv, b_small, width, act_in=None):
        src = ht if act_in is None else act_in
        ph.op(ph.act, lambda h: h.activation(out=junk, in_=src, func=AF.Square, accum_out=ss), reads=[b_ht], writes=[b_junk, b_small])
        ph.op(ph.act, lambda h: h.activation(out=rt, in_=ss, func=AF.Sqrt, scale=1.0 / width, bias=self.eps_ap), reads=[b_small, self.b_eps], writes=[b_small])
        ph.op(ph.dve, lambda h: h.reciprocal(out=rinv, in_=rt), reads=[b_small], writes=[b_small])

    def p1a(self, l, src):
        nc, S, nb = self.nc, self.S, self.nb
        with ExitStack() as st:
            ph, sb, pst, cs = self.begin(f"p1a{l}", st)
            ident, b_ident = cs["ident"]
            vp = sb("vp", [128, NVP], F32); b_vp = Buf()
            ph.load(vp[:], self.E("vecP")[l], b_vp)
            NW = 2304
            win = sb("win", [128, NDC, NW], BF16); b_win = Buf()
            stg = [sb(f"stg{i}", [128, NW], F32) for i in range(2)]; b_stg = [Buf(), Buf()]
            for dc in range(NDC):
                s = dc % 2
                ph.load(stg[s][:], self.E("w_in")[l, dc * 128:(dc + 1) * 128, 0:NW], b_stg[s])
                ph.op(ph.dve, lambda h, s=s, dc=dc: h.tensor_scalar(out=win[:, dc, :], in0=stg[s][:], scalar1=vp[:, VP_GIN + dc:VP_GIN + dc + 1], scalar2=None, op0=ALU.mult),
                      reads=[b_stg[s], b_vp], writes=[b_win])
            hts = [sb(f"ht{i}", [128, D], F32) for i in range(2)]; b_ht = [Buf(), Buf()]
            junk = sb("junk", [128, D], BF16); b_junk = Buf()
            sm = sb("sm", [128, 4], F32); b_sm = Buf()
            xn = sb("xn", [128, D], BF16); b_xn = Buf()
            xnT = sb("xnT", [128, NDC, 128], BF16); b_xnT = Buf()
            fm = sb("fm", [128, 12, 128], BF16); b_fm = Buf()
            vas = sb("vas", [128, 768], BF16); b_vas = Buf()
            psB = pst("psB", [128, 2, 1024], BF16); b_psB = [Buf(), Buf()]
            psF = pst("psF", [128, 5, 512], F32); b_psF = [Buf() for _ in range(5)]

            def ld(i):
                ph.load(hts[i % 2][:], src[i * 128:(i + 1) * 128, :], b_ht[i % 2])

            ld(0)
            for i in range(nb):
                if i + 1 < nb:
                    ld(i + 1)
                ht, bh = hts[i % 2], b_ht[i % 2]
                self.rms_tile(ph, ht[:], bh, junk[:], b_junk, sm[:, 0:1], sm[:, 1:2], sm[:, 2:3], b_sm, D)
                ph.op(ph.dve, lambda h, ht=ht: h.tensor_scalar(out=xn[:], in0=ht[:], scalar1=sm[:, 2:3], scalar2=None, op0=ALU.mult),
                      reads=[bh, b_sm], writes=[b_xn])
                for dc in range(NDC):
                    ph.op(ph.pe, lambda h, dc=dc: h.transpose(out=psB[:, dc // 8, (dc % 8) * 128:(dc % 8 + 1) * 128], in_=xn[:, dc * 128:(dc + 1) * 128], identity=ident[:]),
                          reads=[b_xn, b_ident], writes=[b_psB[dc // 8]], pe_acc=True)
                for k in range(2):
                    ph.op(ph.act, lambda h, k=k: h.copy(out=xnT[:, k * 8:(k + 1) * 8, :], in_=psB[:, k, :]), reads=[b_psB[k]], writes=[b_xnT])
                for c in range(12):
                    bk = c // 4
                    for dc in range(NDC):
                        ph.op(ph.pe, lambda h, c=c, dc=dc, bk=bk: h.matmul(psF[:, bk, (c % 4) * 128:(c % 4 + 1) * 128], lhsT=win[:, dc, c * 128:(c + 1) * 128], rhs=xnT[:, dc, :], start=(dc == 0), stop=(dc == NDC - 1)),
                              reads=[b_win, b_xnT], writes=[b_psF[bk]], pe_acc=True)
                for (bk, c0, n) in ((3, 0, 512), (4, 512, 256)):
                    for dc in range(NDC):
                        ph.op(ph.pe, lambda h, bk=bk, c0=c0, n=n, dc=dc: h.matmul(psF[:, bk, 0:n], lhsT=xnT[:, dc, :], rhs=win[:, dc, 1536 + c0:1536 + c0 + n], start=(dc == 0), stop=(dc == NDC - 1)),
                              reads=[b_win, b_xnT], writes=[b_psF[bk]], pe_acc=True)
                for bk in range(3):
                    eng = ph.act if bk % 2 == 0 else ph.dve
                    if eng is ph.act:
                        ph.op(eng, lambda h, bk=bk: h.copy(out=fm[:, bk * 4:(bk + 1) * 4, :], in_=psF[:, bk, :]), reads=[b_psF[bk]], writes=[b_fm])
                    else:
                        ph.op(eng, lambda h, bk=bk: h.tensor_copy(out=fm[:, bk * 4:(bk + 1) * 4, :], in_=psF[:, bk, :]), reads=[b_psF[bk]], writes=[b_fm])
                ph.op(ph.dve, lambda h: h.tensor_copy(out=vas[:, 0:512], in_=psF[:, 3, :]), reads=[b_psF[3]], writes=[b_vas])
                ph.op(ph.act, lambda h: h.copy(out=vas[:, 512:768], in_=psF[:, 4, 0:256]), reads=[b_psF[4]], writes=[b_vas])
                tk = slice(i * 128, (i + 1) * 128)
                ph.store(self.qaT[:, :, tk].rearrange("h p t -> p h t"), fm[:, 0:6, :], b_fm)
                ph.store(self.kaT[:, :, tk].rearrange("h p t -> p h t"), fm[:, 6:12, :], b_fm)
                ph.store(self.va[tk, :], vas[:], b_vas)
            ph.emit(st)

    def p1b(self, l, src):
        nc, S, nb = self.nc, self.S, self.nb
        with ExitStack() as st:
            ph, sb, pst, cs = self.begin(f"p1b{l}", st, want=("ident", "mask01T"))
            ident, b_ident = cs["ident"]
            m01, b_m01 = cs["mask01T"]
            vp = sb("vp", [128, NVP], F32); b_vp = Buf()
            ph.load(vp[:], self.E("vecP")[l], b_vp)
            vb3 = sb("vb3", [128, 1536], F32); b_vb3 = Buf()
            ph.load(vb3[:], self.E("vecB")[l, :, VB_LNG:VB_LNG + 1536], b_vb3)
            NW = 1872
            C0 = 2304
            win = sb("win", [128, NDC, NW], BF16); b_win = Buf()
            wuq = sb("wuq", [128, 4, 768], BF16); b_wuq = Buf()
            wqi = sb("wqi", [128, 4, 1024], BF16); b_wqi = Buf()
            wuk = sb("wuk", [128, 2, 768], BF16); b_wuk = Buf()
            wuv = sb("wuv", [128, 2, 768], BF16); b_wuv = Buf()
            cws = sb("cws", [128, 512], BF16); b_cws = Buf()
            stg = [sb(f"stg{i}", [128, NW], F32) for i in range(2)]; b_stg = [Buf(), Buf()]
            cnt = [0]

            def wload(dst, b_dst, srcap, n, gcol):
                s_ = cnt[0] % 2; cnt[0] += 1
                ph.load(stg[s_][:, 0:n], srcap, b_stg[s_])
                ph.op(ph.dve, lambda h: h.tensor_scalar(out=dst, in0=stg[s_][:, 0:n], scalar1=vp[:, gcol:gcol + 1], scalar2=None, op0=ALU.mult),
                      reads=[b_stg[s_], b_vp], writes=[b_dst])

            for dc in range(NDC):
                wload(win[:, dc, :], b_win, self.E("w_in")[l, dc * 128:(dc + 1) * 128, C0:C0 + NW], NW, VP_GIN + dc)
            for rc in range(4):
                wload(wuq[:, rc, :], b_wuq, self.E("b_w_uq")[l, rc * 128:(rc + 1) * 128, :], 768, VP_CQ + rc)
                wload(wqi[:, rc, :], b_wqi, self.E("b_w_qidx")[l, rc * 128:(rc + 1) * 128, :], 1024, VP_CQ + rc)
            for rc in range(2):
                wload(wuk[:, rc, :], b_wuk, self.E("b_w_uk")[l, rc * 128:(rc + 1) * 128, :], 768, VP_CKV + rc)
                wload(wuv[:, rc, :], b_wuv, self.E("b_w_uv")[l, rc * 128:(rc + 1) * 128, :], 768, VP_CKV + rc)
            s_ = cnt[0] % 2; cnt[0] += 1
            ph.load(stg[s_][:, 0:512], self.E("cwsT")[l], b_stg[s_])
            for g in range(4):
                ph.op(ph.dve, lambda h, g=g, s_=s_: h.tensor_tensor(out=cws[:, g * 128:(g + 1) * 128], in0=stg[s_][:, g * 128:(g + 1) * 128], in1=m01[:], op=ALU.mult),
                      reads=[b_stg[s_], b_m01], writes=[b_cws])

            hts = [sb(f"ht{i}", [128, D], F32) for i in range(2)]; b_ht = [Buf(), Buf()]
            junk = sb("junk", [128, D], BF16); b_junk = Buf()
            sm = sb("sm", [128, 16], F32); b_sm = Buf()
            xn = sb("xn", [128, D], BF16); b_xn = Buf()
            xnT = sb("xnT", [128, NDC, 128], BF16); b_xnT = Buf()
            cn = sb("cn", [128, 768], BF16); b_cn = Buf()
            cT = sb("cT", [128, 6, 128], BF16); b_cT = Buf()
            gv = sb("gv", [128, 512], F32); b_gv = Buf()
            tmp = sb("tmp", [128, 512], F32); b_tmp = Buf()
            vn = sb("vn", [128, 512], BF16); b_vn = Buf()
            uT = sb("uT", [128, 4, 128], F32); b_uT = Buf()
            fm = sb("fm", [128, 24, 128], BF16); b_fm = Buf()
            kix = sb("kix", [64, 128], BF16); b_kix = Buf()
            vbs = sb("vbs", [128, 768], BF16); b_vbs = Buf()
            wix = sb("wix", [128, 16], F32); b_wix = Buf()
            psB = pst("psB", [128, 2, 1024], BF16); b_psB = [Buf(), Buf()]
            psF = pst("psF", [128, 6, 512], F32); bF = [Buf() for _ in range(6)]
            WSC = (16 ** -0.5) * (64 ** -0.5)

            def ld(i):
                ph.load(hts[i % 2][:], src[i * 128:(i + 1) * 128, :], b_ht[i % 2])

            def mm(out, lhsT, rhs, first, last, reads, wbuf):
                ph.op(ph.pe, lambda h: h.matmul(out, lhsT=lhsT, rhs=rhs, start=first, stop=last), reads=reads, writes=[wbuf], pe_acc=True)

            ld(0)
            for i in range(nb):
                if i + 1 < nb:
                    ld(i + 1)
                ht, bh = hts[i % 2], b_ht[i % 2]
                self.rms_tile(ph, ht[:], bh, junk[:], b_junk, sm[:, 0:1], sm[:, 1:2], sm[:, 2:3], b_sm, D)
                ph.op(ph.dve, lambda h, ht=ht: h.tensor_scalar(out=xn[:], in0=ht[:], scalar1=sm[:, 2:3], scalar2=None, op0=ALU.mult),
                      reads=[bh, b_sm], writes=[b_xn])
                for dc in range(NDC):
                    ph.op(ph.pe, lambda h, dc=dc: h.transpose(out=psB[:, dc // 8, (dc % 8) * 128:(dc % 8 + 1) * 128], in_=xn[:, dc * 128:(dc + 1) * 128], identity=ident[:]),
                          reads=[b_xn, b_ident], writes=[b_psB[dc // 8]], pe_acc=True)
                for k in range(2):
                    ph.op(ph.act, lambda h, k=k: h.copy(out=xnT[:, k * 8:(k + 1) * 8, :], in_=psB[:, k, :]), reads=[b_psB[k]], writes=[b_xnT])
                for dc in range(NDC):
                    mm(psF[:, 0, :], xnT[:, dc, :], win[:, dc, 0:512], dc == 0, dc == NDC - 1, [b_win, b_xnT], bF[0])
                for dc in range(NDC):
                    mm(psF[:, 1, 0:336], xnT[:, dc, :], win[:, dc, 512:848], dc == 0, dc == NDC - 1, [b_win, b_xnT], bF[1])
                for dc in range(NDC):
                    mm(psF[:, 2, :], xnT[:, dc, :], win[:, dc, 1360:1872], dc == 0, dc == NDC - 1, [b_win, b_xnT], bF[2])
                for dc in range(NDC):
                    mm(psF[0:64, 3, 0:128], win[:, dc, 768:832], xnT[:, dc, :], dc == 0, dc == NDC - 1, [b_win, b_xnT], bF[3])
                for c in range(4):
                    bk, o = (3, 128 + c * 128) if c < 3 else (4, 0)
                    for dc in range(NDC):
                        mm(psF[:, bk, o:o + 128], win[:, dc, 848 + c * 128:848 + (c + 1) * 128], xnT[:, dc, :], dc == 0, dc == NDC - 1, [b_win, b_xnT], bF[bk])
                self.rms_tile(ph, None, bF[0], junk[:, 0:512], b_junk, sm[:, 3:4], sm[:, 4:5], sm[:, 5:6], b_sm, 512, act_in=psF[:, 0, :])
                ph.op(ph.dve, lambda h: h.tensor_scalar(out=cn[:, 0:512], in0=psF[:, 0, :], scalar1=sm[:, 5:6], scalar2=None, op0=ALU.mult), reads=[bF[0], b_sm], writes=[b_cn])
                self.rms_tile(ph, None, bF[1], junk[:, 0:256], b_junk, sm[:, 6:7], sm[:, 7:8], sm[:, 8:9], b_sm, 256, act_in=psF[:, 1, 0:256])
                ph.op(ph.dve, lambda h: h.tensor_scalar(out=cn[:, 512:768], in0=psF[:, 1, 0:256], scalar1=sm[:, 8:9], scalar2=None, op0=ALU.mult), reads=[bF[1], b_sm], writes=[b_cn])
                ph.op(ph.dve, lambda h: h.tensor_scalar(out=wix[:], in0=psF[:, 1, 320:336], scalar1=WSC, scalar2=None, op0=ALU.mult), reads=[bF[1]], writes=[b_wix])
                ph.op(ph.act, lambda h: h.copy(out=kix[:], in_=psF[0:64, 3, 0:128]), reads=[bF[3]], writes=[b_kix])
                ph.op(ph.act, lambda h: h.activation(out=uT[:, 0:3, :], in_=psF[:, 3, 128:512], func=AF.Gelu_apprx_tanh), reads=[bF[3]], writes=[b_uT])
                ph.op(ph.act, lambda h: h.activation(out=uT[:, 3, :], in_=psF[:, 4, 0:128], func=AF.Gelu_apprx_tanh), reads=[bF[4]], writes=[b_uT])
                ph.op(ph.act, lambda h: h.activation(out=gv[:], in_=psF[:, 2, :], func=AF.Gelu_apprx_tanh, accum_out=sm[:, 9:10]), reads=[bF[2]], writes=[b_gv, b_sm])
                ph.op(ph.dve, lambda h: h.tensor_scalar(out=sm[:, 10:11], in0=sm[:, 9:10], scalar1=-1.0 / 512, scalar2=None, op0=ALU.mult), reads=[b_sm], writes=[b_sm])
                ph.op(ph.act, lambda h: h.activation(out=junk[:, 0:512], in_=gv[:], func=AF.Square, bias=sm[:, 10:11], accum_out=sm[:, 11:12]), reads=[b_gv, b_sm], writes=[b_junk, b_sm])
                ph.op(ph.act, lambda h: h.activation(out=sm[:, 12:13], in_=sm[:, 11:12], func=AF.Sqrt, scale=1.0 / 512, bias=self.eps_ap), reads=[b_sm, self.b_eps], writes=[b_sm])
                ph.op(ph.dve, lambda h: h.reciprocal(out=sm[:, 13:14], in_=sm[:, 12:13]), reads=[b_sm], writes=[b_sm])
                ph.op(ph.dve, lambda h: h.tensor_scalar(out=tmp[:], in0=gv[:], scalar1=sm[:, 10:11], scalar2=sm[:, 13:14], op0=ALU.add, op1=ALU.mult), reads=[b_gv, b_sm], writes=[b_tmp])
                ph.op(ph.dve, lambda h: h.tensor_tensor(out=tmp[:], in0=tmp[:], in1=vb3[:, 0:512], op=ALU.mult), reads=[b_vb3], writes=[b_tmp])
                ph.op(ph.dve, lambda h: h.tensor_tensor(out=vn[:], in0=tmp[:], in1=vb3[:, 512:1024], op=ALU.add), reads=[b_tmp, b_vb3], writes=[b_vn])
                for k in range(6):
                    ph.op(ph.pe, lambda h, k=k: h.transpose(out=psB[:, 0, k * 128:(k + 1) * 128], in_=cn[:, k * 128:(k + 1) * 128], identity=ident[:]),
                          reads=[b_cn, b_ident], writes=[b_psB[0]], pe_acc=True)
                ph.op(ph.act, lambda h: h.copy(out=cT[:], in_=psB[:, 0, 0:768]), reads=[b_psB[0]], writes=[b_cT])
                def fm_group(bk, o, wt, b_wt, nrc, rc0, col0, reads_extra=()):
                    for rc in range(nrc):
                        mm(psF[:, bk, o:o + 128], wt[:, rc, col0:col0 + 128], cT[:, rc0 + rc, :], rc == 0, rc == nrc - 1, [b_wt, b_cT], bF[bk])
                for hh in range(4):
                    fm_group(0, hh * 128, wuq, b_wuq, 4, 0, hh * 128)
                for hh in range(4, 6):
                    fm_group(1, (hh - 4) * 128, wuq, b_wuq, 4, 0, hh * 128)
                for hh in range(2):
                    fm_group(1, 256 + hh * 128, wuk, b_wuk, 2, 4, hh * 128)
                for hh in range(2, 6):
                    fm_group(2, (hh - 2) * 128, wuk, b_wuk, 2, 4, hh * 128)
                for pr in range(4):
                    fm_group(3, pr * 128, wqi, b_wqi, 4, 0, pr * 128)
                for pr in range(4, 8):
                    fm_group(4, (pr - 4) * 128, wqi, b_wqi, 4, 0, pr * 128)
                for rc in range(2):
                    mm(psF[:, 5, :], cT[:, 4 + rc, :], wuv[:, rc, 0:512], rc == 0, rc == 1, [b_wuv, b_cT], bF[5])
                ph.op(ph.act, lambda h: h.copy(out=fm[:, 0:4, :], in_=psF[:, 0, :]), reads=[bF[0]], writes=[b_fm])
                ph.op(ph.dve, lambda h: h.tensor_copy(out=fm[:, 4:6, :], in_=psF[:, 1, 0:256]), reads=[bF[1]], writes=[b_fm])
                ph.op(ph.dve, lambda h: h.tensor_copy(out=fm[:, 6:8, :], in_=psF[:, 1, 256:512]), reads=[bF[1]], writes=[b_fm])
                ph.op(ph.act, lambda h: h.copy(out=fm[:, 8:12, :], in_=psF[:, 2, :]), reads=[bF[2]], writes=[b_fm])
                ph.op(ph.dve, lambda h: h.tensor_copy(out=fm[:, 12:16, :], in_=psF[:, 3, :]), reads=[bF[3]], writes=[b_fm])
                ph.op(ph.act, lambda h: h.copy(out=fm[:, 16:20, :], in_=psF[:, 4, :]), reads=[bF[4]], writes=[b_fm])
                ph.op(ph.dve, lambda h: h.tensor_copy(out=vbs[:, 0:512], in_=psF[:, 5, :]), reads=[bF[5]], writes=[b_vbs])
                for rc in range(2):
                    mm(psF[:, 0, 0:256], cT[:, 4 + rc, :], wuv[:, rc, 512:768], rc == 0, rc == 1, [b_wuv, b_cT], bF[0])
                ph.op(ph.act, lambda h: h.copy(out=vbs[:, 512:768], in_=psF[:, 0, 0:256]), reads=[bF[0]], writes=[b_vbs])
                for g in range(4):
                    mm(psF[:, 1, g * 128:(g + 1) * 128], vn[:, g * 128:(g + 1) * 128], cws[:, g * 128:(g + 1) * 128], True, True, [b_vn, b_cws], bF[1])
                ph.op(ph.dve, lambda h: h.tensor_tensor(out=tmp[:], in0=psF[:, 1, :], in1=vb3[:, 1024:1536], op=ALU.add), reads=[bF[1], b_vb3], writes=[b_tmp])
                ph.op(ph.dve, lambda h: h.tensor_tensor(out=fm[:, 20:24, :], in0=tmp[:], in1=uT[:], op=ALU.mult), reads=[b_tmp, b_uT], writes=[b_fm])
                tk = slice(i * 128, (i + 1) * 128)
                ph.store(self.qbT[:, :, tk].rearrange("h p t -> p h t"), fm[:, 0:6, :], b_fm)
                ph.store(self.kbT[:, :, tk].rearrange("h p t -> p h t"), fm[:, 6:12, :], b_fm)
                ph.store(self.qidxT[:, :, tk].rearrange("h p t -> p h t"), fm[:, 12:20, :], b_fm)
                ph.store(self.mixT[12:16, :, tk].rearrange("h p t -> p h t"), fm[:, 20:24, :], b_fm)
                ph.store(self.vb[tk, :], vbs[:], b_vbs)
                ph.store(self.kidxT[:, tk], kix[:], b_kix)
                ph.store(self.widx[tk, :], wix[:], b_wix)
            ph.emit(st)


    def attn(self, l, kind):
        nc, S, nb, ng = self.nc, self.S, self.nb, self.ng
        isA = kind == "A"
        qT_d, kT_d, v_d = (self.qaT, self.kaT, self.va) if isA else (self.qbT, self.kbT, self.vb)
        H0 = 0 if isA else 6
        nmap = 2 if isA else 1
        scale = 64 ** -0.5 if isA else 128 ** -0.5
        lam_init = 0.8 - 0.6 * math.exp(-0.3 * l)
        with ExitStack() as st:
            ph, sb, pst, cs = self.begin(f"att{kind}{l}", st, want=("ident", "causnegT"))
            ident, b_ident = cs["ident"]
            cneg, b_cneg = cs["causnegT"]
            kT = sb("kT", [128, 6, S], BF16); b_kT = Buf()
            ph.load(kT[:], kT_d.rearrange("h p t -> p h t"), b_kT)
            v1 = sb("v1", [128, nb, 6, 129], BF16); b_v1 = Buf()
            ph.op(ph.pool, lambda h: h.memset(v1[:, :, :, 128:129], 1.0), writes=[b_v1])
            for j0 in range(nb):
                ph.dma(v1[:, j0, :, 0:128], v_d[j0 * 128:(j0 + 1) * 128, :].rearrange("p (h d) -> p h d", d=128), writes=[b_v1], sbuf=b_v1)
            b31 = sb("b31", [128, 12], F32); b_b31 = Buf()
            ph.load(b31[:], self.E("b31"), b_b31)
            braw = sb("braw", [128, 6, 2, 128], F32); b_braw = Buf()
            ph.load(braw[:], self.E("biasT")[:, H0 * 256:(H0 + 6) * 256].rearrange("p (h k q) -> p h k q", h=6, k=2), b_braw)
            bt = sb("bt", [128, 6, 2, 128], BF16); b_bt = Buf()
            for hh in range(6):
                ph.op(ph.dve, lambda h, hh=hh: h.tensor_tensor(out=braw[:, hh, 0, :], in0=braw[:, hh, 0, :], in1=cneg[:], op=ALU.add), reads=[b_cneg], writes=[b_braw])
                ph.op(ph.dve, lambda h, hh=hh: h.tensor_scalar(out=bt[:, hh, :, :], in0=braw[:, hh, :, :], scalar1=b31[:, H0 + hh:H0 + hh + 1], scalar2=1.0 / scale, op0=ALU.subtract, op1=ALU.mult),
                      reads=[b_braw, b_b31], writes=[b_bt])
            sm = sb("sm", [128, 32], F32); b_sm = Buf()
            if isA:
                vbl = sb("vbl", [128, 256], F32); b_vbl = Buf()
                ph.load(vbl[:], self.E("vecB")[l, :, VB_LAM:VB_LAM + 256], b_vbl)
                pr = sb("pr", [128, 128], F32); b_pr = Buf()
                ph.op(ph.dve, lambda h: h.tensor_tensor(out=pr[:, 0:64], in0=vbl[:, 0:64], in1=vbl[:, 64:128], op=ALU.mult), reads=[b_vbl], writes=[b_pr])
                ph.op(ph.dve, lambda h: h.tensor_tensor(out=pr[:, 64:128], in0=vbl[:, 128:192], in1=vbl[:, 192:256], op=ALU.mult), reads=[b_vbl], writes=[b_pr])
                ph.op(ph.dve, lambda h: h.reduce_sum(out=sm[:, 0:1], in_=pr[:, 0:64], axis=AX.X), reads=[b_pr], writes=[b_sm])
                ph.op(ph.dve, lambda h: h.reduce_sum(out=sm[:, 1:2], in_=pr[:, 64:128], axis=AX.X), reads=[b_pr], writes=[b_sm])
                ph.op(ph.act, lambda h: h.activation(out=sm[:, 2:4], in_=sm[:, 0:2], func=AF.Exp), reads=[b_sm], writes=[b_sm])
                ph.op(ph.dve, lambda h: h.tensor_tensor(out=sm[:, 4:5], in0=sm[:, 2:3], in1=sm[:, 3:4], op=ALU.subtract), reads=[b_sm], writes=[b_sm])
                ph.op(ph.dve, lambda h: h.tensor_scalar(out=sm[:, 5:6], in0=sm[:, 4:5], scalar1=lam_init, scalar2=-1.0, op0=ALU.add, op1=ALU.mult), reads=[b_sm], writes=[b_sm])
            b_qg = [Buf(), Buf()]
            if not isA:
                qg = [sb(f"qg{i}", [128, 6, 512], BF16) for i in range(2)]
            if not isA:
                mt = [sb(f"mt{i}", [128, nb, 512], BF16) for i in range(2)]; b_mt = [Buf(), Buf()]
            pts = [sb(f"pt{i}", [128, 512], BF16) for i in range(3)]; b_pt = [Buf() for _ in range(3)]
            o0 = sb("o0", [128, 4, 128], F32); b_o0 = Buf()
            on = sb("on", [128, 4, 128], BF16); b_on = Buf()
            junkf = sb("junkf", [128, 128], F32); b_junk = Buf()
            outT = [sb(f"outT{i}", [128, 512], BF16) for i in range(2)]; b_outT = [Buf(), Buf()]
            psS = pst("psS", [128, 3, 512], F32); b_psS = [Buf() for _ in range(3)]
            psA = pst("psA", [128, 4, 512], F32); b_psA = [Buf() for _ in range(4)]
            psT = pst("psT", [128, 1024], BF16); b_psT = Buf()
            rows = lambda m: slice(m * 64, (m + 1) * 64) if isA else slice(0, 128)

            if isA:
                qz = [[sb(f"qz{m}_{i}", [128, 6, 512], BF16) for i in range(2)] for m in range(2)]
                for i in range(2):
                    ph.op(ph.pool, lambda h, i=i: h.memset(qz[0][i][64:128, :, :], 0.0), writes=[b_qg[i]])
                    ph.op(ph.pool, lambda h, i=i: h.memset(qz[1][i][0:64, :, :], 0.0), writes=[b_qg[i]])

            def ldg(G):
                if isA:
                    srcq = qT_d[:, :, G * 512:(G + 1) * 512].rearrange("h p t -> p h t")
                    ph.dma(qz[0][G % 2][0:64, :, :], srcq[0:64], writes=[b_qg[G % 2]], sbuf=b_qg[G % 2])
                    ph.dma(qz[1][G % 2][64:128, :, :], srcq[64:128], writes=[b_qg[G % 2]], sbuf=b_qg[G % 2])
                else:
                    ph.load(qg[G % 2][:], qT_d[:, :, G * 512:(G + 1) * 512].rearrange("h p t -> p h t"), b_qg[G % 2])
                if not isA:
                    nj = 4 * G + 4
                    ph.load(mt[G % 2][:, 0:nj, :], self.MTd[G, :, 0:nj, :], b_mt[G % 2])

            cst = [0]
            cout = [0]
            ldg(0)
            for G in range(ng):
                if G + 1 < ng:
                    ldg(G + 1)
                bq = b_qg[G % 2]
                nj = 4 * G + 4
                steps = [(hh, m, j) for hh in range(6) for m in range(nmap) for j in range(nj)]
                slot = {}

                def emit_qk(idx, G=G, bq=bq):
                    hh, m, j = steps[idx]
                    c0 = max(0, j - 4 * G)
                    cols = slice(c0 * 128, 512)
                    sbk = cst[0] % 3; cst[0] += 1
                    slot[idx] = sbk
                    q_ = qz[m][G % 2] if isA else qg[G % 2]
                    near = [(k, 4 * G + k - j) for k in range(4) if 0 <= 4 * G + k - j <= 1]
                    ph.op(ph.pe, lambda h: h.matmul(psS[:, sbk, cols], lhsT=kT[:, hh, j * 128:(j + 1) * 128], rhs=q_[:, hh, cols], start=True, stop=(len(near) == 0)),
                          reads=[b_kT, bq], writes=[b_psS[sbk]], pe_acc=True)
                    for ii, (k, kd) in enumerate(near):
                        ph.op(ph.pe, lambda h, k=k, kd=kd, ii=ii: h.matmul(psS[:, sbk, k * 128:(k + 1) * 128], lhsT=ident[:], rhs=bt[:, hh, kd, :], start=False, stop=(ii == len(near) - 1)),
                              reads=[b_ident, b_bt], writes=[b_psS[sbk]], pe_acc=True)

                pending = []
                emit_qk(0)
                for idx, (hh, m, j) in enumerate(steps):
                    if idx + 1 < len(steps):
                        emit_qk(idx + 1)
                    aset = ((G * 6 + hh) * nmap + m) % 2
                    accb = (2 * aset, 2 * aset + 1)
                    c0 = max(0, j - 4 * G)
                    cols = slice(c0 * 128, 512)
                    sbk = slot[idx]
                    pt, bp = pts[sbk], b_pt[sbk]
                    ph.op(ph.act, lambda h, pt=pt, sbk=sbk, cols=cols, hh=hh: h.activation(out=pt[:, cols], in_=psS[:, sbk, cols], func=AF.Exp, bias=b31[:, H0 + hh:H0 + hh + 1], scale=scale),
                          reads=[b_psS[sbk], b_b31], writes=[bp])
                    if not isA:
                        mt_ = mt[G % 2]
                        ph.op(ph.dve, lambda h, pt=pt, cols=cols, j=j, mt_=mt_: h.tensor_tensor(out=pt[:, cols], in0=pt[:, cols], in1=mt_[:, j, cols], op=ALU.mult),
                              reads=[b_mt[G % 2]], writes=[bp])
                    for k in range(c0, 4):
                        ab = accb[k // 2]
                        ph.op(ph.pe, lambda h, ab=ab, k=k, pt=pt, j=j, hh=hh, G=G: h.matmul(psA[:, ab, (k % 2) * 129:(k % 2) * 129 + 129], lhsT=pt[:, k * 128:(k + 1) * 128], rhs=v1[:, j, hh, :], start=(j == 0 and k % 2 == 0), stop=(j == 4 * G + k), skip_group_check=True),
                              reads=[bp, b_v1], writes=[b_psA[ab]], pe_acc=True)
                    if pending and idx - pending[0][0] >= 3:
                        pending.pop(0)[1]()
                    if j == nj - 1:
                        for k in range(4):
                            ab = accb[k // 2]; o = (k % 2) * 129
                            ph.op(ph.dve, lambda h, ab=ab, o=o, k=k: h.reciprocal(out=sm[:, 8 + k:9 + k], in_=psA[:, ab, o + 128:o + 129]), reads=[b_psA[ab]], writes=[b_sm])
                            if isA and m == 0:
                                ph.op(ph.dve, lambda h, ab=ab, o=o, k=k: h.tensor_scalar(out=o0[:, k, :], in0=psA[:, ab, o:o + 128], scalar1=sm[:, 8 + k:9 + k], scalar2=None, op0=ALU.mult), reads=[b_psA[ab], b_sm], writes=[b_o0])
                            elif isA:
                                ph.op(ph.dve, lambda h, k=k: h.tensor_tensor(out=sm[:, 12 + k:13 + k], in0=sm[:, 8 + k:9 + k], in1=sm[:, 5:6], op=ALU.mult), reads=[b_sm], writes=[b_sm])
                                ph.op(ph.dve, lambda h, ab=ab, o=o, k=k: h.scalar_tensor_tensor(out=o0[:, k, :], in0=psA[:, ab, o:o + 128], scalar=sm[:, 12 + k:13 + k], in1=o0[:, k, :], op0=ALU.mult, op1=ALU.add), reads=[b_psA[ab], b_sm], writes=[b_o0])
                                ph.op(ph.dve, lambda h, k=k: h.tensor_tensor(out=junkf[:], in0=o0[:, k, :], in1=o0[:, k, :], op=ALU.mult), reads=[b_o0], writes=[b_junk])
                                ph.op(ph.dve, lambda h, k=k: h.reduce_sum(out=sm[:, 16 + k:17 + k], in_=junkf[:], axis=AX.X), reads=[b_junk], writes=[b_sm])
                            else:
                                ph.op(ph.dve, lambda h, ab=ab, o=o, k=k: h.tensor_scalar(out=on[:, k, :], in0=psA[:, ab, o:o + 128], scalar1=sm[:, 8 + k:9 + k], scalar2=None, op0=ALU.mult), reads=[b_psA[ab], b_sm], writes=[b_on])
                        if isA and m == 0:
                            continue

                        def epi2(hh=hh, G=G):
                            if isA:
                                ph.op(ph.act, lambda h: h.activation(out=sm[:, 20:24], in_=sm[:, 16:20], func=AF.Ln, scale=1.0 / 128, bias=self.eps_ap), reads=[b_sm, self.b_eps], writes=[b_sm])
                                ph.op(ph.act, lambda h: h.activation(out=sm[:, 24:28], in_=sm[:, 20:24], func=AF.Exp, scale=-0.5), reads=[b_sm], writes=[b_sm])
                                for k in range(4):
                                    ph.op(ph.dve, lambda h, k=k: h.tensor_scalar(out=on[:, k, :], in0=o0[:, k, :], scalar1=sm[:, 24 + k:25 + k], scalar2=None, op0=ALU.mult), reads=[b_o0, b_sm], writes=[b_on])
                            for k in range(4):
                                ph.op(ph.pe, lambda h, k=k: h.transpose(out=psT[:, k * 128:(k + 1) * 128], in_=on[:, k, :], identity=ident[:]), reads=[b_on, b_ident], writes=[b_psT], pe_acc=True)
                            oi = cout[0] % 2; cout[0] += 1
                            ph.op(ph.dve, lambda h, oi=oi: h.tensor_copy(out=outT[oi][:], in_=psT[:, 0:512]), reads=[b_psT], writes=[b_outT[oi]])
                            ph.store(self.mixT[H0 + hh, :, G * 512:(G + 1) * 512], outT[oi][:], b_outT[oi])

                        pending.append((idx, epi2))
                while pending:
                    pending.pop(0)[1]()
            ph.emit(st)


    def bindex(self, l):
        nc, S, nb, ng = self.nc, self.S, self.nb, self.ng
        with ExitStack() as st:
            ph, sb, pst, cs = self.begin(f"bidx{l}", st, want=("ident", "causneg", "pow2"))
            ident, b_ident = cs["ident"]
            cneg, b_cneg = cs["causneg"]
            pow2, b_pow2 = cs["pow2"]
            kx = sb("kx", [128, S], BF16); b_kx = Buf()
            ph.dma(kx[0:64, :], self.kidxT, writes=[b_kx], sbuf=b_kx)
            ph.dma(kx[64:128, :], self.kidxT, writes=[b_kx], sbuf=b_kx)
            b_qi = [Buf(), Buf()]
            qz = [[sb(f"qz{e}_{i}", [128, 8, 128], BF16) for i in range(2)] for e in range(2)]
            for i in range(2):
                ph.op(ph.pool, lambda h, i=i: h.memset(qz[0][i][64:128, :, :], 0.0), writes=[b_qi[i]])
                ph.op(ph.pool, lambda h, i=i: h.memset(qz[1][i][0:64, :, :], 0.0), writes=[b_qi[i]])
            wi = [sb(f"wi{i}", [128, 16], F32) for i in range(2)]; b_wi = [Buf(), Buf()]
            wab = [sb(f"wab{i}", [128, 16], F32) for i in range(2)]; b_wab = [Buf(), Buf()]
            wsg = [sb(f"wsg{i}", [128, 16], F32) for i in range(2)]
            dsg = [sb(f"dsg{i}", [128, 16, 128], BF16) for i in range(2)]; b_dsg = [Buf(), Buf()]
            scs = [sb(f"sc{i}", [128, S], F32) for i in range(2)]; b_scs = [Buf(), Buf()]
            NR = 4
            rl = [sb(f"rl{i}", [128, 512], BF16) for i in range(NR)]; b_rl = [Buf() for _ in range(NR)]
            junkb = sb("junkb", [128, S], BF16); b_junkb = Buf()
            msk = sb("msk", [128, S], BF16); b_msk = Buf()
            MTg = sb("MTg", [128, nb, 512], BF16); b_MTg = Buf()
            ph.op(ph.pool, lambda h: h.memset(MTg[:], 0.0), writes=[b_MTg])
            sm = sb("sm", [128, 64], F32); b_sm = Buf()
            psD = pst("psD", [128, 3, 512], F32); b_psD = [Buf() for _ in range(3)]
            psC = pst("psC", [128, 2, 512], F32); b_psC = [Buf(), Buf()]
            psT = pst("psT", [128, 2, 1024], BF16); b_psT = [Buf(), Buf()]
            WH = 16

            def ldq(i):
                tk = slice(i * 128, (i + 1) * 128)
                srcq = self.qidxT[:, :, tk].rearrange("h p t -> p h t")
                ph.dma(qz[0][i % 2][0:64, :, :], srcq[0:64], writes=[b_qi[i % 2]], sbuf=b_qi[i % 2])
                ph.dma(qz[1][i % 2][64:128, :, :], srcq[64:128], writes=[b_qi[i % 2]], sbuf=b_qi[i % 2])
                ph.load(wi[i % 2][:], self.widx[tk, :], b_wi[i % 2])

            cd = [0]
            cr = [0]
            cc = [0]
            ct = [0]

            def scores(i):
                p_ = i % 2
                bq, w_, bw = b_qi[p_], wi[p_], b_wi[p_]
                sc, b_sc = scs[p_], b_scs[p_]
                Si = (i + 1) * 128
                ph.op(ph.dve, lambda h: h.tensor_scalar(out=wab[p_][:], in0=w_[:], scalar1=-1.0, scalar2=None, op0=ALU.mult), reads=[bw], writes=[b_wab[p_]])
                ph.op(ph.dve, lambda h: h.tensor_tensor(out=wab[p_][:], in0=wab[p_][:], in1=w_[:], op=ALU.max), reads=[bw], writes=[b_wab[p_]])
                ph.op(ph.dve, lambda h: h.tensor_scalar(out=wsg[p_][:], in0=w_[:], scalar1=0.0, scalar2=2.0, op0=ALU.is_ge, op1=ALU.mult), reads=[bw], writes=[b_dsg[p_]])
                ph.op(ph.dve, lambda h: h.tensor_scalar(out=wsg[p_][:], in0=wsg[p_][:], scalar1=-1.0, scalar2=None, op0=ALU.add), writes=[b_dsg[p_]])
                for g in range(16):
                    ph.op(ph.dve, lambda h, g=g: h.tensor_scalar(out=dsg[p_][:, g, :], in0=ident[:], scalar1=wsg[p_][:, g:g + 1], scalar2=None, op0=ALU.mult), reads=[b_ident], writes=[b_dsg[p_]])
                def chunk(c):
                    n = min(512, Si - c * 512)
                    k0 = c * 512
                    cb = cc[0] % 2; cc[0] += 1
                    slots = {}

                    def dots(g):
                        db = cd[0] % 3; cd[0] += 1
                        slots[g] = db
                        ph.op(ph.pe, lambda h: h.matmul(psD[:, db, 0:n], lhsT=qz[g % 2][p_][:, g // 2, :], rhs=kx[:, k0:k0 + n], start=True, stop=True),
                              reads=[bq, b_kx], writes=[b_psD[db]], pe_acc=True)

                    dots(0)
                    for g in range(16):
                        if g + 1 < 16:
                            dots(g + 1)
                        db = slots[g]
                        r_ = cr[0] % NR; cr[0] += 1
                        ph.op(ph.act, lambda h, db=db, r_=r_, g=g: h.activation(out=rl[r_][:, 0:n], in_=psD[:, db, 0:n], func=AF.Relu, scale=wab[p_][:, g:g + 1]), reads=[b_psD[db], b_wab[p_]], writes=[b_rl[r_]])
                        ph.op(ph.pe, lambda h, r_=r_, g=g: h.matmul(psC[:, cb, 0:n], lhsT=dsg[p_][:, g, :], rhs=rl[r_][:, 0:n], start=(g == 0), stop=(g == 15)),
                              reads=[b_dsg[p_], b_rl[r_]], writes=[b_psC[cb]], pe_acc=True)
                    ph.op(ph.act, lambda h: h.copy(out=sc[:, k0:k0 + n], in_=psC[:, cb, 0:n]), reads=[b_psC[cb]], writes=[b_sc])

                for c in range((Si + 511) // 512):
                    chunk(c)

            def select(i):
                p_ = i % 2
                sc, b_sc = scs[p_], b_scs[p_]
                Si = (i + 1) * 128
                ph.op(ph.dve, lambda h: h.tensor_tensor(out=sc[:, i * 128:(i + 1) * 128], in0=sc[:, i * 128:(i + 1) * 128], in1=cneg[:], op=ALU.add), reads=[b_cneg], writes=[b_sc])
                if i < 2:
                    ph.op(ph.dve, lambda h: h.memset(sm[:, 13:14], -1e29), writes=[b_sm])
                else:
                    ph.op(ph.dve, lambda h: h.max(out=sm[:, 0:8], in_=sc[:, 0:Si]), reads=[b_sc], writes=[b_sm])
                    ph.op(ph.dve, lambda h: h.tensor_reduce(out=sm[:, 8:9], in_=sc[:, 0:i * 128], op=ALU.min, axis=AX.X), reads=[b_sc], writes=[b_sm])
                    ph.op(ph.dve, lambda h: h.tensor_tensor(out=sm[:, 9:10], in0=sm[:, 0:1], in1=sm[:, 8:9], op=ALU.subtract), reads=[b_sm], writes=[b_sm])
                    ph.op(ph.dve, lambda h: h.tensor_scalar(out=sm[:, WH:WH + NBIS + 1], in0=pow2[:, 0:NBIS + 1], scalar1=sm[:, 9:10], scalar2=None, op0=ALU.mult), reads=[b_sm, b_pow2], writes=[b_sm])
                    ph.op(ph.dve, lambda h: h.tensor_tensor(out=sm[:, 10:11], in0=sm[:, 8:9], in1=sm[:, WH:WH + 1], op=ALU.add), reads=[b_sm], writes=[b_sm])
                    for k in range(NBIS):
                        ph.op(ph.dve, lambda h: h.tensor_scalar(out=junkb[:, 0:Si], in0=sc[:, 0:Si], scalar1=sm[:, 10:11], scalar2=0.0, op0=ALU.is_ge, op1=ALU.add, accum_out=sm[:, 11:12]),
                              reads=[b_sc, b_sm], writes=[b_junkb, b_sm])
                        ph.op(ph.dve, lambda h: h.tensor_scalar(out=sm[:, 12:13], in0=sm[:, 11:12], scalar1=float(TOPK), scalar2=0.5, op0=ALU.is_ge, op1=ALU.subtract), reads=[b_sm], writes=[b_sm])
                        ph.op(ph.dve, lambda h, k=k: h.scalar_tensor_tensor(out=sm[:, 10:11], in0=sm[:, WH + k:WH + k + 1], scalar=sm[:, 12:13], in1=sm[:, 10:11], op0=ALU.mult, op1=ALU.add), reads=[b_sm], writes=[b_sm])
                    ph.op(ph.dve, lambda h: h.tensor_tensor(out=sm[:, 13:14], in0=sm[:, 10:11], in1=sm[:, WH + NBIS:WH + NBIS + 1], op=ALU.subtract), reads=[b_sm], writes=[b_sm])
                ph.op(ph.dve, lambda h: h.tensor_scalar(out=msk[:, 0:Si], in0=sc[:, 0:Si], scalar1=sm[:, 13:14], scalar2=None, op0=ALU.is_ge), reads=[b_sc, b_sm], writes=[b_msk])

            def transposes(i):
                kq = i % 4
                for j0 in range(0, i + 1, 8):
                    nj = min(8, i + 1 - j0)
                    tb = ct[0] % 2; ct[0] += 1
                    for jj in range(nj):
                        j = j0 + jj
                        ph.op(ph.pe, lambda h, tb=tb, jj=jj, j=j: h.transpose(out=psT[:, tb, jj * 128:(jj + 1) * 128], in_=msk[:, j * 128:(j + 1) * 128], identity=ident[:]),
                              reads=[b_msk, b_ident], writes=[b_psT[tb]], pe_acc=True)
                    ph.op(ph.dve, lambda h, tb=tb, nj=nj, j0=j0, kq=kq: h.tensor_copy(out=MTg[:, j0:j0 + nj, kq * 128:(kq + 1) * 128], in_=psT[:, tb, 0:nj * 128].rearrange("p (j q) -> p j q", q=128)),
                          reads=[b_psT[tb]], writes=[b_MTg])
                if kq == 3:
                    G = i // 4
                    ph.store(self.MTd[G, :, 0:4 * G + 4, :], MTg[:, 0:4 * G + 4, :], b_MTg)

            ldq(0)
            scores(0)
            for i in range(nb):
                if i + 1 < nb:
                    ldq(i + 1)
                    scores(i + 1)
                select(i)
                transposes(i)
            ph.emit(st)

    def memkv(self, l):
        nc = self.nc
        with ExitStack() as st:
            ph, sb, pst, cs = self.begin(f"mkv{l}", st)
            ident, b_ident = cs["ident"]
            vp = sb("vp", [128, NVP], F32); b_vp = Buf()
            ph.load(vp[:], self.E("vecP")[l], b_vp)
            wkv = sb("wkv", [128, NDC, 1024], BF16); b_wkv = Buf()
            stg = [sb(f"stg{i}", [128, 1024], F32) for i in range(2)]; b_stg = [Buf(), Buf()]
            for dc in range(NDC):
                s_ = dc % 2
                ph.load(stg[s_][:], self.E("x_wkv")[l, dc * 128:(dc + 1) * 128, :], b_stg[s_])
                ph.op(ph.dve, lambda h, s_=s_, dc=dc: h.tensor_scalar(out=wkv[:, dc, :], in0=stg[s_][:], scalar1=vp[:, VP_MEMG + dc:VP_MEMG + dc + 1], scalar2=None, op0=ALU.mult),
                      reads=[b_stg[s_], b_vp], writes=[b_wkv])
            hts = [sb(f"ht{i}", [128, D], F32) for i in range(2)]; b_ht = [Buf(), Buf()]
            junk = sb("junk", [128, D], BF16); b_junk = Buf()
            sm = sb("sm", [128, 4], F32); b_sm = Buf()
            xn = sb("xn", [128, D], BF16); b_xn = Buf()
            xnT = sb("xnT", [128, NDC, 128], BF16); b_xnT = Buf()
            kst = sb("kst", [128, 4, 128], BF16); b_kst = Buf()
            vst = sb("vst", [128, 512], BF16); b_vst = Buf()
            psB = pst("psB", [128, 2, 1024], BF16); b_psB = [Buf(), Buf()]
            psF = pst("psF", [128, 2, 512], F32); bF = [Buf(), Buf()]
            for i in range(2):
                ph.load(hts[i][:], self.E("mem")[i * 128:(i + 1) * 128, :], b_ht[i])
            for i in range(2):
                ht, bh = hts[i], b_ht[i]
                self.rms_tile(ph, ht[:], bh, junk[:], b_junk, sm[:, 0:1], sm[:, 1:2], sm[:, 2:3], b_sm, D)
                ph.op(ph.dve, lambda h, ht=ht: h.tensor_scalar(out=xn[:], in0=ht[:], scalar1=sm[:, 2:3], scalar2=None, op0=ALU.mult), reads=[bh, b_sm], writes=[b_xn])
                for dc in range(NDC):
                    ph.op(ph.pe, lambda h, dc=dc: h.transpose(out=psB[:, dc // 8, (dc % 8) * 128:(dc % 8 + 1) * 128], in_=xn[:, dc * 128:(dc + 1) * 128], identity=ident[:]),
                          reads=[b_xn, b_ident], writes=[b_psB[dc // 8]], pe_acc=True)
                for k in range(2):
                    ph.op(ph.act, lambda h, k=k: h.copy(out=xnT[:, k * 8:(k + 1) * 8, :], in_=psB[:, k, :]), reads=[b_psB[k]], writes=[b_xnT])
                for hh in range(4):
                    for dc in range(NDC):
                        ph.op(ph.pe, lambda h, hh=hh, dc=dc: h.matmul(psF[:, 0, hh * 128:(hh + 1) * 128], lhsT=wkv[:, dc, hh * 128:(hh + 1) * 128], rhs=xnT[:, dc, :], start=(dc == 0), stop=(dc == NDC - 1)),
                              reads=[b_wkv, b_xnT], writes=[bF[0]], pe_acc=True)
                for dc in range(NDC):
                    ph.op(ph.pe, lambda h, dc=dc: h.matmul(psF[:, 1, :], lhsT=xnT[:, dc, :], rhs=wkv[:, dc, 512:1024], start=(dc == 0), stop=(dc == NDC - 1)),
                          reads=[b_wkv, b_xnT], writes=[bF[1]], pe_acc=True)
                ph.op(ph.act, lambda h: h.copy(out=kst[:], in_=psF[:, 0, :]), reads=[bF[0]], writes=[b_kst])
                ph.op(ph.dve, lambda h: h.tensor_copy(out=vst[:], in_=psF[:, 1, :]), reads=[bF[1]], writes=[b_vst])
                ph.store(self.kmT[:, :, i * 128:(i + 1) * 128], kst[:], b_kst)
                ph.store(self.vm[i * 128:(i + 1) * 128, :], vst[:], b_vst)
            ph.emit(st)

    def p3(self, l):
        nc, S, nb = self.nc, self.S, self.nb
        lam_init = 0.8 - 0.6 * math.exp(-0.3 * l)
        xscale = 128 ** -0.5
        hsrc = self.E("x") if l == 0 else self.h
        with ExitStack() as st:
            ph, sb, pst, cs = self.begin(f"p3{l}", st)
            ident, b_ident = cs["ident"]
            vp = sb("vp", [128, NVP], F32); b_vp = Buf()
            ph.load(vp[:], self.E("vecP")[l], b_vp)
            sg = sb("sg", [128, 1], F32); b_sg = Buf()
            ph.op(ph.dve, lambda h: h.tensor_scalar(out=sg[:], in0=vp[:, VP_SUBG:VP_SUBG + 1], scalar1=1.0 - lam_init, scalar2=None, op0=ALU.mult), reads=[b_vp], writes=[b_sg])
            pg = sb("pg", [128, 2, D], F32); b_pg = Buf()
            ph.load(pg[:, 0, :], self.E("vecB")[l, :, VB_MIXPOST:VB_MIXPOST + D], b_pg)
            ph.load(pg[:, 1, :], self.E("vecB")[l, :, VB_XPOST:VB_XPOST + D], b_pg)
            wout = sb("wout", [128, NDC, D], BF16); b_wout = Buf()
            xwq = sb("xwq", [128, NDC, 512], BF16); b_xwq = Buf()
            xwo = sb("xwo", [128, 4, D], BF16); b_xwo = Buf()
            kmT = sb("kmT", [128, 4, 256], BF16); b_kmT = Buf()
            ph.load(kmT[:], self.kmT, b_kmT)
            vm1 = sb("vm1", [128, 2, 4, 129], BF16); b_vm1 = Buf()
            ph.op(ph.pool, lambda h: h.memset(vm1[:, :, :, 128:129], 1.0), writes=[b_vm1])
            for mc in range(2):
                ph.dma(vm1[:, mc, :, 0:128], self.vm[mc * 128:(mc + 1) * 128, :].rearrange("p (h d) -> p h d", d=128), writes=[b_vm1], sbuf=b_vm1)
            stg = [sb(f"stg{i}", [128, D], F32) for i in range(2)]; b_stg = [Buf(), Buf()]
            cnt = [0]

            def wload(dst, b_dst, srcap, n, scal):
                s_ = cnt[0] % 2; cnt[0] += 1
                ph.load(stg[s_][:, 0:n], srcap, b_stg[s_])
                if scal is None:
                    ph.op(ph.dve, lambda h: h.tensor_copy(out=dst, in_=stg[s_][:, 0:n]), reads=[b_stg[s_]], writes=[b_dst])
                else:
                    ph.op(ph.dve, lambda h: h.tensor_scalar(out=dst, in0=stg[s_][:, 0:n], scalar1=scal, scalar2=None, op0=ALU.mult), reads=[b_stg[s_], b_vp, b_sg], writes=[b_dst])

            for c in range(NDC):
                wload(wout[:, c, :], b_wout, self.E("w_out")[l, c * 128:(c + 1) * 128, :], D, sg[:, 0:1] if c < 6 else None)
            for dc in range(NDC):
                wload(xwq[:, dc, :], b_xwq, self.E("x_wq")[l, dc * 128:(dc + 1) * 128, :], 512, vp[:, VP_XPRE + dc:VP_XPRE + dc + 1])
            for hh in range(4):
                wload(xwo[:, hh, :], b_xwo, self.E("x_wo")[l, hh * 128:(hh + 1) * 128, :], D, None)
            hts = [sb(f"ht{i}", [128, D], F32) for i in range(2)]; b_ht = [Buf(), Buf()]
            mxs = [sb(f"mx{i}", [128, NDC, 128], BF16) for i in range(2)]; b_mx = [Buf(), Buf()]
            junk = sb("junk", [128, D], BF16); b_junk = Buf()
            tmp = sb("tmp", [128, D], F32); b_tmp = Buf()
            sm = sb("sm", [128, 24], F32); b_sm = Buf()
            xn = sb("xn", [128, D], BF16); b_xn = Buf()
            xnT = sb("xnT", [128, NDC, 128], BF16); b_xnT = Buf()
            qx = sb("qx", [128, 4, 128], BF16); b_qx = Buf()
            pl = sb("pl", [128, 8, 128], BF16); b_pl = Buf()
            on = sb("on", [128, 4, 128], BF16); b_on = Buf()
            oT = sb("oT", [128, 4, 128], BF16); b_oT = Buf()
            psY = pst("psY", [128, 4, 512], F32); b_psY = Buf()
            psB = pst("psB", [128, 2, 1024], BF16); b_psB = [Buf(), Buf()]
            psX = pst("psX", [128, 2, 512], F32); b_psX = Buf()

            def ld(i):
                tk = slice(i * 128, (i + 1) * 128)
                ph.load(hts[i % 2][:], hsrc[tk, :], b_ht[i % 2])
                ph.load(mxs[i % 2][:], self.mixT[:, :, tk].rearrange("c p t -> p c t"), b_mx[i % 2])

            def post(which, ht, bh):
                ph.op(ph.act, lambda h: h.activation(out=junk[:], in_=psY[:].rearrange("p a b -> p (a b)"), func=AF.Square, accum_out=sm[:, 0:1]), reads=[b_psY], writes=[b_junk, b_sm])
                ph.op(ph.act, lambda h: h.activation(out=sm[:, 1:2], in_=sm[:, 0:1], func=AF.Sqrt, scale=1.0 / D, bias=self.eps_ap), reads=[b_sm, self.b_eps], writes=[b_sm])
                ph.op(ph.dve, lambda h: h.reciprocal(out=sm[:, 2:3], in_=sm[:, 1:2]), reads=[b_sm], writes=[b_sm])
                ph.op(ph.dve, lambda h: h.scalar_tensor_tensor(out=tmp[:], in0=psY[:].rearrange("p a b -> p (a b)"), scalar=sm[:, 2:3], in1=pg[:, which, :], op0=ALU.mult, op1=ALU.mult), reads=[b_psY, b_sm, b_pg], writes=[b_tmp])
                ph.op(ph.dve, lambda h: h.tensor_tensor(out=ht[:], in0=tmp[:], in1=ht[:], op=ALU.add), reads=[b_tmp], writes=[bh])

            ld(0)
            for i in range(nb):
                if i + 1 < nb:
                    ld(i + 1)
                ht, bh, mx, bm = hts[i % 2], b_ht[i % 2], mxs[i % 2], b_mx[i % 2]
                for n in range(4):
                    for c in range(NDC):
                        ph.op(ph.pe, lambda h, n=n, c=c, mx=mx: h.matmul(psY[:, n, :], lhsT=mx[:, c, :], rhs=wout[:, c, n * 512:(n + 1) * 512], start=(c == 0), stop=(c == NDC - 1)),
                              reads=[bm, b_wout], writes=[b_psY], pe_acc=True)
                post(0, ht, bh)
                self.rms_tile(ph, ht[:], bh, junk[:], b_junk, sm[:, 3:4], sm[:, 4:5], sm[:, 5:6], b_sm, D)
                ph.op(ph.dve, lambda h, ht=ht: h.tensor_scalar(out=xn[:], in0=ht[:], scalar1=sm[:, 5:6], scalar2=None, op0=ALU.mult), reads=[bh, b_sm], writes=[b_xn])
                for dc in range(NDC):
                    ph.op(ph.pe, lambda h, dc=dc: h.transpose(out=psB[:, dc // 8, (dc % 8) * 128:(dc % 8 + 1) * 128], in_=xn[:, dc * 128:(dc + 1) * 128], identity=ident[:]),
                          reads=[b_xn, b_ident], writes=[b_psB[dc // 8]], pe_acc=True)
                for k in range(2):
                    ph.op(ph.act, lambda h, k=k: h.copy(out=xnT[:, k * 8:(k + 1) * 8, :], in_=psB[:, k, :]), reads=[b_psB[k]], writes=[b_xnT])
                for hh in range(4):
                    for dc in range(NDC):
                        ph.op(ph.pe, lambda h, hh=hh, dc=dc: h.matmul(psX[:, 0, hh * 128:(hh + 1) * 128], lhsT=xwq[:, dc, hh * 128:(hh + 1) * 128], rhs=xnT[:, dc, :], start=(dc == 0), stop=(dc == NDC - 1)),
                              reads=[b_xwq, b_xnT], writes=[b_psX], pe_acc=True)
                ph.op(ph.act, lambda h: h.copy(out=qx[:], in_=psX[:, 0, :]), reads=[b_psX], writes=[b_qx])
                for hh in range(4):
                    for mc in range(2):
                        u = hh * 2 + mc
                        ph.op(ph.pe, lambda h, hh=hh, mc=mc, u=u: h.matmul(psX[:, u // 4, (u % 4) * 128:(u % 4 + 1) * 128], lhsT=kmT[:, hh, mc * 128:(mc + 1) * 128], rhs=qx[:, hh, :], start=True, stop=True),
                              reads=[b_kmT, b_qx], writes=[b_psX], pe_acc=True)
                ph.op(ph.act, lambda h: h.activation(out=pl[:], in_=psX[:].rearrange("p a b -> p (a b)"), func=AF.Exp, scale=xscale), reads=[b_psX], writes=[b_pl])
                for hh in range(4):
                    for mc in range(2):
                        ph.op(ph.pe, lambda h, hh=hh, mc=mc: h.matmul(psX[:, hh // 2, (hh % 2) * 129:(hh % 2) * 129 + 129], lhsT=pl[:, hh * 2 + mc, :], rhs=vm1[:, mc, hh, :], start=(hh % 2 == 0 and mc == 0), stop=(mc == 1), skip_group_check=True),
                              reads=[b_pl, b_vm1], writes=[b_psX], pe_acc=True)
                for hh in range(4):
                    o = (hh % 2) * 129
                    ph.op(ph.dve, lambda h, hh=hh, o=o: h.reciprocal(out=sm[:, 8 + hh:9 + hh], in_=psX[:, hh // 2, o + 128:o + 129]), reads=[b_psX], writes=[b_sm])
                    ph.op(ph.dve, lambda h, hh=hh, o=o: h.tensor_scalar(out=on[:, hh, :], in0=psX[:, hh // 2, o:o + 128], scalar1=sm[:, 8 + hh:9 + hh], scalar2=None, op0=ALU.mult), reads=[b_psX, b_sm], writes=[b_on])
                for hh in range(4):
                    ph.op(ph.pe, lambda h, hh=hh: h.transpose(out=psB[:, 0, hh * 128:(hh + 1) * 128], in_=on[:, hh, :], identity=ident[:]), reads=[b_on, b_ident], writes=[b_psB[0]], pe_acc=True)
                ph.op(ph.act, lambda h: h.copy(out=oT[:], in_=psB[:, 0, 0:512]), reads=[b_psB[0]], writes=[b_oT])
                for n in range(4):
                    for hh in range(4):
                        ph.op(ph.pe, lambda h, n=n, hh=hh: h.matmul(psY[:, n, :], lhsT=oT[:, hh, :], rhs=xwo[:, hh, n * 512:(n + 1) * 512], start=(hh == 0), stop=(hh == 3)),
                              reads=[b_oT, b_xwo], writes=[b_psY], pe_acc=True)
                post(1, ht, bh)
                ph.store(self.h[i * 128:(i + 1) * 128, :], ht[:], bh)
            ph.emit(st)


    def p3a(self, l):
        nc, S, nb = self.nc, self.S, self.nb
        lam_init = 0.8 - 0.6 * math.exp(-0.3 * l)
        hsrc = self.E("x") if l == 0 else self.h
        with ExitStack() as st:
            ph, sb, pst, cs = self.begin(f"p3a{l}", st, want=())
            vp = sb("vp", [128, NVP], F32); b_vp = Buf()
            ph.load(vp[:], self.E("vecP")[l], b_vp)
            sg = sb("sg", [128, 1], F32); b_sg = Buf()
            ph.op(ph.dve, lambda h: h.tensor_scalar(out=sg[:], in0=vp[:, VP_SUBG:VP_SUBG + 1], scalar1=1.0 - lam_init, scalar2=None, op0=ALU.mult), reads=[b_vp], writes=[b_sg])
            pg = sb("pg", [128, D], F32); b_pg = Buf()
            ph.load(pg[:], self.E("vecB")[l, :, VB_MIXPOST:VB_MIXPOST + D], b_pg)
            wout = sb("wout", [128, NDC, D], BF16); b_wout = Buf()
            stg = [sb(f"stg{i}", [128, D], F32) for i in range(2)]; b_stg = [Buf(), Buf()]
            for c in range(NDC):
                s_ = c % 2
                ph.load(stg[s_][:], self.E("w_out")[l, c * 128:(c + 1) * 128, :], b_stg[s_])
                if c < 6:
                    ph.op(ph.dve, lambda h, s_=s_, c=c: h.tensor_scalar(out=wout[:, c, :], in0=stg[s_][:], scalar1=sg[:, 0:1], scalar2=None, op0=ALU.mult), reads=[b_stg[s_], b_sg], writes=[b_wout])
                else:
                    ph.op(ph.act, lambda h, s_=s_, c=c: h.copy(out=wout[:, c, :], in_=stg[s_][:]), reads=[b_stg[s_]], writes=[b_wout])
            hts = [sb(f"ht{i}", [128, D], F32) for i in range(2)]; b_ht = [Buf(), Buf()]
            mxs = [sb(f"mx{i}", [128, NDC, 128], BF16) for i in range(2)]; b_mx = [Buf(), Buf()]
            junk = sb("junk", [128, D], BF16); b_junk = Buf()
            tmp = sb("tmp", [128, D], F32); b_tmp = Buf()
            sm = sb("sm", [128, 4], F32); b_sm = Buf()
            psY = pst("psY", [128, 8, 512], F32); b_psY = [Buf(), Buf()]

            def ld(i):
                tk = slice(i * 128, (i + 1) * 128)
                ph.load(hts[i % 2][:], hsrc[tk, :], b_ht[i % 2])
                ph.load(mxs[i % 2][:], self.mixT[:, :, tk].rearrange("c p t -> p c t"), b_mx[i % 2])

            ld(0)
            for i in range(nb):
                if i + 1 < nb:
                    ld(i + 1)
                ht, bh, mx, bm = hts[i % 2], b_ht[i % 2], mxs[i % 2], b_mx[i % 2]
                ys = i % 2
                for n in range(4):
                    for c in range(NDC):
                        ph.op(ph.pe, lambda h, n=n, c=c, mx=mx, ys=ys: h.matmul(psY[:, ys * 4 + n, :], lhsT=mx[:, c, :], rhs=wout[:, c, n * 512:(n + 1) * 512], start=(c == 0), stop=(c == NDC - 1)),
                              reads=[bm, b_wout], writes=[b_psY[ys]], pe_acc=True)
                yv = psY[:, ys * 4:ys * 4 + 4, :].rearrange("p a b -> p (a b)")
                ph.op(ph.act, lambda h, yv=yv: h.activation(out=junk[:], in_=yv, func=AF.Square, accum_out=sm[:, 0:1]), reads=[b_psY[ys]], writes=[b_junk, b_sm])
                ph.op(ph.act, lambda h: h.activation(out=sm[:, 1:2], in_=sm[:, 0:1], func=AF.Sqrt, scale=1.0 / D, bias=self.eps_ap), reads=[b_sm, self.b_eps], writes=[b_sm])
                ph.op(ph.dve, lambda h: h.reciprocal(out=sm[:, 2:3], in_=sm[:, 1:2]), reads=[b_sm], writes=[b_sm])
                ph.op(ph.dve, lambda h, yv=yv: h.scalar_tensor_tensor(out=tmp[:], in0=yv, scalar=sm[:, 2:3], in1=pg[:], op0=ALU.mult, op1=ALU.mult), reads=[b_psY[ys], b_sm, b_pg], writes=[b_tmp])
                ph.op(ph.pool, lambda h, ht=ht: h.tensor_tensor(out=ht[:], in0=tmp[:], in1=ht[:], op=ALU.add), reads=[b_tmp], writes=[bh])
                ph.store(self.h[i * 128:(i + 1) * 128, :], ht[:], bh)
            ph.emit(st)

    def p3b(self, l):
        nc, S, nb = self.nc, self.S, self.nb
        xscale = 128 ** -0.5
        with ExitStack() as st:
            ph, sb, pst, cs = self.begin(f"p3b{l}", st)
            ident, b_ident = cs["ident"]
            vp = sb("vp", [128, NVP], F32); b_vp = Buf()
            ph.load(vp[:], self.E("vecP")[l], b_vp)
            pg = sb("pg", [128, D], F32); b_pg = Buf()
            ph.load(pg[:], self.E("vecB")[l, :, VB_XPOST:VB_XPOST + D], b_pg)
            xwq = sb("xwq", [128, NDC, 512], BF16); b_xwq = Buf()
            xwo = sb("xwo", [128, 4, D], BF16); b_xwo = Buf()
            kmT = sb("kmT", [128, 4, 256], BF16); b_kmT = Buf()
            ph.load(kmT[:], self.kmT, b_kmT)
            vm1 = sb("vm1", [128, 2, 4, 129], BF16); b_vm1 = Buf()
            ph.op(ph.pool, lambda h: h.memset(vm1[:, :, :, 128:129], 1.0), writes=[b_vm1])
            for mc in range(2):
                ph.dma(vm1[:, mc, :, 0:128], self.vm[mc * 128:(mc + 1) * 128, :].rearrange("p (h d) -> p h d", d=128), writes=[b_vm1], sbuf=b_vm1)
            stg = [sb(f"stg{i}", [128, D], F32) for i in range(2)]; b_stg = [Buf(), Buf()]
            cnt = [0]
            for dc in range(NDC):
                s_ = cnt[0] % 2; cnt[0] += 1
                ph.load(stg[s_][:, 0:512], self.E("x_wq")[l, dc * 128:(dc + 1) * 128, :], b_stg[s_])
                ph.op(ph.dve, lambda h, s_=s_, dc=dc: h.tensor_scalar(out=xwq[:, dc, :], in0=stg[s_][:, 0:512], scalar1=vp[:, VP_XPRE + dc:VP_XPRE + dc + 1], scalar2=None, op0=ALU.mult), reads=[b_stg[s_], b_vp], writes=[b_xwq])
            for hh in range(4):
                s_ = cnt[0] % 2; cnt[0] += 1
                ph.load(stg[s_][:], self.E("x_wo")[l, hh * 128:(hh + 1) * 128, :], b_stg[s_])
                ph.op(ph.act, lambda h, s_=s_, hh=hh: h.copy(out=xwo[:, hh, :], in_=stg[s_][:]), reads=[b_stg[s_]], writes=[b_xwo])
            hts = [sb(f"ht{i}", [128, D], F32) for i in range(3)]; b_ht = [Buf() for _ in range(3)]
            junk = sb("junk", [128, D], BF16); b_junk = Buf()
            tmp = sb("tmp", [128, D], F32); b_tmp = Buf()
            sm = sb("sm", [128, 24], F32); b_sm = Buf()
            sm2 = sb("sm2", [128, 8], F32); b_sm2 = Buf()
            xn = sb("xn", [128, D], BF16); b_xn = Buf()
            xnT = sb("xnT", [128, NDC, 128], BF16); b_xnT = Buf()
            qx = sb("qx", [128, 4, 128], BF16); b_qx = Buf()
            pl = sb("pl", [128, 8, 128], BF16); b_pl = Buf()
            on = sb("on", [128, 4, 128], BF16); b_on = Buf()
            oT = sb("oT", [128, 4, 128], BF16); b_oT = Buf()
            psY = pst("psY", [128, 4, 512], F32); b_psY = Buf()
            psB = pst("psB", [128, 2, 1024], BF16); b_psB = [Buf(), Buf()]
            psL = pst("psL", [128, 2, 512], F32); b_psL = Buf()
            psQ = psL[:, 0, :]; b_psQ = b_psL
            psX = psB; b_psX = b_psB[0]

            def ld(i):
                ph.load(hts[i % 3][:], self.h[i * 128:(i + 1) * 128, :], b_ht[i % 3])

            def front(i):
                ht, bh = hts[i % 3], b_ht[i % 3]
                self.rms_tile(ph, ht[:], bh, junk[:], b_junk, sm2[:, 0:1], sm2[:, 1:2], sm2[:, 2:3], b_sm2, D)
                ph.op(ph.dve, lambda h: h.tensor_scalar(out=xn[:], in0=ht[:], scalar1=sm2[:, 2:3], scalar2=None, op0=ALU.mult), reads=[bh, b_sm2], writes=[b_xn])
                for dc in range(NDC):
                    ph.op(ph.pe, lambda h, dc=dc: h.transpose(out=psB[:, dc // 8, (dc % 8) * 128:(dc % 8 + 1) * 128], in_=xn[:, dc * 128:(dc + 1) * 128], identity=ident[:]),
                          reads=[b_xn, b_ident], writes=[b_psB[dc // 8]], pe_acc=True)
                for k in range(2):
                    ph.op(ph.act, lambda h, k=k: h.copy(out=xnT[:, k * 8:(k + 1) * 8, :], in_=psB[:, k, :]), reads=[b_psB[k]], writes=[b_xnT])
                for hh in range(4):
                    for dc in range(NDC):
                        ph.op(ph.pe, lambda h, hh=hh, dc=dc: h.matmul(psQ[:, hh * 128:(hh + 1) * 128], lhsT=xwq[:, dc, hh * 128:(hh + 1) * 128], rhs=xnT[:, dc, :], start=(dc == 0), stop=(dc == NDC - 1)),
                              reads=[b_xwq, b_xnT], writes=[b_psQ], pe_acc=True)
                ph.op(ph.act, lambda h: h.copy(out=qx[:], in_=psQ), reads=[b_psQ], writes=[b_qx])

            def mid(i):
                for hh in range(4):
                    for mc in range(2):
                        u = hh * 2 + mc
                        ph.op(ph.pe, lambda h, hh=hh, mc=mc, u=u: h.matmul(psL[:, u // 4, (u % 4) * 128:(u % 4 + 1) * 128], lhsT=kmT[:, hh, mc * 128:(mc + 1) * 128], rhs=qx[:, hh, :], start=True, stop=True),
                              reads=[b_kmT, b_qx], writes=[b_psL], pe_acc=True)
                ph.op(ph.act, lambda h: h.activation(out=pl[:], in_=psL[:].rearrange("p a b -> p (a b)"), func=AF.Exp, scale=xscale), reads=[b_psL], writes=[b_pl])
                for hh in range(4):
                    for mc in range(2):
                        ph.op(ph.pe, lambda h, hh=hh, mc=mc: h.matmul(psL[:, hh // 2, (hh % 2) * 129:(hh % 2) * 129 + 129], lhsT=pl[:, hh * 2 + mc, :], rhs=vm1[:, mc, hh, :], start=(hh % 2 == 0 and mc == 0), stop=(mc == 1), skip_group_check=True),
                              reads=[b_pl, b_vm1], writes=[b_psL], pe_acc=True)
                for hh in range(4):
                    o = (hh % 2) * 129
                    ph.op(ph.dve, lambda h, hh=hh, o=o: h.reciprocal(out=sm[:, 8 + hh:9 + hh], in_=psL[:, hh // 2, o + 128:o + 129]), reads=[b_psL], writes=[b_sm])
                    ph.op(ph.dve, lambda h, hh=hh, o=o: h.tensor_scalar(out=on[:, hh, :], in0=psL[:, hh // 2, o:o + 128], scalar1=sm[:, 8 + hh:9 + hh], scalar2=None, op0=ALU.mult), reads=[b_psL, b_sm], writes=[b_on])
                for hh in range(4):
                    ph.op(ph.pe, lambda h, hh=hh: h.transpose(out=psX[:, 0, hh * 128:(hh + 1) * 128], in_=on[:, hh, :], identity=ident[:]), reads=[b_on, b_ident], writes=[b_psX], pe_acc=True)
                ph.op(ph.act, lambda h: h.copy(out=oT[:], in_=psX[:, 0, 0:512]), reads=[b_psX], writes=[b_oT])
                for n in range(4):
                    for hh in range(4):
                        ph.op(ph.pe, lambda h, n=n, hh=hh: h.matmul(psY[:, n, :], lhsT=oT[:, hh, :], rhs=xwo[:, hh, n * 512:(n + 1) * 512], start=(hh == 0), stop=(hh == 3)),
                              reads=[b_oT, b_xwo], writes=[b_psY], pe_acc=True)

            def tail(i):
                ht, bh = hts[i % 3], b_ht[i % 3]
                yv = psY[:].rearrange("p a b -> p (a b)")
                ph.op(ph.act, lambda h: h.activation(out=junk[:], in_=yv, func=AF.Square, accum_out=sm[:, 0:1]), reads=[b_psY], writes=[b_junk, b_sm])
                ph.op(ph.act, lambda h: h.activation(out=sm[:, 1:2], in_=sm[:, 0:1], func=AF.Sqrt, scale=1.0 / D, bias=self.eps_ap), reads=[b_sm, self.b_eps], writes=[b_sm])
                ph.op(ph.dve, lambda h: h.reciprocal(out=sm[:, 2:3], in_=sm[:, 1:2]), reads=[b_sm], writes=[b_sm])
                ph.op(ph.dve, lambda h: h.scalar_tensor_tensor(out=tmp[:], in0=yv, scalar=sm[:, 2:3], in1=pg[:], op0=ALU.mult, op1=ALU.mult), reads=[b_psY, b_sm, b_pg], writes=[b_tmp])
                ph.op(ph.pool, lambda h: h.tensor_tensor(out=ht[:], in0=tmp[:], in1=ht[:], op=ALU.add), reads=[b_tmp], writes=[bh])
                ph.store(self.h[i * 128:(i + 1) * 128, :], ht[:], bh)

            ld(0)
            if nb > 1:
                ld(1)
            front(0)
            for i in range(nb):
                if i + 2 < nb:
                    ld(i + 2)
                mid(i)
                if i + 1 < nb:
                    front(i + 1)
                tail(i)
            ph.emit(st)

    def ffn(self, l):
        nc, S = self.nc, self.S
        TG = min(1024, S)
        ngr = S // TG
        nt = TG // 128
        nh = TG // 512
        with ExitStack() as st:
            ph, sb, pst, cs = self.begin(f"ffn{l}", st)
            ident, b_ident = cs["ident"]
            gB = sb("gB", [128, D], F32); b_gB = Buf()
            ph.load(gB[:], self.E("vecB")[l, :, VB_FPRE:VB_FPRE + D], b_gB)
            xnT = sb("xnT", [128, NDC, TG], BF16); b_xnT = Buf()
            actT = sb("actT", [128, NFC, TG], BF16); b_actT = Buf()
            gus = [sb(f"gus{i}", [128, 2, NDC, 128], F32) for i in range(2)]; b_gus = [Buf(), Buf()]
            gub = [sb(f"gub{i}", [128, 2, NDC, 128], BF16) for i in range(2)]; b_gub = [Buf(), Buf()]
            NWD = 6
            wds = [sb(f"wds{i}", [128, 256], F32) for i in range(NWD)]; b_wds = [Buf() for _ in range(NWD)]
            wdb = [sb(f"wdb{i}", [128, 256], BF16) for i in range(NWD)]; b_wdb = [Buf() for _ in range(NWD)]
            ht = sb("ht", [128, D], F32); b_ht = Buf()
            xn = sb("xn", [128, D], BF16); b_xn = Buf()
            sgt = sb("sgt", [128, 512], F32); b_sgt = Buf()
            yst = sb("yst", [128, nt, 256], F32); b_yst = Buf()
            sm = sb("sm", [128, 4], F32); b_sm = Buf()
            psB = pst("psB", [128, 1024], BF16); b_psB = Buf()
            psGU = pst("psGU", [128, 3, 512], F32); b_psGU = [Buf() for _ in range(3)]
            psD = pst("psD", [128, 4, 512], F32); b_psD = [Buf() for _ in range(4)]
            hsrc = self.h
            cgu = [0]
            cwd = [0]
            cbank = [0]
            for gr in range(ngr):
                hring = [(ht[:], b_ht), (yst[:].rearrange("p t c -> p (t c)"), b_yst)]

                def lda(t, gr=gr):
                    r0 = gr * TG + t * 128
                    ph.load(hring[t % 2][0], hsrc[r0:r0 + 128, :], hring[t % 2][1])

                lda(0)
                for t in range(nt):
                    if t + 1 < nt:
                        lda(t + 1)
                    hta, bha = hring[t % 2]
                    self.rms_tile(ph, hta, bha, xn[:], b_xn, sm[:, 0:1], sm[:, 1:2], sm[:, 2:3], b_sm, D)
                    ph.op(ph.dve, lambda h, hta=hta: h.scalar_tensor_tensor(out=xn[:], in0=hta, scalar=sm[:, 2:3], in1=gB[:], op0=ALU.mult, op1=ALU.mult), reads=[bha, b_sm, b_gB], writes=[b_xn])
                    for k in range(2):
                        for dd in range(8):
                            dc = k * 8 + dd
                            ph.op(ph.pe, lambda h, dd=dd, dc=dc: h.transpose(out=psB[:, dd * 128:(dd + 1) * 128], in_=xn[:, dc * 128:(dc + 1) * 128], identity=ident[:]),
                                  reads=[b_xn, b_ident], writes=[b_psB], pe_acc=True)
                        ph.op(ph.act, lambda h, k=k, t=t: h.copy(out=xnT[:, k * 8:(k + 1) * 8, t * 128:(t + 1) * 128], in_=psB[:].rearrange("p (a b) -> p a b", b=128)), reads=[b_psB], writes=[b_xnT])
                for fc in range(NFC):
                    s_ = cgu[0] % 2; cgu[0] += 1
                    for c in range(2):
                        ph.dma(gus[s_][:, c, :, :], self.E("f_w_gu")[l, :, c, fc * 128:(fc + 1) * 128].rearrange("(dc p) f -> p dc f", p=128), writes=[b_gus[s_]], sbuf=b_gus[s_])
                    ph.op(ph.pool, lambda h, s_=s_: h.tensor_copy(out=gub[s_][:], in_=gus[s_][:]), reads=[b_gus[s_]], writes=[b_gub[s_]])
                    for hf in range(nh):
                        bks = []
                        for c in range(2):
                            bk = cbank[0] % 3; cbank[0] += 1
                            bks.append(bk)
                            for dc in range(NDC):
                                ph.op(ph.pe, lambda h, bk=bk, c=c, dc=dc, s_=s_, hf=hf: h.matmul(psGU[:, bk, :], lhsT=gub[s_][:, c, dc, :], rhs=xnT[:, dc, hf * 512:(hf + 1) * 512], start=(dc == 0), stop=(dc == NDC - 1)),
                                      reads=[b_gub[s_], b_xnT], writes=[b_psGU[bk]], pe_acc=True)
                        ph.op(ph.act, lambda h, bk=bks[0]: h.activation(out=sgt[:], in_=psGU[:, bk, :], func=AF.Silu), reads=[b_psGU[bks[0]]], writes=[b_sgt])
                        ph.op(ph.dve, lambda h, bk=bks[1], fc=fc, hf=hf: h.tensor_tensor(out=actT[:, fc, hf * 512:(hf + 1) * 512], in0=sgt[:], in1=psGU[:, bk, :], op=ALU.mult), reads=[b_sgt, b_psGU[bks[1]]], writes=[b_actT])
                for dcol in range(8):
                    for fc in range(NFC):
                        w_ = cwd[0] % NWD; cwd[0] += 1
                        ph.load(wds[w_][:], self.E("f_w_down")[l, fc * 128:(fc + 1) * 128, dcol * 256:(dcol + 1) * 256], b_wds[w_])
                        if cwd[0] % 2 == 0:
                            ph.op(ph.act, lambda h, w_=w_: h.copy(out=wdb[w_][:], in_=wds[w_][:]), reads=[b_wds[w_]], writes=[b_wdb[w_]])
                        else:
                            ph.op(ph.dve, lambda h, w_=w_: h.tensor_copy(out=wdb[w_][:], in_=wds[w_][:]), reads=[b_wds[w_]], writes=[b_wdb[w_]])
                        for t in range(nt):
                            ph.op(ph.pe, lambda h, t=t, fc=fc, w_=w_: h.matmul(psD[:, t // 2, (t % 2) * 256:(t % 2) * 256 + 256], lhsT=actT[:, fc, t * 128:(t + 1) * 128], rhs=wdb[w_][:], start=(fc == 0 and t % 2 == 0), stop=(fc == NFC - 1), skip_group_check=True),
                                  reads=[b_actT, b_wdb[w_]], writes=[b_psD[t // 2]], pe_acc=True)
                    for b in range(nt // 2):
                        if b % 2 == 0:
                            ph.op(ph.dve, lambda h, b=b: h.tensor_copy(out=yst[:, 2 * b:2 * b + 2, :], in_=psD[:, b, :].rearrange("p (a c) -> p a c", c=256)), reads=[b_psD[b]], writes=[b_yst])
                        else:
                            ph.op(ph.act, lambda h, b=b: h.copy(out=yst[:, 2 * b:2 * b + 2, :], in_=psD[:, b, :].rearrange("p (a c) -> p a c", c=256)), reads=[b_psD[b]], writes=[b_yst])
                    ph.store(self.yscr[gr * TG:(gr + 1) * TG, dcol * 256:(dcol + 1) * 256].rearrange("(t p) c -> p t c", p=128), yst[:], b_yst)
            ph.emit(st)

    def ffn_post(self, l, dst):
        nc, S, nb = self.nc, self.S, self.nb
        with ExitStack() as st:
            ph, sb, pst, cs = self.begin(f"fpo{l}", st, want=())
            pg = sb("pg", [128, D], F32); b_pg = Buf()
            ph.load(pg[:], self.E("vecB")[l, :, VB_FPOST:VB_FPOST + D], b_pg)
            hts = [sb(f"ht{i}", [128, D], F32) for i in range(2)]; b_ht = [Buf(), Buf()]
            yts = [sb(f"yt{i}", [128, D], F32) for i in range(2)]; b_yt = [Buf(), Buf()]
            junk = sb("junk", [128, D], BF16); b_junk = Buf()
            sm = sb("sm", [128, 4], F32); b_sm = Buf()

            def ld(i):
                tk = slice(i * 128, (i + 1) * 128)
                ph.load(hts[i % 2][:], self.h[tk, :], b_ht[i % 2])
                ph.load(yts[i % 2][:], self.yscr[tk, :], b_yt[i % 2])

            ld(0)
            for i in range(nb):
                if i + 1 < nb:
                    ld(i + 1)
                ht, bh, yt, by = hts[i % 2], b_ht[i % 2], yts[i % 2], b_yt[i % 2]
                self.rms_tile(ph, yt[:], by, junk[:], b_junk, sm[:, 0:1], sm[:, 1:2], sm[:, 2:3], b_sm, D)
                ph.op(ph.dve, lambda h, yt=yt: h.scalar_tensor_tensor(out=yt[:], in0=yt[:], scalar=sm[:, 2:3], in1=pg[:], op0=ALU.mult, op1=ALU.mult), reads=[b_sm, b_pg], writes=[by])
                ph.op(ph.pool, lambda h, yt=yt, ht=ht: h.tensor_tensor(out=ht[:], in0=yt[:], in1=ht[:], op=ALU.add), reads=[by], writes=[bh])
                ph.store(dst[i * 128:(i + 1) * 128, :], ht[:], bh)
            ph.emit(st)


def rel_bucket_np(dist):
    n = np.maximum(dist, 0)
    nf = np.maximum(n, 1).astype(np.float32)
    large = 16 + (np.log(nf / np.float32(16)) / np.float32(math.log(128 / 16)) * np.float32(16)).astype(np.int32)
    large = np.minimum(large, 31)
    return np.where(n < 16, n, large)


def host_consts():
    c = np.zeros((128, 128 * 4 + 32), np.float32)
    c[:, 0:128] = np.eye(128, dtype=np.float32)
    s = np.arange(128)[:, None]
    t = np.arange(128)[None, :]
    c[:, 128:256] = (s <= t).astype(np.float32)
    c[:, 256:384] = np.where(t <= s, 0.0, -1e30)
    c[:, 384:512] = np.where(s <= t, 0.0, NEG)
    c[:, 512:544] = (0.5 ** (np.arange(32) + 1))[None, :]
    return c


def prep_inputs(inp, S, L):
    f = lambda a: np.ascontiguousarray(np.asarray(a, dtype=np.float32)[:L])
    shared = {}
    shared["w_in"] = f(inp["w_in"])
    shared["w_out"] = f(inp["w_out"])
    shared["b_w_uq"] = f(inp["b_w_uq"]).reshape(L, 512, 768)
    shared["b_w_qidx"] = f(inp["b_w_qidx"]).reshape(L, 512, 1024)
    shared["b_w_uk"] = f(inp["b_w_uk"]).reshape(L, 256, 768)
    shared["b_w_uv"] = f(inp["b_w_uv"]).reshape(L, 256, 768)
    shared["cwsT"] = f(np.transpose(np.asarray(inp["c_w_s"], dtype=np.float32)[:L], (0, 3, 1, 2))).reshape(L, 128, 512)
    shared["x_wq"] = f(inp["x_wq"]).reshape(L, D, 512)
    shared["x_wkv"] = f(inp["x_wkv"]).reshape(L, D, 1024)
    shared["x_wo"] = f(inp["x_wo"]).reshape(L, 512, D)
    shared["f_w_gu"] = f(inp["f_w_gu"])
    shared["f_w_down"] = f(inp["f_w_down"])
    vp = np.zeros((L, 128, NVP), np.float32)
    vb = np.zeros((L, 128, NVB), np.float32)
    pp = lambda v, n: np.asarray(v, np.float32).reshape(n, 128).T
    for l in range(L):
        vp[l, :, VP_GIN:VP_GIN + 16] = pp(inp["mix_pre_g"][l], 16)
        vp[l, :, VP_CQ:VP_CQ + 4] = pp(inp["b_cq_g"][l], 4)
        vp[l, :, VP_CKV:VP_CKV + 2] = pp(inp["b_ckv_g"][l], 2)
        vp[l, :, VP_XPRE:VP_XPRE + 16] = pp(inp["x_pre_g"][l], 16)
        vp[l, :, VP_MEMG:VP_MEMG + 16] = pp(inp["mem_g"][l], 16)
        vp[l, :, VP_FPRE:VP_FPRE + 16] = pp(inp["f_pre_g"][l], 16)
        vp[l, :, VP_SUBG] = np.asarray(inp["a_sub_g"][l], np.float32)
        vb[l, :, VB_MIXPOST:VB_MIXPOST + D] = np.asarray(inp["mix_post_g"][l])[None, :]
        vb[l, :, VB_XPOST:VB_XPOST + D] = np.asarray(inp["x_post_g"][l])[None, :]
        vb[l, :, VB_FPOST:VB_FPOST + D] = np.asarray(inp["f_post_g"][l])[None, :]
        vb[l, :, VB_LNG:VB_LNG + 512] = np.asarray(inp["c_ln_g"][l])[None, :]
        vb[l, :, VB_LNB:VB_LNB + 512] = np.asarray(inp["c_ln_b"][l])[None, :]
        vb[l, :, VB_BS:VB_BS + 512] = np.asarray(inp["c_b_s"][l]).reshape(1, 512)
        vb[l, :, VB_LAM:VB_LAM + 256] = np.asarray(inp["a_lambda"][l]).reshape(1, 256)
        vb[l, :, VB_FPRE:VB_FPRE + D] = np.asarray(inp["f_pre_g"][l])[None, :]
    shared["vecP"] = vp
    shared["vecB"] = vb
    rb = np.asarray(inp["rel_bias"], np.float32)
    s = np.arange(128)[:, None]
    q = np.arange(128)[None, :]
    bt = np.zeros((128, 12, 2, 128), np.float32)
    for kind in range(2):
        idx = rel_bucket_np(128 * kind + q - s)
        bt[:, :, kind, :] = np.transpose(rb[idx], (0, 2, 1))
    shared["biasT"] = bt.reshape(128, 12 * 2 * 128)
    shared["b31"] = np.ascontiguousarray(np.broadcast_to(rb[31][None, :], (128, 12)))
    shared["consts"] = host_consts()
    return shared


_CACHE = {}


def kernel(**inputs):
    x = np.asarray(inputs["x"], np.float32)
    B, S, _ = x.shape
    L = int(np.asarray(inputs["w_in"]).shape[0])
    key = (S, L)
    if key not in _CACHE:
        _CACHE[key] = Kern(S, L).build()
    nc = _CACHE[key]
    shared = prep_inputs(inputs, S, L)
    mem = np.asarray(inputs["mem"], np.float32)
    ncore = B
    in_maps = []
    for c in range(ncore):
        m = dict(shared)
        m["x"] = np.ascontiguousarray(x[c])
        m["mem"] = np.ascontiguousarray(mem[c])
        in_maps.append(m)
    res = run_bass_kernel_spmd(nc, in_maps, core_ids=list(range(ncore)))
    out = np.stack([np.asarray(res.results[b]["out"], np.float32) for b in range(B)], axis=0)
    return out
```
